# Optimizing a Trainium2 kernel written in Bass

```python
import math
import jax, jax.numpy as jnp
from jax import lax
import numpy as np

D_MODEL = 1024
BATCH = 16
SEQ = 2048
DEPTH = 1

D_MIX = D_MODEL
D_SSM = D_MIX // 2
D_POOL = D_MIX - D_SSM
SSM_HEAD_DIM = 64
SSM_HEADS = D_SSM // SSM_HEAD_DIM
SSM_GROUPS = 2
SSM_STATE = 128
SSM_CONV = 4
SSM_CHUNK = 128
D_XBC = D_SSM + 2 * SSM_GROUPS * SSM_STATE
DT_MIN = 1e-3
DT_MAX = 1e-1
POOL_WINDOWS = (2, 4, 8, 16)
N_POOL_GROUPS = len(POOL_WINDOWS)
POOL_GROUP = D_POOL // N_POOL_GROUPS
D_IN_PROJ = D_SSM + D_XBC + SSM_HEADS + D_POOL
N_EXPERT_GROUPS = 4
EXPERTS_PER_GROUP = 4
N_EXPERTS = N_EXPERT_GROUPS * EXPERTS_PER_GROUP
TOP_K_INNER = 2
D_FF_EXPERT = 512
LN_EPS = 1e-5
RMS_EPS = 1e-5
DEEPNORM_ALPHA = (2.0 * DEPTH) ** 0.25
DEEPNORM_BETA = (8.0 * DEPTH) ** -0.25

kernel_name = "hybrid_ssd_pool_hmoe_deepnorm"


def layer_norm(x, g, b):
    xf = x.astype(jnp.float32)
    mu = jnp.mean(xf, axis=-1, keepdims=True)
    var = jnp.mean(jnp.square(xf - mu), axis=-1, keepdims=True)
    y = (xf - mu) * lax.rsqrt(var + LN_EPS)
    return (y * g.astype(jnp.float32) + b.astype(jnp.float32)).astype(x.dtype)


def causal_depthwise_conv(u, w, b):
    k_width = w.shape[0]
    seq = u.shape[1]
    up = jnp.pad(u, ((0, 0), (k_width - 1, 0), (0, 0)))
    out = b
    for k in range(k_width):
        out = out + up[:, k:k + seq, :] * w[k]
    return out


def segsum_decay(a):
    t = a.shape[-1]
    ae = jnp.broadcast_to(a[..., :, None], a.shape + (t,))
    strict = jnp.tril(jnp.ones((t, t), dtype=bool), -1)
    cs = jnp.cumsum(jnp.where(strict, ae, 0.0), axis=-2)
    lower = jnp.tril(jnp.ones((t, t), dtype=bool), 0)
    return jnp.where(lower, jnp.exp(cs), 0.0)


def ssd_chunked(xh, dt, a_head, bm, cm):
    bsz, seq, nh, hp = xh.shape
    ng, ns = bm.shape[2], bm.shape[3]
    nj = nh // ng
    q = SSM_CHUNK
    nc = seq // q
    x = (xh * dt[..., None]).reshape(bsz, nc, q, ng, nj, hp)
    a = (dt * a_head).reshape(bsz, nc, q, ng, nj).transpose(0, 3, 4, 1, 2)
    bc = bm.reshape(bsz, nc, q, ng, ns)
    cc = cm.reshape(bsz, nc, q, ng, ns)
    a_cum = jnp.cumsum(a, axis=-1)
    decay = segsum_decay(a)
    scores = jnp.einsum("bclgn,bcsgn->bgcls", cc, bc)
    y_diag = jnp.einsum("bgcls,bgjcls,bcsgjp->bclgjp", scores, decay, x)
    decay_states = jnp.exp(a_cum[..., -1:] - a_cum)
    states = jnp.einsum("bclgn,bgjcl,bclgjp->bcgjpn", bc, decay_states, x)
    chunk_decay = jnp.exp(a_cum[..., -1])

    def step(h, inp):
        s, d = inp
        return h * d[..., None, None] + s, h

    h0 = jnp.zeros((bsz, ng, nj, hp, ns), dtype=states.dtype)
    _, prev = lax.scan(step, h0, (jnp.moveaxis(states, 1, 0), jnp.moveaxis(chunk_decay, -1, 0)))
    prev = jnp.moveaxis(prev, 0, 1)
    y_off = jnp.einsum("bclgn,bcgjpn,bgjcl->bclgjp", cc, prev, jnp.exp(a_cum))
    return (y_diag + y_off).reshape(bsz, seq, nh, hp)


def multiscale_pool(u, w_pool, b_pool, scale):
    bsz, seq, _ = u.shape
    uf = u.astype(jnp.float32)
    cs = jnp.cumsum(uf, axis=1)
    pos = jnp.arange(seq, dtype=jnp.float32) + 1.0
    outs = []
    for i, w in enumerate(POOL_WINDOWS):
        sl = slice(i * POOL_GROUP, (i + 1) * POOL_GROUP)
        csg = cs[..., sl]
        lag = jnp.pad(csg[:, :seq - w], ((0, 0), (w, 0), (0, 0)))
        cnt = jnp.minimum(pos, float(w))[:, None]
        outs.append((csg - lag) / cnt - uf[..., sl])
    p = jnp.stack(outs, axis=2)
    mixed = jnp.einsum("blgc,gcd->blgd", p, w_pool.astype(jnp.float32)) + b_pool.astype(jnp.float32)
    return (mixed.reshape(bsz, seq, D_POOL) * scale.astype(jnp.float32)).astype(u.dtype)


def token_mixer(x, w_in, conv_w, conv_b, dt_bias, a_log, d_skip, ssm_norm_g,
                w_pool, b_pool, pool_scale, w_out):
    bsz, seq, _ = x.shape
    proj = x @ w_in
    s1, s2, s3 = D_SSM, D_SSM + D_XBC, D_SSM + D_XBC + SSM_HEADS
    z, xbc, dt_raw, u = proj[..., :s1], proj[..., s1:s2], proj[..., s2:s3], proj[..., s3:]
    xbc = jax.nn.silu(causal_depthwise_conv(xbc, conv_w, conv_b)).astype(jnp.float32)
    nbc = SSM_GROUPS * SSM_STATE
    xs = xbc[..., :D_SSM].reshape(bsz, seq, SSM_HEADS, SSM_HEAD_DIM)
    bm = xbc[..., D_SSM:D_SSM + nbc].reshape(bsz, seq, SSM_GROUPS, SSM_STATE)
    cm = xbc[..., D_SSM + nbc:].reshape(bsz, seq, SSM_GROUPS, SSM_STATE)
    dt = jax.nn.softplus(dt_raw.astype(jnp.float32) + dt_bias.astype(jnp.float32))
    a_head = -jnp.exp(a_log.astype(jnp.float32))
    y = ssd_chunked(xs, dt, a_head, bm, cm) + xs * d_skip.astype(jnp.float32)[:, None]
    y = y.reshape(bsz, seq, D_SSM) * jax.nn.silu(z.astype(jnp.float32))
    yg = y.reshape(bsz, seq, SSM_GROUPS, D_SSM // SSM_GROUPS)
    yg = yg * lax.rsqrt(jnp.mean(jnp.square(yg), axis=-1, keepdims=True) + RMS_EPS)
    y_ssd = (yg.reshape(bsz, seq, D_SSM) * ssm_norm_g.astype(jnp.float32)).astype(x.dtype)
    y_pool = multiscale_pool(u, w_pool, b_pool, pool_scale)
    return jnp.concatenate([y_ssd, y_pool], axis=-1) @ w_out


def hierarchical_moe(x, w_router_group, b_router_group, w_router_expert, b_router_expert,
                     w_gate, w_up, w_down):
    bsz, seq, d = x.shape
    xt = x.reshape(-1, d)
    xf = xt.astype(jnp.float32)
    g_logits = xf @ w_router_group.astype(jnp.float32) + b_router_group.astype(jnp.float32)
    g_prob = jax.nn.softmax(g_logits, axis=-1)
    g_w, g_idx = lax.top_k(g_prob, 1)
    e_logits = (xf @ w_router_expert.astype(jnp.float32) + b_router_expert.astype(jnp.float32))
    e_logits = e_logits.reshape(-1, N_EXPERT_GROUPS, EXPERTS_PER_GROUP)
    e_sel = jnp.take_along_axis(e_logits, g_idx[:, :, None], axis=1)[:, 0]
    top_v, top_i = lax.top_k(e_sel, TOP_K_INNER)
    comb = g_w * jax.nn.softmax(top_v, axis=-1)
    eid = g_idx * EXPERTS_PER_GROUP + top_i
    dense_w = jnp.sum(jax.nn.one_hot(eid, N_EXPERTS, dtype=jnp.float32) * comb[..., None], axis=1)
    dense_w = dense_w.astype(xt.dtype)
    y = jnp.zeros_like(xt)
    for e in range(N_EXPERTS):
        h = jax.nn.silu(xt @ w_gate[e]) * (xt @ w_up[e])
        y = y + dense_w[:, e:e + 1] * (h @ w_down[e])
    return y.reshape(bsz, seq, d)


def setup_inputs(seed: int = 0) -> dict:
    key = jax.random.key(seed)
    ks = jax.random.split(key, 32)
    f32 = jnp.float32

    def nrm(k, shape, scale):
        return jax.random.normal(k, shape, f32) * scale

    x = nrm(ks[0], (BATCH, SEQ, D_MODEL), 1.0)
    ln0_g = 1.0 + nrm(ks[1], (D_MODEL,), 0.02)
    ln0_b = nrm(ks[2], (D_MODEL,), 0.02)
    w_in = nrm(ks[3], (DEPTH, D_MODEL, D_IN_PROJ), D_MODEL ** -0.5)
    conv_w = nrm(ks[4], (DEPTH, SSM_CONV, D_XBC), SSM_CONV ** -0.5)
    conv_b = nrm(ks[5], (DEPTH, D_XBC), 0.02)
    uu = jax.random.uniform(ks[6], (DEPTH, SSM_HEADS), f32)
    dt0 = jnp.exp(uu * (math.log(DT_MAX) - math.log(DT_MIN)) + math.log(DT_MIN))
    dt_bias = dt0 + jnp.log(-jnp.expm1(-dt0))
    a_log = jnp.log(jax.random.uniform(ks[7], (DEPTH, SSM_HEADS), f32, 1.0, 16.0))
    d_skip = 1.0 + nrm(ks[8], (DEPTH, SSM_HEADS), 0.02)
    ssm_norm_g = 1.0 + nrm(ks[9], (DEPTH, D_SSM), 0.02)
    w_pool = nrm(ks[10], (DEPTH, N_POOL_GROUPS, POOL_GROUP, POOL_GROUP), POOL_GROUP ** -0.5)
    b_pool = nrm(ks[11], (DEPTH, N_POOL_GROUPS, POOL_GROUP), 0.02)
    pool_scale = 1.0 + nrm(ks[12], (DEPTH, D_POOL), 0.02)
    w_out = nrm(ks[13], (DEPTH, D_MIX, D_MODEL), D_MIX ** -0.5 * DEEPNORM_BETA)
    ln1_g = 1.0 + nrm(ks[14], (DEPTH, D_MODEL), 0.02)
    ln1_b = nrm(ks[15], (DEPTH, D_MODEL), 0.02)
    w_router_group = nrm(ks[16], (DEPTH, D_MODEL, N_EXPERT_GROUPS), D_MODEL ** -0.5)
    b_router_group = nrm(ks[17], (DEPTH, N_EXPERT_GROUPS), 0.01)
    w_router_expert = nrm(ks[18], (DEPTH, D_MODEL, N_EXPERTS), D_MODEL ** -0.5)
    b_router_expert = nrm(ks[19], (DEPTH, N_EXPERTS), 0.01)
    w_gate = nrm(ks[20], (DEPTH, N_EXPERTS, D_MODEL, D_FF_EXPERT), D_MODEL ** -0.5)
    w_up = nrm(ks[21], (DEPTH, N_EXPERTS, D_MODEL, D_FF_EXPERT), D_MODEL ** -0.5)
    w_down = nrm(ks[22], (DEPTH, N_EXPERTS, D_FF_EXPERT, D_MODEL), D_FF_EXPERT ** -0.5 * DEEPNORM_BETA)
    ln2_g = 1.0 + nrm(ks[23], (DEPTH, D_MODEL), 0.02)
    ln2_b = nrm(ks[24], (DEPTH, D_MODEL), 0.02)
    return {"x": x, "ln0_g": ln0_g, "ln0_b": ln0_b, "w_in": w_in, "conv_w": conv_w,
            "conv_b": conv_b, "dt_bias": dt_bias, "a_log": a_log, "d_skip": d_skip,
            "ssm_norm_g": ssm_norm_g, "w_pool": w_pool, "b_pool": b_pool,
            "pool_scale": pool_scale, "w_out": w_out, "ln1_g": ln1_g, "ln1_b": ln1_b,
            "w_router_group": w_router_group, "b_router_group": b_router_group,
            "w_router_expert": w_router_expert, "b_router_expert": b_router_expert,
            "w_gate": w_gate, "w_up": w_up, "w_down": w_down, "ln2_g": ln2_g, "ln2_b": ln2_b}


def reference(x, ln0_g, ln0_b, w_in, conv_w, conv_b, dt_bias, a_log, d_skip, ssm_norm_g,
              w_pool, b_pool, pool_scale, w_out, ln1_g, ln1_b, w_router_group, b_router_group,
              w_router_expert, b_router_expert, w_gate, w_up, w_down, ln2_g, ln2_b):
    x = layer_norm(x, ln0_g, ln0_b)
    for layer in range(DEPTH):
        mix = token_mixer(x, w_in[layer], conv_w[layer], conv_b[layer], dt_bias[layer],
                          a_log[layer], d_skip[layer], ssm_norm_g[layer], w_pool[layer],
                          b_pool[layer], pool_scale[layer], w_out[layer])
        x = layer_norm(DEEPNORM_ALPHA * x + mix, ln1_g[layer], ln1_b[layer])
        ffn = hierarchical_moe(x, w_router_group[layer], b_router_group[layer],
                               w_router_expert[layer], b_router_expert[layer],
                               w_gate[layer], w_up[layer], w_down[layer])
        x = layer_norm(DEEPNORM_ALPHA * x + ffn, ln2_g[layer], ln2_b[layer])
    return x
```

```python
import contextlib
import math

import numpy as np
import ml_dtypes

import concourse.bass as bass
import concourse.mybir as mybir
from concourse.bass_utils import run_bass_kernel_spmd

F32 = mybir.dt.float32
BF16 = mybir.dt.bfloat16
I32 = mybir.dt.int32
AF = mybir.ActivationFunctionType
ALU = mybir.AluOpType
AX = mybir.AxisListType

D = 1024
NCORES = 8
TOK_PER_CORE = 4096
UNIT = 1024
LN_EPS = 1e-5
RMS_EPS = 1e-5
ALPHA = 2.0 ** 0.25
NE = 16
WINDOWS = (2, 4, 8, 16)

C_ID, C_U, C_LS, C_MASK, C_AI, C_ONES, C_BAND = 0, 128, 256, 384, 512, 640, 768
C_MISC = 768 + 12 * 128
C_US = C_MISC + 64
C_S = C_US + 128
NCONST = C_S + 64
NCH = 32
TQ = 4
TS = 128 * TQ
NTILE = NE + (2 * TOK_PER_CORE) // TS
NSLOT = NTILE * TS
PP_G0, PP_B0, PP_G1, PP_B1, PP_CW, PP_CB, PP_PS, PP_PB = 0, 8, 16, 24, 32, 64, 72, 76
NPP = 80
RP_DTB, RP_ALOG, RP_DSKIP, RP_BR, RP_NG = 0, 8, 16, 24, 44
NRP = 556


class Buf:
    def __init__(self, t, dsem=None):
        self.t = t
        self.w = {}
        self.r = {}
        self.dsem = dsem
        self.dcount = 0


class Eng:
    def __init__(self, name, eng, sem):
        self.name, self.eng, self.sem = name, eng, sem
        self.count = 0
        self.waited = {}
        self.free = 0.0


class _FakeEng:
    def __getattr__(self, name):
        def f(*a, **k):
            k["_op"] = name
            k["_args"] = a
            return k
        return f


def _free_size(ap):
    try:
        n = 1
        for d in ap.shape[1:]:
            n *= int(d)
        return n
    except Exception:
        return 256


def _est_cost(kind, en, fn):
    fe = _FakeEng()
    try:
        if kind == "mm":
            tot = 0.0
            for f in fn:
                k = f(fe)
                n = _free_size(k.get("out"))
                src = k.get("lhsT", k.get("in_"))
                f32 = getattr(src, "dtype", None) == F32
                if k["_op"] == "transpose":
                    tot += 0.45 if f32 else 0.11
                elif f32:
                    tot += max(0.2, n / 600.0)
                else:
                    tot += max(0.06, n / 2400.0 + 0.01)
            return tot
        if kind == "dma":
            return 0.15 if en in ("sp", "act") else 1.2
        k = fn(fe)
        out = k.get("out", k.get("ap", k["_args"][0] if k["_args"] else None))
        n = _free_size(out)
        if en == "act":
            return 0.27 + n * 0.00065
        if en == "dve":
            return 0.1 + n / 960.0
        if en == "pool":
            return 0.3 + n * 0.002
    except Exception:
        pass
    return 0.5


SCHED = True
HOP_PE = 12.0
HOP = 3.0
DMA_LAT = 3.0


class _Rec:
    __slots__ = ("kind", "en", "fn", "reads", "writes", "pwrites", "nowait", "extra", "cost")


class Tracker:
    def __init__(self, nc, st):
        self.nc = nc
        self.st = st
        self.E = {}
        for name, eng in (("pe", nc.tensor), ("act", nc.scalar), ("dve", nc.vector),
                          ("pool", nc.gpsimd), ("sp", nc.sync)):
            self.E[name] = Eng(name, eng, st.enter_context(nc.semaphore("sem_" + name)))
        self.out_toks = {}
        self.rec = None
        self.fin = {}

    def record(self):
        self.rec = []

    def stop(self):
        r, self.rec = self.rec, None
        return r

    def _wait(self, e, toks, skip=None):
        for k, (sem, val) in toks.items():
            if k == skip:
                continue
            if e.waited.get(k, 0) >= val:
                continue
            e.eng.wait_ge(sem, val)
            e.waited[k] = val

    def _pre(self, e, reads, writes, pwrites, skip_self=False):
        for b in reads:
            self._wait(e, b.w)
        for b in writes:
            self._wait(e, b.w)
            self._wait(e, b.r)
        for b in pwrites:
            self._wait(e, b.w, skip=id(e.sem))
            self._wait(e, b.r)

    def _post(self, tok, reads, writes, pwrites):
        k = id(tok[0])
        for b in reads:
            b.r[k] = tok
        for b in writes:
            b.w = {k: tok}
            b.r = {}
        for b in pwrites:
            b.w[k] = tok

    def op(self, en, fn, reads=(), writes=(), pwrites=()):
        if self.rec is not None:
            r = _Rec(); r.kind, r.en, r.fn, r.reads, r.writes, r.pwrites, r.nowait, r.extra = "op", en, fn, reads, writes, pwrites, (), None
            r.cost = _est_cost("op", en, fn)
            self.rec.append(r)
            return None
        return self._op(en, fn, reads, writes, pwrites)

    def _op(self, en, fn, reads=(), writes=(), pwrites=()):
        e = self.E[en]
        self._pre(e, reads, writes, pwrites)
        inst = fn(e.eng)
        e.count += 1
        inst.then_inc(e.sem, 1)
        tok = (e.sem, e.count)
        self._post(tok, reads, writes, pwrites)
        return tok

    def mm(self, fns, reads=(), pwrites=()):
        if self.rec is not None:
            r = _Rec(); r.kind, r.en, r.fn, r.reads, r.writes, r.pwrites, r.nowait, r.extra = "mm", "pe", fns, reads, (), pwrites, (), None
            r.cost = _est_cost("mm", "pe", fns)
            self.rec.append(r)
            return None
        return self._mm(fns, reads, pwrites)

    def _mm(self, fns, reads=(), pwrites=()):
        e = self.E["pe"]
        self._pre(e, reads, (), pwrites)
        inst = None
        for fn in fns:
            inst = fn(e.eng)
        e.count += 1
        inst.then_inc(e.sem, 1)
        tok = (e.sem, e.count)
        self._post(tok, reads, (), pwrites)
        return tok

    def dma(self, qn, out, in_, reads=(), pwrites=(), nowait=(), semb=None, is_out=False, fn=None):
        if self.rec is not None:
            r = _Rec(); r.kind, r.en, r.fn, r.reads, r.writes, r.pwrites, r.nowait = "dma", qn, fn, reads, (), pwrites, nowait
            r.extra = (out, in_, semb, is_out)
            r.cost = _est_cost("dma", qn, fn)
            self.rec.append(r)
            return None
        return self._dma(qn, out, in_, reads, pwrites, nowait, semb, is_out, fn)

    def _ready(self, en, reads, writes, pwrites):
        t = 0.0
        own = id(self.E[en].sem)
        hop = HOP_PE if en == "pe" else HOP
        for b in reads:
            for k, (sem, val) in b.w.items():
                t = max(t, self.fin.get((k, val), 0.0) + (0.0 if k == own else hop))
        for b in list(writes) + list(pwrites):
            for d in (b.w, b.r):
                for k, (sem, val) in d.items():
                    t = max(t, self.fin.get((k, val), 0.0) + (0.0 if k == own else hop))
        return t

    def est_start(self, r):
        return max(self.E[r.en].free, self._ready(r.en, r.reads, r.writes, r.pwrites))

    def emit(self, r):
        e = self.E[r.en]
        start = self.est_start(r)
        if r.kind == "op":
            tok = self._op(r.en, r.fn, r.reads, r.writes, r.pwrites)
        elif r.kind == "mm":
            tok = self._mm(r.fn, r.reads, r.pwrites)
        else:
            out, in_, semb, is_out = r.extra
            tok = self._dma(r.en, out, in_, r.reads, r.pwrites, r.nowait, semb, is_out, r.fn)
        e.free = start + r.cost
        fin = e.free + (DMA_LAT if r.kind == "dma" else 0.0)
        self.fin[(id(tok[0]), tok[1])] = fin

    def _dma(self, qn, out, in_, reads=(), pwrites=(), nowait=(), semb=None, is_out=False, fn=None):
        e = self.E[qn]
        if semb is not None:
            sb = semb
        elif nowait:
            sb = next((b for b in reads if b.dsem is not None), nowait[0])
        elif pwrites:
            sb = pwrites[0]
        else:
            sb = reads[0]
        for b in reads:
            self._wait(e, b.w)
        for b in pwrites:
            self._wait(e, b.w, skip=id(sb.dsem))
            self._wait(e, b.r)
        inst = fn(e.eng) if fn is not None else e.eng.dma_start(out=out, in_=in_)
        sb.dcount += 16
        inst.then_inc(sb.dsem, 16)
        tok = (sb.dsem, sb.dcount)
        self._post(tok, reads, (), list(pwrites) + list(nowait))
        if is_out:
            self.out_toks[id(sb.dsem)] = tok
        return tok


def build(n_units=4, stop_after=4, debug=False):
    nc = bass.Bass("TRN2", target_bir_lowering=False)
    ntok = n_units * UNIT
    dt_ = lambda name, shape, dt=F32, kind="ExternalInput": nc.dram_tensor(name, shape, dt, kind=kind).ap()
    xc = dt_("xc", [TOK_PER_CORE, D])
    consts_d = dt_("consts", [128, NCONST])
    pp_d = dt_("pp", [128, NPP])
    rowp_d = dt_("rowp", [NRP])
    ln2_d = dt_("ln2rows", [2, D])
    wr_d = dt_("wr", [D, 20])
    identb_d = dt_("identb", [128, 128], BF16)
    w_in_d = dt_("w_in", [D, 2056])
    w_out_d = dt_("w_out", [D, D])
    w_pool_d = dt_("w_pool", [4, 128, 128])
    w_gate_d = dt_("w_gate", [NE * 256, 2048])
    w_up_d = dt_("w_up", [NE * 256, 2048])
    w_down_d = dt_("w_down", [NE * 256, 2048])
    yc = dt_("yc", [TOK_PER_CORE, D], F32, "ExternalOutput")
    SK = "ExternalOutput" if debug else "Internal"
    x1s_d = dt_("x1s", [TOK_PER_CORE + 1, D], BF16, SK)
    res_d = dt_("res", [TOK_PER_CORE, D], F32, SK)
    sinfo_d = dt_("sinfo", [NSLOT, 4], I32, SK)
    ybuf_d = dt_("ybuf", [2 * TOK_PER_CORE, D], F32, SK)
    wsc_d = dt_("wsc", [NE * 128, 12288], BF16, "Internal")

    with contextlib.ExitStack() as st:
        tr = Tracker(nc, st)
        cnt = [0]

        def sb(shape, dt=F32, dma=False):
            cnt[0] += 1
            t = st.enter_context(nc.sbuf_tensor("sb%d" % cnt[0], shape, dt))
            ds = st.enter_context(nc.semaphore("ds%d" % cnt[0])) if dma else None
            return Buf(t, ds)

        def view(b, ap):
            return ap

        consts = sb([128, NCONST], F32, dma=True)
        pp = sb([128, NPP], F32, dma=True)
        rowp = sb([128, NRP], F32, dma=True)
        ln2b = sb([128, 2, D], F32, dma=True)
        wr = sb([128, 8, 20], F32, dma=True)
        wpool = sb([128, 4, 128], BF16, dma=True)
        identb = sb([128, 128], BF16, dma=True)
        R0 = sb([128, 8 * 2056], BF16, dma=True)
        R1 = sb([128, 12288], BF16, dma=True)
        dwall_t = sb([128, NCH, 16], F32)
        dwall = [Buf(dwall_t.t) for _ in range(NCH)]
        posall_t = sb([128, NCH, 16], F32)
        posall = [Buf(posall_t.t) for _ in range(NCH)]
        run = sb([128, 16], F32)
        xg = [sb([128, 4, D], BF16, dma=True) for _ in range(2)]
        xgT = [sb([128, 8, 512], BF16, dma=True) for _ in range(2)]
        sit = [sb([128, TQ, 4], I32, dma=True) for _ in range(2)]
        rstage = [sb([128, D], F32, dma=True) for _ in range(2)]
        x1stage = [sb([128, D], BF16, dma=True) for _ in range(2)]
        payall = sb([128, NCH, 2, 4], I32, dma=True)
        slotiA = sb([128, NCH, 2], I32)
        smallsB = sb([128, 192], F32)
        widx = sb([128, NTILE], I32)
        teb = sb([128, NTILE], F32)
        x1s_b = Buf(None, st.enter_context(nc.semaphore("dsx1s")))
        res_b = Buf(None, st.enter_context(nc.semaphore("dsres")))
        sinfo_b = Buf(None, st.enter_context(nc.semaphore("dssinfo")))
        sinfo_pre = Buf(None, st.enter_context(nc.semaphore("dssinfopre")))
        ybuf_b = Buf(None, st.enter_context(nc.semaphore("dsybuf")))
        wsc_b = Buf(None, st.enter_context(nc.semaphore("dswsc")))
        cvs = [Buf(None, st.enter_context(nc.semaphore("dscv%d" % i))) for i in range(2)]
        ysem = [Buf(None, st.enter_context(nc.semaphore("dsys%d" % i))) for i in range(2)]
        l2s = [Buf(None, st.enter_context(nc.semaphore("dsl2%d" % i))) for i in range(4)]
        S = sb([128, 512], F32)
        Sb = sb([128, 512], BF16)
        utok = [sb([128, 512], F32) for _ in range(2)]
        ub = [sb([128, 8, 131], F32) for _ in range(2)]
        io = [sb([128, D], F32, dma=True) for _ in range(2)]
        xh32 = sb([128, D], F32)
        xh32c = sb([128, D], F32)
        xT32s = [sb([128, 8, 128], F32) for _ in range(2)]
        x1T32 = sb([128, 8, 128], F32)
        xnT = sb([128, 8, 128], BF16)
        X1 = sb([128, 1024], F32)
        cacc = [Buf(X1.t) for _ in range(8)]
        X2 = sb([128, 1024], F32)
        xsT32 = sb([128, 4, 128], F32)
        BT32 = sb([128, 2, 128], F32)
        BTb = sb([128, 2, 128], BF16)
        CTb = sb([128, 2, 128], BF16)
        szc = sb([128, 512], F32)
        MT = sb([128, 8, 128], BF16)
        scm = sb([128, 2, 128], F32)
        xs_tok = sb([128, 512], F32)
        xdt = sb([128, 512], BF16)
        xdec = sb([128, 512], BF16)
        Btok = sb([128, 256], BF16)
        ybuf = sb([128, 512], F32)
        ynb = sb([128, 512], BF16)
        ycatTs = [sb([128, 8, 128], BF16) for _ in range(2)]
        pT = sb([128, 4, 128], BF16)
        smalls_t = sb([128, 512], F32)
        sm_off = [0]

        def small(n):
            o = sm_off[0]
            sm_off[0] += n
            assert sm_off[0] <= 512
            b = Buf(smalls_t.t)
            b.ap = smalls_t.t[:, o:o + n]
            return b

        stats = small(12); mv = small(2); lnv = small(1); rs = small(1)
        statsC = small(12); mvC = small(2); lnvC = small(1); rsC = small(1)
        stats4 = [small(12) for _ in range(4)]; mv4 = [small(2) for _ in range(4)]; lnv4 = [small(1) for _ in range(4)]
        rs4 = [small(1) for _ in range(4)]; nmr4 = [small(1) for _ in range(4)]
        ahead = small(8); psb = small(4)
        xdtb = small(8); axb = small(8); e1 = small(8); l1 = small(8); dtv = small(8); av = small(8)
        acs = small(8); eac = small(8); dd = small(8); dsv = small(8); dtds = small(8); cdv = small(8)
        ssq = small(2); lns = small(2); rs2 = small(2)
        lg = small(20); gmax = small(1); ngmax = small(1); gexp = small(4); gsum = small(1); gw = small(1)
        gmask = small(4); t44 = small(16); esel = small(4); m1 = small(1); mask1 = small(4); esel2 = small(4)
        m2 = small(1); mask2 = small(4); d21 = small(1); e21 = small(1); den = small(1); p1 = small(1)
        c1 = small(1); c2 = small(1); wsel = small(4)
        dtvs = [dtv, small(8)]; avs = [av, small(8)]
        ind = small(16); nt = small(16); incl = small(16); base = small(16); ones16 = small(16)
        slotf = small(16); cin = small(16); cex = small(16); sel = [small(16), small(16)]; tmp16 = [small(16), small(16)]
        slk = small(2); wk = small(2); tokf = small(1); dstf = small(2)

        class _V:
            pass

        def alias(ap):
            b = Buf(None)
            v = _V()
            v.ap = ap
            b.t = _T(ap)
            return b

        class _T:
            def __init__(self, ap):
                self._ap = ap

            def __getitem__(self, key):
                return self._ap[key] if key != slice(None) else self._ap

        xg0f = xg[0].t[:].rearrange("p q d -> p (q d)").bitcast(F32)
        xg1f = xg[1].t[:].rearrange("p q d -> p (q d)").bitcast(F32)
        xsT32s = [xsT32, alias(xg0f[:, 0:512].rearrange("p (m t) -> p m t", m=4))]
        BT32s = [BT32, alias(xg0f[:, 512:768].rearrange("p (g t) -> p g t", g=2))]
        szcs = [szc, alias(xg0f[:, 768:1280])]
        utok3 = [utok[0], utok[1], alias(xg0f[:, 1280:1792])]
        BTbs = [BTb, alias(xg0f[:, 1792:1920].bitcast(BF16).rearrange("p (g t) -> p g t", g=2))]
        CTbs = [CTb, alias(xg0f[:, 1920:2048].bitcast(BF16).rearrange("p (g t) -> p g t", g=2))]
        xT32s.append(alias(xg1f[:, 0:1024].rearrange("p (j t) -> p j t", j=8)))
        XG_ALIAS = [[xsT32s[1], BT32s[1], szcs[1], utok3[2], BTbs[1], CTbs[1]], [xT32s[2]]]
        banks = []
        for i in range(8):
            t = st.enter_context(nc.psum_tensor("ps%d" % i, [128, 512], F32))
            banks.append(Buf(t))
        bank_i = [0]

        def bank():
            b = banks[bank_i[0] % 7]
            bank_i[0] += 1
            return b

        pool_a = [0]
        pool_b = [0]
        pool_c = [0]

        def bank_a():
            b = banks[pool_a[0] % 2]
            pool_a[0] += 1
            return b

        def bank_b():
            b = banks[2 + pool_b[0] % 3]
            pool_b[0] += 1
            return b

        def bank_c():
            b = banks[5 + pool_c[0] % 2]
            pool_c[0] += 1
            return b

        C = consts.t
        ident32 = C[:, C_ID:C_ID + 128]
        Umat = C[:, C_U:C_U + 128]
        Lsmat = C[:, C_LS:C_LS + 128]
        mask01 = C[:, C_MASK:C_MASK + 128]
        alphaI = C[:, C_AI:C_AI + 128]
        ones = C[:, C_ONES:C_ONES + 128]
        iota_p = C[:, C_MISC:C_MISC + 1]
        c2ph = C[:, C_MISC + 1:C_MISC + 3]
        sconst = C[:, C_S:C_S + NTILE]
        cconst = C[:, C_S:C_S + NCH]
        Ustrict = C[:, C_US:C_US + 128]
        rev_e = C[:, C_MISC + 35:C_MISC + 51]

        def band(kind, g):
            o = C_BAND + (kind * 4 + g) * 128
            return C[:, o:o + 128]

        P = pp.t
        RW = rowp.t

        tr.dma("sp", consts.t[:], consts_d[:, :], pwrites=[consts])
        tr.dma("sp", pp.t[:], pp_d[:, :], pwrites=[pp])
        tr.dma("sp", rowp.t[:], rowp_d.partition_broadcast(128), pwrites=[rowp])
        for i in range(2):
            tr.dma("sp", ln2b.t[:, i, :], ln2_d[i, :].partition_broadcast(128), pwrites=[ln2b])
        tr.dma("sp", wr.t[:], wr_d.rearrange("(j p) n -> p j n", p=128), pwrites=[wr])
        tr.dma("sp", identb.t[:], identb_d[:, :], pwrites=[identb])
        tr.dma("pool", wpool.t[:], w_pool_d.rearrange("g c d -> c g d"), pwrites=[wpool])
        tr.op("act", lambda e: e.activation(out=ahead.ap, in_=RW[:, RP_ALOG:RP_ALOG + 8], func=AF.Exp),
              reads=[rowp], writes=[ahead])
        tr.op("dve", lambda e: e.tensor_scalar(out=ahead.ap, in0=ahead.ap, scalar1=-1.0, scalar2=None, op0=ALU.mult),
              reads=[], writes=[ahead])
        tr.op("dve", lambda e: e.tensor_tensor(out=psb.ap, in0=P[:, PP_PB:PP_PB + 4], in1=P[:, PP_PS:PP_PS + 4], op=ALU.mult),
              reads=[pp], writes=[psb])

        tr.op("pool", lambda e: e.memset(run.t[:], 0.0), writes=[run])
        tr.op("pool", lambda e: e.memset(ones16.ap, 1.0), writes=[ones16])
        zrow_ap = io[1].t[0:1, 0:512].bitcast(BF16)
        tr.op("pool", lambda e: e.memset(io[1].t[0:1, 0:512], 0.0), writes=[io[1]])
        tr.dma("sp", x1s_d[TOK_PER_CORE:TOK_PER_CORE + 1, :], zrow_ap, reads=[io[1]], nowait=[x1s_b])
        padt_ap = io[0].t[:, 0:NSLOT // 32].bitcast(I32).rearrange("p (a f) -> p a f", f=4)
        tr.op("pool", lambda e: e.memset(padt_ap[:, :, 0:1], 1000000), pwrites=[io[0]])
        tr.op("pool", lambda e: e.memset(padt_ap[:, :, 1:2], 0), pwrites=[io[0]])
        tr.op("pool", lambda e: e.memset(padt_ap[:, :, 2:3], 1000000), pwrites=[io[0]])
        tr.op("pool", lambda e: e.memset(padt_ap[:, :, 3:4], 0), pwrites=[io[0]])
        tr.dma("sp", sinfo_d.rearrange("(p a) f -> p a f", p=128), padt_ap, reads=[io[0]], pwrites=[sinfo_pre])
        tr.op("pool", lambda e: e.memset(payall.t[:], 0), writes=[payall])

        reg_ns = nc.gpsimd.alloc_register("bc_nslot")
        nc.gpsimd.reg_mov(reg_ns, NSLOT - 1)
        reg_nx = nc.gpsimd.alloc_register("bc_nx")
        nc.gpsimd.reg_mov(reg_nx, TOK_PER_CORE)
        reg_nw = nc.gpsimd.alloc_register("bc_nw")
        nc.gpsimd.reg_mov(reg_nw, NE * 128 - 1)
        reg_ny = nc.gpsimd.alloc_register("bc_ny")
        nc.gpsimd.reg_mov(reg_ny, 2 * TOK_PER_CORE - 1)

        def rstd_from(var_ap, var_bufs, scale, eps, lnbuf, outbuf):
            tr.op("act", lambda e: e.activation(out=lnbuf.ap, in_=var_ap, func=AF.Ln, bias=eps_ap(eps), scale=scale),
                  reads=var_bufs + [epsb], writes=[lnbuf])
            tr.op("act", lambda e: e.activation(out=outbuf.ap, in_=lnbuf.ap, func=AF.Exp, scale=-0.5),
                  reads=[lnbuf], writes=[outbuf])

        epsb = small(2)
        tr.op("pool", lambda e: e.memset(epsb.ap[:, 0:1], LN_EPS), writes=[epsb])
        one_b = small(1)
        tr.op("pool", lambda e: e.memset(one_b.ap, 1.0), writes=[one_b])

        def eps_ap(eps):
            return epsb.ap[:, 0:1]

        def load_mixer_weights():
            w3 = R0.t[:, 0:8 * 2056].rearrange("p (j n) -> p j n", j=8)
            src = w_in_d.rearrange("(j p) n -> p j n", p=128)
            tr.dma("pool", w3[:, :, 0:1024], src[:, :, 0:1024], pwrites=[R0])
            tr.dma("pool", w3[:, :, 1024:2056], src[:, :, 1024:2056], pwrites=[R0])
            wo3 = R1.t[:, 0:8192].rearrange("p (j n) -> p j n", j=8)
            tr.dma("pool", wo3, w_out_d.rearrange("(j p) n -> p j n", p=128), pwrites=[R1])

        win = R0.t[:, 0:8 * 2056].rearrange("p (j n) -> p j n", j=8)
        wout = R1.t[:, 0:8192].rearrange("p (j n) -> p j n", j=8)

        def chunk_A(u, c):
            gtok = u * UNIT + c * 128
            gc = u * 8 + c
            cs = (gtok % 2048) // 128
            first = cs == 0
            xT32_ = xT32s[gc % 3]
            xt = io[c % 2]
            ubc, ubp = ub[c % 2], ub[(c + 1) % 2]
            utc = utok3[gc % 3]
            sl = gc % 2
            xsT32_, BT32_, BTb_, CTb_, szc_, dtv_, av_ = xsT32s[sl], BT32s[sl], BTbs[sl], CTbs[sl], szcs[sl], dtvs[sl], avs[sl]
            pS = banks[7]
            tr.dma("sp", xt.t[:], xc[gtok:gtok + 128, :], pwrites=[xt])
            for hlf in range(2):
                tr.op("dve", lambda e, hlf=hlf: e.bn_stats(out=stats.ap[:, hlf * 6:hlf * 6 + 6], in_=xt.t[:, hlf * 512:(hlf + 1) * 512]),
                      reads=[xt], pwrites=[stats])
            tr.op("dve", lambda e: e.bn_aggr(out=mv.ap, in_=stats.ap), reads=[stats], writes=[mv])
            rstd_from(mv.ap[:, 1:2], [mv], 1.0, LN_EPS, lnv, rs)
            tr.op("dve", lambda e: e.tensor_scalar(out=xh32.t[:], in0=xt.t[:], scalar1=mv.ap[:, 0:1], scalar2=rs.ap,
                                                   op0=ALU.subtract, op1=ALU.mult),
                  reads=[xt, mv, rs], writes=[xh32])
            pA, pB = bank_a(), bank_a()
            for hb, pb in enumerate((pA, pB)):
                tr.mm([lambda e, j=j, pb=pb: e.transpose(out=pb.t[:, (j % 4) * 128:(j % 4 + 1) * 128],
                                                         in_=xh32.t[:, j * 128:(j + 1) * 128], identity=ident32)
                       for j in range(hb * 4, hb * 4 + 4)], reads=[xh32, consts], pwrites=[pb])
            for j in range(8):
                pb = pA if j < 4 else pB
                tr.op("act", lambda e, j=j, pb=pb: e.activation(out=xT32_.t[:, j, :], in_=pb.t[:, (j % 4) * 128:(j % 4 + 1) * 128],
                                                                func=AF.Identity, scale=P[:, PP_G0 + j:PP_G0 + j + 1],
                                                                bias=P[:, PP_B0 + j:PP_B0 + j + 1]),
                      reads=[pb, pp], pwrites=[xT32_])
            tr.op("dve", lambda e: e.tensor_copy(out=xnT.t[:], in_=xT32_.t[:]), reads=[xT32_], writes=[xnT])
            pX = [bank_a(), bank_a()]
            for hb in range(2):
                fns = []
                for m in range(hb * 4, hb * 4 + 4):
                    for j in range(8):
                        fns.append(lambda e, m=m, j=j, hb=hb: e.matmul(
                            out=pX[hb].t[:, (m % 4) * 128:(m % 4 + 1) * 128],
                            lhsT=win[:, j, 512 + m * 128:512 + (m + 1) * 128], rhs=xnT.t[:, j, :],
                            start=(j == 0), stop=(j == 7)))
                tr.mm(fns, reads=[xnT, R0], pwrites=[pX[hb]])
            for hb in range(2):
                tr.op("act", lambda e, hb=hb: e.activation(out=ubc.t[:, hb * 4:hb * 4 + 4, 3:131],
                                                           in_=pX[hb].t[:].rearrange("p (m t) -> p m t", m=4), func=AF.Identity),
                      reads=[pX[hb]], pwrites=[ubc])
            pZ = bank_a()
            tr.mm([lambda e, j=j: e.matmul(out=pZ.t[:], lhsT=xnT.t[:, j, :], rhs=win[:, j, 0:512], start=(j == 0), stop=(j == 7))
                   for j in range(8)], reads=[xnT, R0], pwrites=[pZ])
            pU = bank_a()
            tr.mm([lambda e, j=j: e.matmul(out=pU.t[:], lhsT=xnT.t[:, j, :], rhs=win[:, j, 1544:2056], start=(j == 0), stop=(j == 7))
                   for j in range(8)], reads=[xnT, R0], pwrites=[pU])
            tr.mm([lambda e, j=j: e.matmul(out=pS.t[:, 0:8], lhsT=xnT.t[:, j, :], rhs=win[:, j, 1536:1544], start=(j == 0), stop=(j == 7))
                   for j in range(8)], reads=[xnT, R0], pwrites=[pS])
            if first:
                tr.op("pool", lambda e: e.memset(ubc.t[:, :, 0:3], 0.0), pwrites=[ubc])
            else:
                tr.op("pool", lambda e: e.tensor_copy(out=ubc.t[:, :, 0:3], in_=ubp.t[:, :, 128:131]), reads=[ubp], pwrites=[ubc])
            cav = X1.t[:].rearrange("p (m t) -> p m t", m=8)
            for m in range(8):
                tr.op("pool", lambda e, m=m: e.tensor_scalar(out=cav[:, m, :], in0=ubc.t[:, m, 0:128],
                                                             scalar1=P[:, PP_CW + m * 4:PP_CW + m * 4 + 1],
                                                             scalar2=P[:, PP_CB + m:PP_CB + m + 1], op0=ALU.mult, op1=ALU.add),
                      reads=[ubc, pp], writes=[cacc[m]], pwrites=[X1])
            for k in range(1, 4):
                for m in range(8):
                    tr.op("dve", lambda e, m=m, k=k: e.scalar_tensor_tensor(
                        out=cav[:, m, :], in0=ubc.t[:, m, k:k + 128], scalar=P[:, PP_CW + m * 4 + k:PP_CW + m * 4 + k + 1],
                        in1=cav[:, m, :], op0=ALU.mult, op1=ALU.add), reads=[ubc, pp], writes=[cacc[m]])
            tr.op("act", lambda e: e.activation(out=xsT32_.t[:], in_=cav[:, 0:4, :], func=AF.Silu), reads=cacc[0:4], writes=[xsT32_])
            tr.op("act", lambda e: e.activation(out=BT32_.t[:], in_=cav[:, 4:6, :], func=AF.Silu), reads=cacc[4:6], writes=[BT32_])
            tr.op("act", lambda e: e.activation(out=CTb_.t[:], in_=cav[:, 6:8, :], func=AF.Silu), reads=cacc[6:8], writes=[CTb_])
            tr.op("act", lambda e: e.activation(out=szc_.t[:], in_=pZ.t[:], func=AF.Silu), reads=[pZ], writes=[szc_])
            tr.op("pool", lambda e: e.tensor_copy(out=BTb_.t[:], in_=BT32_.t[:]), reads=[BT32_], writes=[BTb_])
            tr.op("act", lambda e: e.activation(out=utc.t[:], in_=pU.t[:], func=AF.Identity), reads=[pU], writes=[utc])
            tr.op("dve", lambda e: e.tensor_tensor(out=xdtb.ap, in0=pS.t[:, 0:8], in1=RW[:, RP_DTB:RP_DTB + 8], op=ALU.add),
                  reads=[pS, rowp], writes=[xdtb])
            tr.op("act", lambda e: e.activation(out=axb.ap, in_=xdtb.ap, func=AF.Abs), reads=[xdtb], writes=[axb])
            tr.op("act", lambda e: e.activation(out=e1.ap, in_=axb.ap, func=AF.Exp, scale=-1.0), reads=[axb], writes=[e1])
            tr.op("act", lambda e: e.activation(out=l1.ap, in_=e1.ap, func=AF.Ln, bias=one_b.ap, scale=1.0), reads=[e1, one_b], writes=[l1])
            tr.op("dve", lambda e: e.scalar_tensor_tensor(out=dtv_.ap, in0=xdtb.ap, scalar=0.0, in1=l1.ap, op0=ALU.max, op1=ALU.add),
                  reads=[xdtb, l1], writes=[dtv_])
            tr.op("dve", lambda e: e.tensor_tensor(out=av_.ap, in0=dtv_.ap, in1=ahead.ap, op=ALU.mult), reads=[dtv_, ahead], writes=[av_])
        def chunk_B(u, c):
            gtok = u * UNIT + c * 128
            gc = u * 8 + c
            cs = (gtok % 2048) // 128
            first = cs == 0
            ycatT_ = ycatTs[gc % 2]
            utc, utp = utok3[gc % 3], utok3[(gc + 2) % 3]
            sl = gc % 2
            xsT32_, BT32_, BTb_, CTb_, szc_, dtv_, av_ = xsT32s[sl], BT32s[sl], BTbs[sl], CTbs[sl], szcs[sl], dtvs[sl], avs[sl]
            pS = banks[7]
            if first:
                tr.op("pool", lambda e: e.memset(Sb.t[:], 0.0), writes=[Sb])
            tr.mm([lambda e: e.matmul(out=pS.t[:, 8:16], lhsT=Umat, rhs=av_.ap, start=True, stop=True),
                   lambda e: e.matmul(out=pS.t[:, 16:24], lhsT=ones, rhs=av_.ap, start=True, stop=True)],
                  reads=[av_, consts], pwrites=[pS])
            Rt = X2.t[:].rearrange("p (h l) -> p h l", h=8)
            for h in range(8):
                tr.op("dve", lambda e, h=h: e.tensor_scalar(out=Rt[:, h, :], in0=Umat, scalar1=av_.ap[:, h:h + 1], scalar2=None, op0=ALU.mult),
                      reads=[av_, consts], pwrites=[X2])
            pD = [bank_b(), bank_b()]
            for hb in range(2):
                tr.mm([lambda e, hb=hb: e.matmul(out=pD[hb].t[:], lhsT=Lsmat, rhs=X2.t[:, hb * 512:(hb + 1) * 512], start=True, stop=True)],
                      reads=[X2, consts], pwrites=[pD[hb]])
            for hb in range(2):
                tr.op("act", lambda e, hb=hb: e.activation(out=X2.t[:, hb * 512:(hb + 1) * 512], in_=pD[hb].t[:], func=AF.Exp),
                      reads=[pD[hb]], pwrites=[X2])
            scv = pS.t[:, 256:512].rearrange("p (g l) -> p g l", g=2)
            tr.mm([lambda e, g=g: e.matmul(out=scv[:, g, :], lhsT=BTb_.t[:, g, :], rhs=CTb_.t[:, g, :], start=True, stop=True)
                   for g in range(2)], reads=[BTb_, CTb_], pwrites=[pS])
            tr.op("dve", lambda e: e.tensor_tensor(out=scm.t[:], in0=scv, in1=mask01.unsqueeze(1).to_broadcast([128, 2, 128]), op=ALU.mult),
                  reads=[consts, pS], writes=[scm])
            for g in range(2):
                tr.op("dve", lambda e, g=g: e.tensor_tensor(out=MT.t[:, g * 4:(g + 1) * 4, :], in0=Rt[:, g * 4:(g + 1) * 4, :],
                                                            in1=scm.t[:, g, :].unsqueeze(1).to_broadcast([128, 4, 128]), op=ALU.mult),
                      reads=[X2, scm], pwrites=[MT])
            pXs = bank_b()
            tr.mm([lambda e, m=m: e.transpose(out=pXs.t[:, m * 128:(m + 1) * 128], in_=xsT32_.t[:, m, :], identity=ident32)
                   for m in range(4)], reads=[xsT32_, consts], pwrites=[pXs])
            pBt = bank_b()
            tr.mm([lambda e, g=g: e.transpose(out=pBt.t[:, g * 128:(g + 1) * 128], in_=BT32_.t[:, g, :], identity=ident32)
                   for g in range(2)], reads=[BT32_, consts], pwrites=[pBt])
            tr.op("act", lambda e: e.activation(out=xs_tok.t[:], in_=pXs.t[:], func=AF.Identity), reads=[pXs], writes=[xs_tok])
            tr.op("act", lambda e: e.activation(out=Btok.t[:], in_=pBt.t[:, 0:256], func=AF.Identity), reads=[pBt], writes=[Btok])
            tr.op("act", lambda e: e.activation(out=eac.ap, in_=pS.t[:, 8:16], func=AF.Exp), reads=[pS], writes=[eac])
            tr.op("act", lambda e: e.activation(out=acs.ap, in_=pS.t[:, 8:16], func=AF.Identity), reads=[pS], writes=[acs])
            tr.op("act", lambda e: e.activation(out=cdv.ap, in_=pS.t[:, 16:24], func=AF.Exp), reads=[pS], writes=[cdv])
            tr.op("dve", lambda e: e.tensor_tensor(out=dd.ap, in0=pS.t[:, 16:24], in1=acs.ap, op=ALU.subtract), reads=[pS, acs], writes=[dd])
            tr.op("act", lambda e: e.activation(out=dsv.ap, in_=dd.ap, func=AF.Exp), reads=[dd], writes=[dsv])
            tr.op("dve", lambda e: e.tensor_tensor(out=dtds.ap, in0=dtv_.ap, in1=dsv.ap, op=ALU.mult), reads=[dtv_, dsv], writes=[dtds])
            xs3 = pXs.t[:].rearrange("p (h q) -> p h q", h=8)
            tr.op("dve", lambda e: e.tensor_tensor(out=xdt.t[:].rearrange("p (h q) -> p h q", h=8), in0=xs3,
                                                   in1=dtv_.ap.unsqueeze(2).to_broadcast([128, 8, 64]), op=ALU.mult),
                  reads=[pXs, dtv_], writes=[xdt])
            tr.op("dve", lambda e: e.tensor_tensor(out=xdec.t[:].rearrange("p (h q) -> p h q", h=8), in0=xs3,
                                                   in1=dtds.ap.unsqueeze(2).to_broadcast([128, 8, 64]), op=ALU.mult),
                  reads=[pXs, dtds], writes=[xdec])
            pYd = bank_b()
            tr.mm([lambda e, h=h: e.matmul(out=pYd.t[:, h * 64:(h + 1) * 64], lhsT=MT.t[:, h, :], rhs=xdt.t[:, h * 64:(h + 1) * 64],
                                           start=True, stop=True) for h in range(8)], reads=[MT, xdt], pwrites=[pYd])
            pYo = bank_b()
            tr.mm([lambda e, g=g: e.matmul(out=pYo.t[:, g * 256:(g + 1) * 256], lhsT=CTb_.t[:, g, :], rhs=Sb.t[:, g * 256:(g + 1) * 256],
                                           start=True, stop=True) for g in range(2)], reads=[CTb_, Sb], pwrites=[pYo])
            pSt = bank_b()
            tr.mm([lambda e, g=g: e.matmul(out=pSt.t[:, g * 256:(g + 1) * 256], lhsT=Btok.t[:, g * 128:(g + 1) * 128],
                                           rhs=xdec.t[:, g * 256:(g + 1) * 256], start=True, stop=True) for g in range(2)],
                  reads=[Btok, xdec], pwrites=[pSt])
            y3 = ybuf.t[:].rearrange("p (h q) -> p h q", h=8)
            tr.op("dve", lambda e: e.tensor_tensor(out=y3, in0=pYo.t[:].rearrange("p (h q) -> p h q", h=8),
                                                   in1=eac.ap.unsqueeze(2).to_broadcast([128, 8, 64]), op=ALU.mult),
                  reads=[pYo, eac], writes=[ybuf])
            tr.op("dve", lambda e: e.tensor_tensor(out=ybuf.t[:], in0=pYd.t[:], in1=ybuf.t[:], op=ALU.add), reads=[pYd], writes=[ybuf])
            tr.op("pool", lambda e: e.tensor_tensor(out=xs_tok.t[:].rearrange("p (h q) -> p h q", h=8),
                                                    in0=xs_tok.t[:].rearrange("p (h q) -> p h q", h=8),
                                                    in1=RW[:, RP_DSKIP:RP_DSKIP + 8].unsqueeze(2).to_broadcast([128, 8, 64]), op=ALU.mult),
                  reads=[rowp], writes=[xs_tok])
            tr.op("pool", lambda e: e.tensor_tensor(out=ybuf.t[:], in0=ybuf.t[:], in1=xs_tok.t[:], op=ALU.add), reads=[xs_tok], writes=[ybuf])
            tr.op("pool", lambda e: e.tensor_tensor(out=ybuf.t[:], in0=ybuf.t[:], in1=szc_.t[:], op=ALU.mult), reads=[szc_], writes=[ybuf])
            for g in range(2):
                tr.op("act", lambda e, g=g: e.activation(out=ynb.t[:, g * 256:(g + 1) * 256], in_=ybuf.t[:, g * 256:(g + 1) * 256],
                                                         func=AF.Square, accum_out=ssq.ap[:, g:g + 1]),
                      reads=[ybuf], pwrites=[ynb, ssq])
            tr.op("act", lambda e: e.activation(out=lns.ap, in_=ssq.ap, func=AF.Ln, bias=epsb.ap[:, 0:1], scale=1.0 / 256.0),
                  reads=[ssq, epsb], writes=[lns])
            tr.op("act", lambda e: e.activation(out=rs2.ap, in_=lns.ap, func=AF.Exp, scale=-0.5), reads=[lns], writes=[rs2])
            for g in range(2):
                tr.op("dve", lambda e, g=g: e.scalar_tensor_tensor(out=ynb.t[:, g * 256:(g + 1) * 256], in0=ybuf.t[:, g * 256:(g + 1) * 256],
                                                                   scalar=rs2.ap[:, g:g + 1],
                                                                   in1=RW[:, RP_NG + g * 256:RP_NG + (g + 1) * 256],
                                                                   op0=ALU.mult, op1=ALU.mult),
                      reads=[ybuf, rs2, rowp], pwrites=[ynb])
            pYt = bank_b()
            pYt_b = pYt.t[:].bitcast(BF16)
            tr.mm([lambda e, m=m: e.transpose(out=pYt_b[:, m * 128:(m + 1) * 128], in_=ynb.t[:, m * 128:(m + 1) * 128], identity=identb.t[:])
                   for m in range(4)], reads=[ynb, identb], pwrites=[pYt])
            tr.op("act", lambda e: e.activation(out=ycatT_.t[:, 0:4, :], in_=pYt_b[:, 0:512].rearrange("p (m t) -> p m t", m=4), func=AF.Identity),
                  reads=[pYt], pwrites=[ycatT_])
            if first:
                tr.op("pool", lambda e: e.memset(S.t[:], 0.0), writes=[S])
            else:
                tr.op("pool", lambda e: e.tensor_tensor(out=S.t[:].rearrange("p (h q) -> p h q", h=8),
                                                        in0=S.t[:].rearrange("p (h q) -> p h q", h=8),
                                                        in1=cdv.ap.unsqueeze(2).to_broadcast([128, 8, 64]), op=ALU.mult),
                      reads=[cdv], writes=[S])
            tr.op("dve", lambda e: e.tensor_tensor(out=S.t[:], in0=pSt.t[:], in1=S.t[:], op=ALU.add), reads=[pSt], writes=[S])
            tr.op("act", lambda e: e.activation(out=Sb.t[:], in_=S.t[:], func=AF.Identity), reads=[S], writes=[Sb])
            pP = bank_b()
            fns = []
            for g in range(4):
                fns.append(lambda e, g=g: e.matmul(out=pP.t[:, g * 128:(g + 1) * 128], lhsT=utc.t[:, g * 128:(g + 1) * 128],
                                                   rhs=band(2 if first else 0, g), start=True, stop=first))
                if not first:
                    fns.append(lambda e, g=g: e.matmul(out=pP.t[:, g * 128:(g + 1) * 128], lhsT=utp.t[:, g * 128:(g + 1) * 128],
                                                       rhs=band(1, g), start=False, stop=True))
            tr.mm(fns, reads=[utc, consts] + ([] if first else [utp]), pwrites=[pP])
            tr.op("act", lambda e: e.activation(out=pT.t[:], in_=pP.t[:].rearrange("p (g t) -> p g t", g=4), func=AF.Identity),
                  reads=[pP], writes=[pT])
            pM = bank_b()
            tr.mm([lambda e, g=g: e.matmul(out=pM.t[:, g * 128:(g + 1) * 128], lhsT=wpool.t[:, g, :], rhs=pT.t[:, g, :], start=True, stop=True)
                   for g in range(4)], reads=[wpool, pT], pwrites=[pM])
            for g in range(4):
                tr.op("act", lambda e, g=g: e.activation(out=ycatT_.t[:, 4 + g, :], in_=pM.t[:, g * 128:(g + 1) * 128], func=AF.Identity,
                                                         scale=P[:, PP_PS + g:PP_PS + g + 1], bias=psb.ap[:, g:g + 1]),
                      reads=[pM, pp, psb], pwrites=[ycatT_])
        def chunk_C(u, c):
            gtok = u * UNIT + c * 128
            gc = u * 8 + c
            xT32_ = xT32s[gc % 3]
            ycatT_ = ycatTs[gc % 2]
            pS = banks[7]
            pO = [bank_c(), bank_c()]
            for hb in range(2):
                fns = [lambda e, j=j, hb=hb: e.matmul(out=pO[hb].t[:], lhsT=ycatT_.t[:, j, :], rhs=wout[:, j, hb * 512:(hb + 1) * 512],
                                                      start=(j == 0), stop=(j == 7)) for j in range(8)]
                for jj in range(4):
                    fns.append(lambda e, jj=jj, hb=hb: e.matmul(out=pO[hb].t[:, jj * 128:(jj + 1) * 128], lhsT=xT32_.t[:, hb * 4 + jj, :],
                                                                rhs=alphaI, start=False, stop=True, skip_group_check=True))
                tr.mm(fns, reads=[ycatT_, R1, xT32_, consts], pwrites=[pO[hb]])
            for hb in range(2):
                tr.op("dve", lambda e, hb=hb: e.bn_stats(out=statsC.ap[:, hb * 6:hb * 6 + 6], in_=pO[hb].t[:]), reads=[pO[hb]], pwrites=[statsC])
            tr.op("dve", lambda e: e.bn_aggr(out=mvC.ap, in_=statsC.ap), reads=[statsC], writes=[mvC])
            rstd_from(mvC.ap[:, 1:2], [mvC], 1.0, LN_EPS, lnvC, rsC)
            for hb in range(2):
                tr.op("dve", lambda e, hb=hb: e.tensor_scalar(out=xh32c.t[:, hb * 512:(hb + 1) * 512], in0=pO[hb].t[:], scalar1=mvC.ap[:, 0:1],
                                                              scalar2=rsC.ap, op0=ALU.subtract, op1=ALU.mult),
                      reads=[pO[hb], mvC, rsC], pwrites=[xh32c])
            pA, pB = bank_c(), bank_c()
            for hb, pb in enumerate((pA, pB)):
                tr.mm([lambda e, j=j, pb=pb: e.transpose(out=pb.t[:, (j % 4) * 128:(j % 4 + 1) * 128],
                                                         in_=xh32c.t[:, j * 128:(j + 1) * 128], identity=ident32)
                       for j in range(hb * 4, hb * 4 + 4)], reads=[xh32c, consts], pwrites=[pb])
            for j in range(8):
                pb = pA if j < 4 else pB
                tr.op("act", lambda e, j=j, pb=pb: e.activation(out=x1T32.t[:, j, :], in_=pb.t[:, (j % 4) * 128:(j % 4 + 1) * 128],
                                                                func=AF.Identity, scale=P[:, PP_G1 + j:PP_G1 + j + 1],
                                                                bias=P[:, PP_B1 + j:PP_B1 + j + 1]),
                      reads=[pb, pp], pwrites=[x1T32])
            pC = [bank_c(), bank_c()]
            rs_, xs_ = rstage[c % 2], x1stage[c % 2]
            for hb in range(2):
                tr.mm([lambda e, jj=jj, hb=hb: e.matmul(out=pC[hb].t[:, jj * 128:(jj + 1) * 128], lhsT=x1T32.t[:, hb * 4 + jj, :], rhs=alphaI,
                                                        start=True, stop=True) for jj in range(4)], reads=[x1T32, consts], pwrites=[pC[hb]])
                tr.op("act", lambda e, hb=hb: e.activation(out=rs_.t[:, hb * 512:(hb + 1) * 512], in_=pC[hb].t[:], func=AF.Identity),
                      reads=[pC[hb]], pwrites=[rs_])
                tr.op("act", lambda e, hb=hb: e.activation(out=xs_.t[:, hb * 512:(hb + 1) * 512], in_=pC[hb].t[:], func=AF.Identity,
                                                           scale=1.0 / ALPHA),
                      reads=[pC[hb]], pwrites=[xs_])
            tr.dma("act", res_d[gtok:gtok + 128, :], rs_.t[:], reads=[rs_], nowait=[res_b])
            tr.dma("act", x1s_d[gtok:gtok + 128, :], xs_.t[:], reads=[xs_], nowait=[x1s_b])
            tr.mm([lambda e, j=j: e.matmul(out=pS.t[:, 32:52], lhsT=x1T32.t[:, j, :], rhs=wr.t[:, j, :], start=(j == 0), stop=(j == 7))
                   for j in range(8)], reads=[x1T32, wr], pwrites=[pS])
            V = lambda en, fn, r, w: tr.op(en, fn, reads=r, writes=w)
            V("dve", lambda e: e.tensor_tensor(out=lg.ap, in0=pS.t[:, 32:52], in1=RW[:, RP_BR:RP_BR + 20], op=ALU.add), [pS, rowp], [lg])
            V("dve", lambda e: e.tensor_reduce(out=gmax.ap, in_=lg.ap[:, 0:4], axis=AX.X, op=ALU.max), [lg], [gmax])
            V("dve", lambda e: e.tensor_scalar(out=ngmax.ap, in0=gmax.ap, scalar1=-1.0, scalar2=None, op0=ALU.mult), [gmax], [ngmax])
            V("act", lambda e: e.activation(out=gexp.ap, in_=lg.ap[:, 0:4], func=AF.Exp, bias=ngmax.ap, scale=1.0, accum_out=gsum.ap),
              [lg, ngmax], [gexp, gsum])
            V("dve", lambda e: e.reciprocal(out=gw.ap, in_=gsum.ap), [gsum], [gw])
            V("dve", lambda e: e.tensor_scalar(out=gmask.ap, in0=lg.ap[:, 0:4], scalar1=gmax.ap, scalar2=None, op0=ALU.is_equal), [lg, gmax], [gmask])
            el = lg.ap[:, 4:20].rearrange("p (g i) -> p g i", g=4)
            t44v = t44.ap.rearrange("p (g i) -> p g i", g=4)
            V("dve", lambda e: e.tensor_tensor(out=t44v, in0=el, in1=gmask.ap.unsqueeze(2).to_broadcast([128, 4, 4]), op=ALU.mult),
              [lg, gmask], [t44])
            V("dve", lambda e: e.tensor_reduce(out=esel.ap, in_=t44.ap.rearrange("p (g i) -> p i g", g=4), axis=AX.X, op=ALU.add), [t44], [esel])
            V("dve", lambda e: e.tensor_reduce(out=m1.ap, in_=esel.ap, axis=AX.X, op=ALU.max), [esel], [m1])
            V("dve", lambda e: e.tensor_scalar(out=mask1.ap, in0=esel.ap, scalar1=m1.ap, scalar2=None, op0=ALU.is_equal), [esel, m1], [mask1])
            V("dve", lambda e: e.scalar_tensor_tensor(out=esel2.ap, in0=mask1.ap, scalar=-1e30, in1=esel.ap, op0=ALU.mult, op1=ALU.add),
              [mask1, esel], [esel2])
            V("dve", lambda e: e.tensor_reduce(out=m2.ap, in_=esel2.ap, axis=AX.X, op=ALU.max), [esel2], [m2])
            V("dve", lambda e: e.tensor_scalar(out=mask2.ap, in0=esel2.ap, scalar1=m2.ap, scalar2=None, op0=ALU.is_equal), [esel2, m2], [mask2])
            V("dve", lambda e: e.tensor_tensor(out=d21.ap, in0=m2.ap, in1=m1.ap, op=ALU.subtract), [m1, m2], [d21])
            V("act", lambda e: e.activation(out=e21.ap, in_=d21.ap, func=AF.Exp), [d21], [e21])
            V("dve", lambda e: e.tensor_scalar(out=den.ap, in0=e21.ap, scalar1=1.0, scalar2=None, op0=ALU.add), [e21], [den])
            V("dve", lambda e: e.reciprocal(out=p1.ap, in_=den.ap), [den], [p1])
            V("dve", lambda e: e.tensor_tensor(out=c1.ap, in0=p1.ap, in1=gw.ap, op=ALU.mult), [p1, gw], [c1])
            V("dve", lambda e: e.tensor_tensor(out=c2.ap, in0=c1.ap, in1=e21.ap, op=ALU.mult), [c1, e21], [c2])
            V("dve", lambda e: e.tensor_scalar(out=wsel.ap, in0=mask1.ap, scalar1=c1.ap, scalar2=None, op0=ALU.mult), [mask1, c1], [wsel])
            V("dve", lambda e: e.scalar_tensor_tensor(out=wsel.ap, in0=mask2.ap, scalar=c2.ap, in1=wsel.ap, op0=ALU.mult, op1=ALU.add),
              [mask2, c2], [wsel])
            V("dve", lambda e: e.tensor_tensor(out=dwall_t.t[:, gc, :].rearrange("p (g i) -> p g i", g=4),
                                               in0=gmask.ap.unsqueeze(2).to_broadcast([128, 4, 4]),
                                               in1=wsel.ap.unsqueeze(1).to_broadcast([128, 4, 4]), op=ALU.mult), [gmask, wsel], [dwall[gc]])
            V("dve", lambda e: e.tensor_scalar(out=ind.ap, in0=dwall_t.t[:, gc, :], scalar1=0.0, scalar2=None, op0=ALU.is_gt), [dwall[gc]], [ind])
            tr.mm([lambda e: e.matmul(out=pS.t[:, 64:80], lhsT=Ustrict, rhs=ind.ap, start=True, stop=True),
                   lambda e: e.matmul(out=pS.t[:, 80:96], lhsT=ones, rhs=ind.ap, start=True, stop=True)],
                  reads=[ind, consts], pwrites=[pS])
            V("dve", lambda e: e.tensor_tensor(out=posall_t.t[:, gc, :], in0=pS.t[:, 64:80], in1=run.t[:], op=ALU.add), [pS, run], [posall[gc]])
            V("dve", lambda e: e.tensor_tensor(out=run.t[:], in0=pS.t[:, 80:96], in1=run.t[:], op=ALU.add), [pS], [run])

        w_src = (w_gate_d, w_up_d, w_down_d)
        pend_store = []

        def conv_store():
            while pend_store:
                i = pend_store.pop(0)
                mi, ex = i % 3, i // 3
                stg = xgT[i % 2]
                sv = stg.t[:].rearrange("p j n -> p (j n)").rearrange("p (h n) -> p h n", h=2)
                tr.dma("sp", wsc_d[ex * 128:(ex + 1) * 128, mi * 4096:(mi + 1) * 4096], stg.t[:].rearrange("p j n -> p (j n)"),
                       reads=[stg], nowait=[wsc_b], semb=cvs[i % 2])

        def conv_load(i):
            mi, ex = i % 3, i // 3
            stg = xgT[i % 2]
            sv = stg.t[:].rearrange("p j n -> p (j n)").rearrange("p (h n) -> p h n", h=2)
            tr.dma("pool", sv, w_src[mi][ex * 256:(ex + 1) * 256, :].rearrange("(p h) n -> p h n", h=2), pwrites=[stg])
            pend_store.append(i)

        def routing_tables():
            V = lambda en, fn, r, w: tr.op(en, fn, reads=r, writes=w)
            V("dve", lambda e: e.tensor_scalar(out=nt.ap, in0=run.t[:], scalar1=0.0, scalar2=None, op0=ALU.is_gt), [run], [nt])
            for k in range(1, (2 * TOK_PER_CORE // 2) // TS):
                V("dve", lambda e, k=k: e.scalar_tensor_tensor(out=nt.ap, in0=run.t[:], scalar=float(TS * k), in1=nt.ap, op0=ALU.is_gt, op1=ALU.add),
                  [run], [nt])
            V("dve", lambda e: e.tensor_tensor_scan(out=incl.ap, data0=ones16.ap, data1=nt.ap, initial=0.0, op0=ALU.mult, op1=ALU.add),
              [ones16, nt], [incl])
            V("dve", lambda e: e.tensor_tensor(out=base.ap, in0=incl.ap, in1=nt.ap, op=ALU.subtract), [incl, nt], [base])
            V("dve", lambda e: e.tensor_scalar(out=base.ap, in0=base.ap, scalar1=float(TS), scalar2=None, op0=ALU.mult), [], [base])
            cmp_ap = X2.t[:, 0:NTILE * 16].rearrange("p (s e) -> p s e", s=NTILE)
            V("dve", lambda e: e.tensor_tensor(out=cmp_ap, in0=incl.ap.unsqueeze(1).to_broadcast([128, NTILE, 16]),
                                               in1=sconst.unsqueeze(2).to_broadcast([128, NTILE, 16]), op=ALU.is_le), [incl, consts], [X2])
            V("dve", lambda e: e.tensor_reduce(out=teb.t[:], in_=cmp_ap, axis=AX.X, op=ALU.add), [X2], [teb])
            V("dve", lambda e: e.tensor_scalar(out=teb.t[:], in0=teb.t[:], scalar1=15.0, scalar2=None, op0=ALU.min), [], [teb])
            wf = X2.t[:, NTILE * 16:NTILE * 17]
            V("dve", lambda e: e.tensor_scalar(out=wf, in0=teb.t[:], scalar1=128.0, scalar2=iota_p, op0=ALU.mult, op1=ALU.add),
              [teb, consts], [X2])
            V("dve", lambda e: e.tensor_copy(out=widx.t[:], in_=wf), [X2], [widx])
            A3 = lambda ap, off: ap[:, off:off + NCH * 16].rearrange("p (c e) -> p c e", c=NCH)
            indA = A3(xh32.t[:], 0)
            tA = A3(xh32.t[:], 512)
            slotA = A3(X1.t[:], 0)
            selA = [A3(X1.t[:], 512), A3(xT32s[0].t[:].rearrange("p j t -> p (j t)"), 0)]
            V("dve", lambda e: e.tensor_scalar(out=indA, in0=dwall_t.t[:], scalar1=0.0, scalar2=None, op0=ALU.is_gt), dwall, [xh32])
            V("dve", lambda e: e.tensor_tensor(out=slotA, in0=posall_t.t[:], in1=base.ap.unsqueeze(1).to_broadcast([128, NCH, 16]), op=ALU.add),
              posall + [base], [X1])
            V("dve", lambda e: e.tensor_tensor(out=tA, in0=indA, in1=rev_e.unsqueeze(1).to_broadcast([128, NCH, 16]), op=ALU.mult), [consts], [xh32])
            mxA = smallsB.t[:, 0:NCH]
            V("dve", lambda e: e.tensor_reduce(out=mxA, in_=tA, axis=AX.X, op=ALU.max), [xh32], [smallsB])
            tr.op("dve", lambda e: e.tensor_tensor(out=selA[0], in0=tA, in1=mxA.unsqueeze(2).to_broadcast([128, NCH, 16]), op=ALU.is_equal),
                  reads=[xh32, smallsB], pwrites=[X1])
            tr.op("dve", lambda e: e.tensor_tensor(out=selA[1], in0=indA, in1=selA[0], op=ALU.subtract), reads=[xh32, X1], writes=[xT32s[0]])
            tokA = smallsB.t[:, 32:64]
            tr.op("dve", lambda e: e.scalar_tensor_tensor(out=tokA, in0=cconst, scalar=128.0, in1=iota_p.to_broadcast([128, NCH]), op0=ALU.mult, op1=ALU.add),
                  reads=[consts], pwrites=[smallsB])
            payA = payall.t[:]
            payAf = payA.bitcast(F32)
            for k in range(2):
                skA = smallsB.t[:, 64 + k * 32:96 + k * 32]
                tr.op("dve", lambda e, k=k: e.tensor_tensor(out=tA, in0=selA[k], in1=slotA, op=ALU.mult), reads=[X1, xT32s[0]], writes=[xh32])
                tr.op("dve", lambda e, k=k, skA=skA: e.tensor_reduce(out=skA, in_=tA, axis=AX.X, op=ALU.add), reads=[xh32], pwrites=[smallsB])
                tr.op("dve", lambda e, k=k, skA=skA: e.tensor_copy(out=slotiA.t[:, :, k], in_=skA), reads=[smallsB], pwrites=[slotiA])
                tr.op("dve", lambda e, k=k: e.tensor_tensor(out=tA, in0=selA[k], in1=dwall_t.t[:], op=ALU.mult), reads=[X1, xT32s[0]] + dwall, writes=[xh32])
                tr.op("dve", lambda e, k=k: e.tensor_reduce(out=payAf[:, :, k, 1], in_=tA, axis=AX.X, op=ALU.add), reads=[xh32], pwrites=[payall])
                tr.op("dve", lambda e, k=k: e.tensor_copy(out=payA[:, :, k, 0], in_=tokA), reads=[smallsB], pwrites=[payall])
                dkA = smallsB.t[:, 128 + k * 32:160 + k * 32]
                tr.op("dve", lambda e, k=k, dkA=dkA: e.tensor_scalar(out=dkA, in0=tokA, scalar1=float(k * TOK_PER_CORE), scalar2=None, op0=ALU.add),
                      reads=[], pwrites=[smallsB])
                tr.op("dve", lambda e, k=k, dkA=dkA: e.tensor_copy(out=payA[:, :, k, 2], in_=dkA), reads=[smallsB], pwrites=[payall])
            for gc in range(NCH):
                for k in range(2):
                    tr.dma("pool", None, None, reads=[payall, slotiA, sinfo_pre], nowait=[sinfo_b],
                           fn=lambda e, k=k, gc=gc: e.indirect_dma_start(out=sinfo_d[:, :],
                                                                         out_offset=bass.IndirectOffsetOnAxis(ap=slotiA.t[:, gc, k:k + 1], axis=0),
                                                                         in_=payall.t[:, gc, k, :], in_offset=None, bounds_check=reg_ns, oob_is_err=False))

        slots = [R0, R1]
        hTs = [X1, X2]
        sgs = [szc, xs_tok]

        def tile_fetch(s_):
            slot = slots[s_ % 2]
            si_, xg_ = sit[s_ % 2], xg[s_ % 2]
            tr.dma("sp", si_.t[:], sinfo_d[s_ * TS:(s_ + 1) * TS, :].rearrange("(q p) f -> p q f", p=128), reads=[sinfo_b], pwrites=[si_])
            for q in range(TQ):
                tr.dma("pool", None, None, reads=[si_, x1s_b], pwrites=[xg_] + XG_ALIAS[s_ % 2],
                       fn=lambda e, q=q: e.indirect_dma_start(out=xg_.t[:, q, :], out_offset=None, in_=x1s_d[:, :],
                                                              in_offset=bass.IndirectOffsetOnAxis(ap=si_.t[:, q, 0:1], axis=0),
                                                              bounds_check=reg_nx, oob_is_err=False))
            tr.dma("pool", None, None, reads=[widx, wsc_b], pwrites=[slot],
                   fn=lambda e: e.indirect_dma_start(out=slot.t[:, 0:12288], out_offset=None, in_=wsc_d[:, :],
                                                     in_offset=bass.IndirectOffsetOnAxis(ap=widx.t[:, s_:s_ + 1], axis=0),
                                                     bounds_check=reg_nw, oob_is_err=False))

        def tile_compute(s_):
            slot = slots[s_ % 2]
            si_, xg_, xgT_ = sit[s_ % 2], xg[s_ % 2], xgT[s_ % 2]
            sif = si_.t[:].bitcast(F32)
            wg = slot.t[:, 0:4096].rearrange("p (j n) -> p j n", j=8)
            wu = slot.t[:, 4096:8192].rearrange("p (j n) -> p j n", j=8)
            wd = slot.t[:, 8192:12288].rearrange("p (f n) -> p f n", f=4)
            for q in range(TQ):
                pT_ = bank()
                pTb = pT_.t[:].bitcast(BF16)
                tr.mm([lambda e, j=j, q=q: e.transpose(out=pTb[:, j * 128:(j + 1) * 128], in_=xg_.t[:, q, j * 128:(j + 1) * 128], identity=identb.t[:])
                       for j in range(8)], reads=[xg_, identb], pwrites=[pT_])
                tr.op("act", lambda e, q=q: e.activation(out=xgT_.t[:, :, q * 128:(q + 1) * 128], in_=pTb.rearrange("p (j t) -> p j t", j=8),
                                                         func=AF.Identity), reads=[pT_], pwrites=[xgT_])
            hT = hTs[s_ % 2]
            hv = hT.t[:].bitcast(BF16).rearrange("p (f n) -> p f n", f=4)
            for f in range(4):
                sg = sgs[f % 2]
                if TS <= 256:
                    pG = bank()
                    gv, uv = pG.t[:, 0:TS], pG.t[:, TS:2 * TS]
                    pU2 = pG
                else:
                    pG, pU2 = bank(), bank()
                    gv, uv = pG.t[:], pU2.t[:]
                tr.mm([lambda e, j=j, f=f: e.matmul(out=gv, lhsT=wg[:, j, f * 128:(f + 1) * 128], rhs=xgT_.t[:, j, 0:TS],
                                                    start=(j == 0), stop=(j == 7)) for j in range(8)], reads=[slot, xgT_], pwrites=[pG])
                tr.mm([lambda e, j=j, f=f: e.matmul(out=uv, lhsT=wu[:, j, f * 128:(f + 1) * 128], rhs=xgT_.t[:, j, 0:TS],
                                                    start=(j == 0), stop=(j == 7)) for j in range(8)], reads=[slot, xgT_], pwrites=[pU2])
                tr.op("act", lambda e: e.activation(out=sg.t[:, 0:TS], in_=gv, func=AF.Silu), reads=[pG], writes=[sg])
                tr.op("dve", lambda e, f=f: e.tensor_tensor(out=hv[:, f, 0:TS], in0=uv, in1=sg.t[:, 0:TS], op=ALU.mult),
                      reads=[pU2, sg], pwrites=[hT] + (cacc if hT is X1 else []))
            for q in range(TQ):
                yo = io[q % 2]
                for hb in range(2):
                    pO = bank()
                    tr.mm([lambda e, f=f, q=q, hb=hb: e.matmul(out=pO.t[:], lhsT=hv[:, f, q * 128:(q + 1) * 128], rhs=wd[:, f, hb * 512:(hb + 1) * 512],
                                                               start=(f == 0), stop=(f == 3)) for f in range(4)], reads=[hT, slot], pwrites=[pO])
                    tr.op("dve", lambda e, q=q, hb=hb: e.tensor_scalar(out=yo.t[:, hb * 512:(hb + 1) * 512], in0=pO.t[:], scalar1=sif[:, q, 1:2],
                                                                       scalar2=None, op0=ALU.mult), reads=[pO, si_], pwrites=[yo])
                tr.dma("pool", None, None, reads=[yo, si_], nowait=[ybuf_b], semb=ysem[q % 2],
                       fn=lambda e, q=q: e.indirect_dma_start(out=ybuf_d[:, :], out_offset=bass.IndirectOffsetOnAxis(ap=si_.t[:, q, 2:3], axis=0),
                                                              in_=yo.t[:], in_offset=None, bounds_check=reg_ny, oob_is_err=False))

        io4 = [io[0], io[1], rstage[0], rstage[1]]
        yb4 = [xg[0], xg[1], xgT[0], xgT[1]]

        def yview(b):
            ap = b.t[:]
            ap = ap.rearrange("p q d -> p (q d)")
            return ap.bitcast(F32).rearrange("p (k d) -> p k d", k=2)

        def ln2_chunk(gc):
            gtok = gc * 128
            ot, yb = io4[gc % 4], yb4[gc % 4]
            yv = yview(yb)
            st_, mv_, lnv_, rs_, nmr_ = stats4[gc % 4], mv4[gc % 4], lnv4[gc % 4], rs4[gc % 4], nmr4[gc % 4]
            tr.dma("sp", ot.t[:], res_d[gtok:gtok + 128, :], reads=[res_b], pwrites=[ot])
            tr.dma("sp", yv, ybuf_d.rearrange("(k t) d -> t k d", k=2)[gtok:gtok + 128, :, :], reads=[ybuf_b], pwrites=[yb],
                   semb=l2s[gc % 4])
            tr.op("pool", lambda e: e.tensor_tensor(out=ot.t[:], in0=ot.t[:], in1=yv[:, 0, :], op=ALU.add), reads=[yb], writes=[ot])
            tr.op("dve", lambda e: e.tensor_tensor(out=ot.t[:], in0=ot.t[:], in1=yv[:, 1, :], op=ALU.add), reads=[yb], writes=[ot])
            for hb in range(2):
                tr.op("dve", lambda e, hb=hb: e.bn_stats(out=st_.ap[:, hb * 6:hb * 6 + 6], in_=ot.t[:, hb * 512:(hb + 1) * 512]),
                      reads=[ot], pwrites=[st_])
            tr.op("dve", lambda e: e.bn_aggr(out=mv_.ap, in_=st_.ap), reads=[st_], writes=[mv_])
            rstd_from(mv_.ap[:, 1:2], [mv_], 1.0, LN_EPS, lnv_, rs_)
            tr.op("dve", lambda e: e.scalar_tensor_tensor(out=nmr_.ap, in0=mv_.ap[:, 0:1], scalar=-1.0, in1=rs_.ap, op0=ALU.mult, op1=ALU.mult),
                  reads=[mv_, rs_], writes=[nmr_])
            tr.op("act", lambda e: e.activation(out=ot.t[:], in_=ot.t[:], func=AF.Identity, scale=rs_.ap, bias=nmr_.ap),
                  reads=[rs_, nmr_], writes=[ot])
            tr.op("dve", lambda e: e.tensor_tensor(out=ot.t[:], in0=ot.t[:], in1=ln2b.t[:, 0, :], op=ALU.mult), reads=[ln2b], writes=[ot])
            tr.op("pool", lambda e: e.tensor_tensor(out=ot.t[:], in0=ot.t[:], in1=ln2b.t[:, 1, :], op=ALU.add), reads=[ln2b], writes=[ot])
            tr.dma("act", yc[gtok:gtok + 128, :], ot.t[:], reads=[ot], is_out=True)

        load_mixer_weights()

        def interleave(*lists):
            lists = [l for l in lists if l]
            idx = [0] * len(lists)
            while True:
                best, bt = None, None
                for k, l in enumerate(lists):
                    if idx[k] < len(l):
                        t = tr.est_start(l[idx[k]]) if SCHED else idx[k] / len(l)
                        if bt is None or t < bt - 1e-9:
                            best, bt = k, t
                if best is None:
                    break
                tr.emit(lists[best][idx[best]])
                idx[best] += 1

        def rec(fn, i):
            if i >= len(chunks):
                return []
            tr.record()
            fn(*chunks[i])
            return tr.stop()

        chunks = [(u, c) for u in range(4) for c in range(8)]
        NCK = len(chunks)
        cstate = {"ci": 0}

        def conv_step(i):
            conv_store()
            if cstate["ci"] < 3 * NE:
                conv_load(cstate["ci"])
                cstate["ci"] += 1
            if i % 2 == 1 and cstate["ci"] < 3 * NE:
                conv_store()
                conv_load(cstate["ci"])
                cstate["ci"] += 1

        chunk_A(*chunks[0])
        done = {"A": 0, "B": -1, "C": -1}
        cur = {"A": None, "B": None, "C": None}
        pos = {"A": 0, "B": 0, "C": 0}
        fns = {"A": chunk_A, "B": chunk_B, "C": chunk_C}

        def eligible(stg):
            k = done[stg] + 1
            if k >= NCK:
                return False
            if stg == "A":
                return done["C"] >= k - 3 and done["B"] >= k - 2
            if stg == "B":
                return done["A"] >= k and done["C"] >= k - 2
            return done["B"] >= k

        while True:
            for stg in ("C", "B", "A"):
                if cur[stg] is None and eligible(stg):
                    k = done[stg] + 1
                    if stg == "A":
                        conv_step(k - 2 if k >= 2 else 0)
                    tr.record()
                    fns[stg](*chunks[k])
                    cur[stg] = tr.stop()
                    pos[stg] = 0
            best, bt = None, None
            for stg in ("C", "B", "A"):
                if cur[stg] is not None:
                    t = tr.est_start(cur[stg][pos[stg]])
                    if bt is None or t < bt - 1e-9:
                        best, bt = stg, t
            if best is None:
                break
            tr.emit(cur[best][pos[best]])
            pos[best] += 1
            if pos[best] >= len(cur[best]):
                done[best] += 1
                cur[best] = None
        while cstate["ci"] < 3 * NE:
            conv_store()
            conv_load(cstate["ci"])
            cstate["ci"] += 1
        ci = cstate["ci"]
        conv_store()
        assert ci == 3 * NE
        if stop_after >= 2:
            routing_tables()
        if stop_after >= 3:
            for i_ in range(2):
                tr.op("dve", lambda e, i_=i_: e.memset(xg[i_].t[:], 0.0), writes=[xg[i_]], pwrites=XG_ALIAS[i_])
            tile_fetch(0)
            for s_ in range(NTILE):
                if s_ + 1 < NTILE:
                    tile_fetch(s_ + 1)
                tile_compute(s_)
        if stop_after >= 4:
            for g0 in range(0, NCH, 4):
                ls = []
                for gc in range(g0, g0 + 4):
                    tr.record()
                    ln2_chunk(gc)
                    ls.append(tr.stop())
                interleave(*ls)
        if debug:
            dbg_d = dt_("dbg", [128, 512], F32, "ExternalOutput")
            dbgs = sb([128, 512], F32, dma=True)
            tr.op("dve", lambda e: e.memset(dbgs.t[:], 0.0), writes=[dbgs])
            tr.op("dve", lambda e: e.tensor_copy(out=dbgs.t[:, 0:16], in_=run.t[:]), reads=[run], pwrites=[dbgs])
            if stop_after >= 2:
                tr.op("dve", lambda e: e.tensor_copy(out=dbgs.t[:, 16:32], in_=nt.ap), reads=[nt], pwrites=[dbgs])
                tr.op("dve", lambda e: e.tensor_copy(out=dbgs.t[:, 32:48], in_=base.ap), reads=[base], pwrites=[dbgs])
                tr.op("dve", lambda e: e.tensor_copy(out=dbgs.t[:, 48:48 + NTILE], in_=teb.t[:]), reads=[teb], pwrites=[dbgs])
                tr.op("dve", lambda e: e.tensor_copy(out=dbgs.t[:, 128:128 + NTILE], in_=widx.t[:]), reads=[widx], pwrites=[dbgs])
            tr.dma("sp", dbg_d[:, :], dbgs.t[:], reads=[dbgs], is_out=True)
            e_ = tr.E["sp"]
            for b_ in (x1s_b, res_b, sinfo_b, ybuf_b):
                tr._wait(e_, b_.w)

        e = tr.E["sp"]
        tr._wait(e, tr.out_toks)
    return nc


def _host_consts():
    c = np.zeros((128, NCONST), np.float32)
    idx = np.arange(128)
    c[:, C_ID:C_ID + 128] = np.eye(128)
    c[:, C_U:C_U + 128] = (idx[:, None] <= idx[None, :])
    c[:, C_LS:C_LS + 128] = (idx[:, None] > idx[None, :])
    c[:, C_MASK:C_MASK + 128] = (idx[None, :] >= idx[:, None])
    c[:, C_AI:C_AI + 128] = np.eye(128) * np.float32(ALPHA)
    c[:, C_ONES:C_ONES + 128] = 1.0
    for g, w in enumerate(WINDOWS):
        tp = idx[:, None]
        t = idx[None, :]
        cur = ((tp <= t) & (tp > t - w)).astype(np.float64) / w - np.eye(128)
        prev = ((tp - 128) > (t - w)).astype(np.float64) / w
        cntf = np.minimum(t + 1, w).astype(np.float64)
        first = ((tp <= t) & (tp > t - w)).astype(np.float64) / cntf - np.eye(128)
        for kind, m in enumerate((cur, prev, first)):
            o = C_BAND + (kind * 4 + g) * 128
            c[:, o:o + 128] = m.astype(np.float32)
    c[:, C_MISC] = idx
    c[:, C_MISC + 1] = 2 * idx
    c[:, C_MISC + 2] = 2 * idx + 1
    c[:, C_MISC + 3:C_MISC + 35] = np.arange(32)[None, :]
    c[:, C_MISC + 35:C_MISC + 51] = (16 - np.arange(16))[None, :]
    c[:, C_US:C_US + 128] = (idx[:, None] < idx[None, :])
    c[:, C_S:C_S + 64] = np.arange(64)[None, :]
    return c


_NC_CACHE = {}


def _prep_inputs(inp):
    f = lambda a: np.ascontiguousarray(np.asarray(a, dtype=np.float32))
    pp = np.zeros((128, NPP), np.float32)
    pp[:, PP_G0:PP_G0 + 8] = f(inp["ln0_g"]).reshape(8, 128).T
    pp[:, PP_B0:PP_B0 + 8] = f(inp["ln0_b"]).reshape(8, 128).T
    pp[:, PP_G1:PP_G1 + 8] = f(inp["ln1_g"])[0].reshape(8, 128).T
    pp[:, PP_B1:PP_B1 + 8] = f(inp["ln1_b"])[0].reshape(8, 128).T
    cw = f(inp["conv_w"])[0]
    pp[:, PP_CW:PP_CW + 32] = cw.reshape(4, 8, 128).transpose(2, 1, 0).reshape(128, 32)
    pp[:, PP_CB:PP_CB + 8] = f(inp["conv_b"])[0].reshape(8, 128).T
    pp[:, PP_PS:PP_PS + 4] = f(inp["pool_scale"])[0].reshape(4, 128).T
    pp[:, PP_PB:PP_PB + 4] = f(inp["b_pool"])[0].reshape(4, 128).T
    rowp = np.concatenate([f(inp["dt_bias"])[0], f(inp["a_log"])[0], f(inp["d_skip"])[0],
                           f(inp["b_router_group"])[0], f(inp["b_router_expert"])[0], f(inp["ssm_norm_g"])[0]]).astype(np.float32)
    assert rowp.shape[0] == NRP
    ln2rows = np.stack([f(inp["ln2_g"])[0], f(inp["ln2_b"])[0]])
    wr = np.ascontiguousarray(np.concatenate([f(inp["w_router_group"])[0], f(inp["w_router_expert"])[0]], axis=1))
    shared = {
        "consts": _host_consts(), "pp": pp, "rowp": rowp, "ln2rows": ln2rows, "wr": wr,
        "identb": np.eye(128, dtype=np.float32).astype(ml_dtypes.bfloat16),
        "w_in": f(inp["w_in"])[0], "w_out": f(inp["w_out"])[0], "w_pool": f(inp["w_pool"])[0],
        "w_gate": np.ascontiguousarray(f(inp["w_gate"])[0].reshape(NE, 8, 128, 512).transpose(0, 2, 1, 3)).reshape(NE * 256, 2048),
        "w_up": np.ascontiguousarray(f(inp["w_up"])[0].reshape(NE, 8, 128, 512).transpose(0, 2, 1, 3)).reshape(NE * 256, 2048),
        "w_down": np.ascontiguousarray(f(inp["w_down"])[0].reshape(NE, 4, 128, 1024).transpose(0, 2, 1, 3)).reshape(NE * 256, 2048),
    }
    return shared


def kernel(**inputs):
    x = np.asarray(inputs["x"], dtype=np.float32)
    shared = _prep_inputs(inputs)
    if "nc" not in _NC_CACHE:
        _NC_CACHE["nc"] = build(4)
    nc = _NC_CACHE["nc"]
    in_maps = []
    for i in range(NCORES):
        m = dict(shared)
        m["xc"] = np.ascontiguousarray(x[2 * i:2 * i + 2].reshape(TOK_PER_CORE, D))
        in_maps.append(m)
    res = run_bass_kernel_spmd(nc, in_maps, core_ids=list(range(NCORES)))
    out = np.empty((16, 2048, D), np.float32)
    for i in range(NCORES):
        out[2 * i:2 * i + 2] = np.asarray(res.results[i]["yc"]).reshape(2, 2048, D)
    return out
```

```python
import contextlib
import math

import numpy as np
import ml_dtypes

import concourse.bass as bass
import concourse.mybir as mybir
from concourse.bass_utils import run_bass_kernel_spmd

F32 = mybir.dt.float32
BF16 = mybir.dt.bfloat16
I32 = mybir.dt.int32
AF = mybir.ActivationFunctionType
ALU = mybir.AluOpType
AX = mybir.AxisListType

D = 1024
NCORES = 8
TOK_PER_CORE = 4096
UNIT = 1024
LN_EPS = 1e-5
RMS_EPS = 1e-5
ALPHA = 2.0 ** 0.25
NE = 16
WINDOWS = (2, 4, 8, 16)

C_ID, C_U, C_LS, C_MASK, C_AI, C_ONES, C_BAND = 0, 128, 256, 384, 512, 640, 768
C_MISC = 768 + 12 * 128
C_US = C_MISC + 64
C_S = C_US + 128
NCONST = C_S + 64
NCH = 32
TQ = 4
TS = 128 * TQ
NTILE = NE + (2 * TOK_PER_CORE) // TS
NSLOT = NTILE * TS
PP_G0, PP_B0, PP_G1, PP_B1, PP_CW, PP_CB, PP_PS, PP_PB = 0, 8, 16, 24, 32, 64, 72, 76
NPP = 80
RP_DTB, RP_ALOG, RP_DSKIP, RP_BR, RP_NG = 0, 8, 16, 24, 44
NRP = 556


class Buf:
    def __init__(self, t, dsem=None):
        self.t = t
        self.w = {}
        self.r = {}
        self.dsem = dsem
        self.dcount = 0


class Eng:
    def __init__(self, name, eng, sem):
        self.name, self.eng, self.sem = name, eng, sem
        self.count = 0
        self.waited = {}
        self.free = 0.0


class _FakeEng:
    def __getattr__(self, name):
        def f(*a, **k):
            k["_op"] = name
            k["_args"] = a
            return k
        return f


def _free_size(ap):
    try:
        n = 1
        for d in ap.shape[1:]:
            n *= int(d)
        return n
    except Exception:
        return 256


def _est_cost(kind, en, fn):
    fe = _FakeEng()
    try:
        if kind == "mm":
            tot = 0.0
            for f in fn:
                k = f(fe)
                n = _free_size(k.get("out"))
                src = k.get("lhsT", k.get("in_"))
                f32 = getattr(src, "dtype", None) == F32
                if k["_op"] == "transpose":
                    tot += 0.45 if f32 else 0.11
                elif f32:
                    tot += max(0.2, n / 600.0)
                else:
                    tot += max(0.06, n / 2400.0 + 0.01)
            return tot
        if kind == "dma":
            return 0.15 if en in ("sp", "act") else 1.2
        k = fn(fe)
        out = k.get("out", k.get("ap", k["_args"][0] if k["_args"] else None))
        n = _free_size(out)
        if en == "act":
            return 0.27 + n * 0.00065
        if en == "dve":
            return 0.1 + n / 960.0
        if en == "pool":
            return 0.3 + n * 0.002
    except Exception:
        pass
    return 0.5


SCHED = True
HOP_PE = 12.0
HOP = 3.0
DMA_LAT = 3.0


class _Rec:
    __slots__ = ("kind", "en", "fn", "reads", "writes", "pwrites", "nowait", "extra", "cost")


class Tracker:
    def __init__(self, nc, st):
        self.nc = nc
        self.st = st
        self.E = {}
        for name, eng in (("pe", nc.tensor), ("act", nc.scalar), ("dve", nc.vector),
                          ("pool", nc.gpsimd), ("sp", nc.sync)):
            self.E[name] = Eng(name, eng, st.enter_context(nc.semaphore("sem_" + name)))
        self.out_toks = {}
        self.rec = None
        self.fin = {}

    def record(self):
        self.rec = []

    def stop(self):
        r, self.rec = self.rec, None
        return r

    def _wait(self, e, toks, skip=None):
        for k, (sem, val) in toks.items():
            if k == skip:
                continue
            if e.waited.get(k, 0) >= val:
                continue
            e.eng.wait_ge(sem, val)
            e.waited[k] = val

    def _pre(self, e, reads, writes, pwrites, skip_self=False):
        for b in reads:
            self._wait(e, b.w)
        for b in writes:
            self._wait(e, b.w)
            self._wait(e, b.r)
        for b in pwrites:
            self._wait(e, b.w, skip=id(e.sem))
            self._wait(e, b.r)

    def _post(self, tok, reads, writes, pwrites):
        k = id(tok[0])
        for b in reads:
            b.r[k] = tok
        for b in writes:
            b.w = {k: tok}
            b.r = {}
        for b in pwrites:
            b.w[k] = tok

    def op(self, en, fn, reads=(), writes=(), pwrites=()):
        if self.rec is not None:
            r = _Rec(); r.kind, r.en, r.fn, r.reads, r.writes, r.pwrites, r.nowait, r.extra = "op", en, fn, reads, writes, pwrites, (), None
            r.cost = _est_cost("op", en, fn)
            self.rec.append(r)
            return None
        return self._op(en, fn, reads, writes, pwrites)

    def _op(self, en, fn, reads=(), writes=(), pwrites=()):
        e = self.E[en]
        self._pre(e, reads, writes, pwrites)
        inst = fn(e.eng)
        e.count += 1
        inst.then_inc(e.sem, 1)
        tok = (e.sem, e.count)
        self._post(tok, reads, writes, pwrites)
        return tok

    def mm(self, fns, reads=(), pwrites=()):
        if self.rec is not None:
            r = _Rec(); r.kind, r.en, r.fn, r.reads, r.writes, r.pwrites, r.nowait, r.extra = "mm", "pe", fns, reads, (), pwrites, (), None
            r.cost = _est_cost("mm", "pe", fns)
            self.rec.append(r)
            return None
        return self._mm(fns, reads, pwrites)

    def _mm(self, fns, reads=(), pwrites=()):
        e = self.E["pe"]
        self._pre(e, reads, (), pwrites)
        inst = None
        for fn in fns:
            inst = fn(e.eng)
        e.count += 1
        inst.then_inc(e.sem, 1)
        tok = (e.sem, e.count)
        self._post(tok, reads, (), pwrites)
        return tok

    def dma(self, qn, out, in_, reads=(), pwrites=(), nowait=(), semb=None, is_out=False, fn=None):
        if self.rec is not None:
            r = _Rec(); r.kind, r.en, r.fn, r.reads, r.writes, r.pwrites, r.nowait = "dma", qn, fn, reads, (), pwrites, nowait
            r.extra = (out, in_, semb, is_out)
            r.cost = _est_cost("dma", qn, fn)
            self.rec.append(r)
            return None
        return self._dma(qn, out, in_, reads, pwrites, nowait, semb, is_out, fn)

    def _ready(self, en, reads, writes, pwrites):
        t = 0.0
        own = id(self.E[en].sem)
        hop = HOP_PE if en == "pe" else HOP
        for b in reads:
            for k, (sem, val) in b.w.items():
                t = max(t, self.fin.get((k, val), 0.0) + (0.0 if k == own else hop))
        for b in list(writes) + list(pwrites):
            for d in (b.w, b.r):
                for k, (sem, val) in d.items():
                    t = max(t, self.fin.get((k, val), 0.0) + (0.0 if k == own else hop))
        return t

    def est_start(self, r):
        return max(self.E[r.en].free, self._ready(r.en, r.reads, r.writes, r.pwrites))

    def emit(self, r):
        e = self.E[r.en]
        start = self.est_start(r)
        if r.kind == "op":
            tok = self._op(r.en, r.fn, r.reads, r.writes, r.pwrites)
        elif r.kind == "mm":
            tok = self._mm(r.fn, r.reads, r.pwrites)
        else:
            out, in_, semb, is_out = r.extra
            tok = self._dma(r.en, out, in_, r.reads, r.pwrites, r.nowait, semb, is_out, r.fn)
        e.free = start + r.cost
        fin = e.free + (DMA_LAT if r.kind == "dma" else 0.0)
        self.fin[(id(tok[0]), tok[1])] = fin

    def _dma(self, qn, out, in_, reads=(), pwrites=(), nowait=(), semb=None, is_out=False, fn=None):
        e = self.E[qn]
        if semb is not None:
            sb = semb
        elif nowait:
            sb = next((b for b in reads if b.dsem is not None), nowait[0])
        elif pwrites:
            sb = pwrites[0]
        else:
            sb = reads[0]
        for b in reads:
            self._wait(e, b.w)
        for b in pwrites:
            self._wait(e, b.w, skip=id(sb.dsem))
            self._wait(e, b.r)
        inst = fn(e.eng) if fn is not None else e.eng.dma_start(out=out, in_=in_)
        sb.dcount += 16
        inst.then_inc(sb.dsem, 16)
        tok = (sb.dsem, sb.dcount)
        self._post(tok, reads, (), list(pwrites) + list(nowait))
        if is_out:
            self.out_toks[id(sb.dsem)] = tok
        return tok


def build(n_units=4, stop_after=4, debug=False):
    nc = bass.Bass("TRN2", target_bir_lowering=False)
    ntok = n_units * UNIT
    dt_ = lambda name, shape, dt=F32, kind="ExternalInput": nc.dram_tensor(name, shape, dt, kind=kind).ap()
    xc = dt_("xc", [TOK_PER_CORE, D])
    consts_d = dt_("consts", [128, NCONST])
    pp_d = dt_("pp", [128, NPP])
    rowp_d = dt_("rowp", [NRP])
    ln2_d = dt_("ln2rows", [2, D])
    wr_d = dt_("wr", [D, 20])
    identb_d = dt_("identb", [128, 128], BF16)
    w_in_d = dt_("w_in", [D, 2056])
    w_out_d = dt_("w_out", [D, D])
    w_pool_d = dt_("w_pool", [4, 128, 128])
    w_gate_d = dt_("w_gate", [NE * 256, 2048])
    w_up_d = dt_("w_up", [NE * 256, 2048])
    w_down_d = dt_("w_down", [NE * 256, 2048])
    yc = dt_("yc", [TOK_PER_CORE, D], F32, "ExternalOutput")
    SK = "ExternalOutput" if debug else "Internal"
    x1s_d = dt_("x1s", [TOK_PER_CORE + 1, D], BF16, SK)
    res_d = dt_("res", [TOK_PER_CORE, D], F32, SK)
    sinfo_d = dt_("sinfo", [NSLOT, 4], I32, SK)
    ybuf_d = dt_("ybuf", [2 * TOK_PER_CORE, D], F32, SK)
    wsc_d = dt_("wsc", [NE * 128, 12288], BF16, "Internal")

    with contextlib.ExitStack() as st:
        tr = Tracker(nc, st)
        cnt = [0]

        def sb(shape, dt=F32, dma=False):
            cnt[0] += 1
            t = st.enter_context(nc.sbuf_tensor("sb%d" % cnt[0], shape, dt))
            ds = st.enter_context(nc.semaphore("ds%d" % cnt[0])) if dma else None
            return Buf(t, ds)

        def view(b, ap):
            return ap

        consts = sb([128, NCONST], F32, dma=True)
        pp = sb([128, NPP], F32, dma=True)
        rowp = sb([128, NRP], F32, dma=True)
        ln2b = sb([128, 2, D], F32, dma=True)
        wr = sb([128, 8, 20], F32, dma=True)
        wpool = sb([128, 4, 128], BF16, dma=True)
        identb = sb([128, 128], BF16, dma=True)
        R0 = sb([128, 8 * 2056], BF16, dma=True)
        R1 = sb([128, 12288], BF16, dma=True)
        dwall_t = sb([128, NCH, 16], F32)
        dwall = [Buf(dwall_t.t) for _ in range(NCH)]
        posall_t = sb([128, NCH, 16], F32)
        posall = [Buf(posall_t.t) for _ in range(NCH)]
        run = sb([128, 16], F32)
        xg = [sb([128, 4, D], BF16, dma=True) for _ in range(2)]
        xgT = [sb([128, 8, 512], BF16, dma=True) for _ in range(2)]
        sit = [sb([128, TQ, 4], I32, dma=True) for _ in range(2)]
        rstage = [sb([128, D], F32, dma=True) for _ in range(2)]
        x1stage = [sb([128, D], BF16, dma=True) for _ in range(2)]
        payall = sb([128, NCH, 2, 4], I32, dma=True)
        slotiA = sb([128, NCH, 2], I32)
        smallsB = sb([128, 192], F32)
        widx = sb([128, NTILE], I32)
        teb = sb([128, NTILE], F32)
        x1s_b = Buf(None, st.enter_context(nc.semaphore("dsx1s")))
        res_b = Buf(None, st.enter_context(nc.semaphore("dsres")))
        sinfo_b = Buf(None, st.enter_context(nc.semaphore("dssinfo")))
        sinfo_pre = Buf(None, st.enter_context(nc.semaphore("dssinfopre")))
        ybuf_b = Buf(None, st.enter_context(nc.semaphore("dsybuf")))
        wsc_b = Buf(None, st.enter_context(nc.semaphore("dswsc")))
        cvs = [Buf(None, st.enter_context(nc.semaphore("dscv%d" % i))) for i in range(2)]
        ysem = [Buf(None, st.enter_context(nc.semaphore("dsys%d" % i))) for i in range(2)]
        l2s = [Buf(None, st.enter_context(nc.semaphore("dsl2%d" % i))) for i in range(4)]
        S = sb([128, 512], F32)
        Sb = sb([128, 512], BF16)
        utok = [sb([128, 512], F32) for _ in range(2)]
        ub = [sb([128, 8, 131], F32) for _ in range(2)]
        io = [sb([128, D], F32, dma=True) for _ in range(2)]
        xh32 = sb([128, D], F32)
        xh32c = sb([128, D], F32)
        xT32s = [sb([128, 8, 128], F32) for _ in range(2)]
        x1T32 = sb([128, 8, 128], F32)
        xnT = sb([128, 8, 128], BF16)
        X1 = sb([128, 1024], F32)
        cacc = [Buf(X1.t) for _ in range(8)]
        X2 = sb([128, 1024], F32)
        xsT32 = sb([128, 4, 128], F32)
        BT32 = sb([128, 2, 128], F32)
        BTb = sb([128, 2, 128], BF16)
        CTb = sb([128, 2, 128], BF16)
        szc = sb([128, 512], F32)
        MT = sb([128, 8, 128], BF16)
        scm = sb([128, 2, 128], F32)
        xs_tok = sb([128, 512], F32)
        xdt = sb([128, 512], BF16)
        xdec = sb([128, 512], BF16)
        Btok = sb([128, 256], BF16)
        ybuf = sb([128, 512], F32)
        ynb = sb([128, 512], BF16)
        ycatTs = [sb([128, 8, 128], BF16) for _ in range(2)]
        pT = sb([128, 4, 128], BF16)
        smalls_t = sb([128, 512], F32)
        sm_off = [0]

        def small(n):
            o = sm_off[0]
            sm_off[0] += n
            assert sm_off[0] <= 512
            b = Buf(smalls_t.t)
            b.ap = smalls_t.t[:, o:o + n]
            return b

        stats = small(12); mv = small(2); lnv = small(1); rs = small(1)
        statsC = small(12); mvC = small(2); lnvC = small(1); rsC = small(1)
        stats4 = [small(12) for _ in range(4)]; mv4 = [small(2) for _ in range(4)]; lnv4 = [small(1) for _ in range(4)]
        rs4 = [small(1) for _ in range(4)]; nmr4 = [small(1) for _ in range(4)]
        ahead = small(8); psb = small(4)
        xdtb = small(8); axb = small(8); e1 = small(8); l1 = small(8); dtv = small(8); av = small(8)
        acs = small(8); eac = small(8); dd = small(8); dsv = small(8); dtds = small(8); cdv = small(8)
        ssq = small(2); lns = small(2); rs2 = small(2)
        lg = small(20); gmax = small(1); ngmax = small(1); gexp = small(4); gsum = small(1); gw = small(1)
        gmask = small(4); t44 = small(16); esel = small(4); m1 = small(1); mask1 = small(4); esel2 = small(4)
        m2 = small(1); mask2 = small(4); d21 = small(1); e21 = small(1); den = small(1); p1 = small(1)
        c1 = small(1); c2 = small(1); wsel = small(4)
        dtvs = [dtv, small(8)]; avs = [av, small(8)]
        ind = small(16); nt = small(16); incl = small(16); base = small(16); ones16 = small(16)
        slotf = small(16); cin = small(16); cex = small(16); sel = [small(16), small(16)]; tmp16 = [small(16), small(16)]
        slk = small(2); wk = small(2); tokf = small(1); dstf = small(2)

        class _V:
            pass

        def alias(ap):
            b = Buf(None)
            v = _V()
            v.ap = ap
            b.t = _T(ap)
            return b

        class _T:
            def __init__(self, ap):
                self._ap = ap

            def __getitem__(self, key):
                return self._ap[key] if key != slice(None) else self._ap

        xg0f = xg[0].t[:].rearrange("p q d -> p (q d)").bitcast(F32)
        xg1f = xg[1].t[:].rearrange("p q d -> p (q d)").bitcast(F32)
        xsT32s = [xsT32, alias(xg0f[:, 0:512].rearrange("p (m t) -> p m t", m=4))]
        BT32s = [BT32, alias(xg0f[:, 512:768].rearrange("p (g t) -> p g t", g=2))]
        szcs = [szc, alias(xg0f[:, 768:1280])]
        utok3 = [utok[0], utok[1], alias(xg0f[:, 1280:1792])]
        BTbs = [BTb, alias(xg0f[:, 1792:1920].bitcast(BF16).rearrange("p (g t) -> p g t", g=2))]
        CTbs = [CTb, alias(xg0f[:, 1920:2048].bitcast(BF16).rearrange("p (g t) -> p g t", g=2))]
        xT32s.append(alias(xg1f[:, 0:1024].rearrange("p (j t) -> p j t", j=8)))
        XG_ALIAS = [[xsT32s[1], BT32s[1], szcs[1], utok3[2], BTbs[1], CTbs[1]], [xT32s[2]]]
        banks = []
        for i in range(8):
            t = st.enter_context(nc.psum_tensor("ps%d" % i, [128, 512], F32))
            banks.append(Buf(t))
        bank_i = [0]

        def bank():
            b = banks[bank_i[0] % 7]
            bank_i[0] += 1
            return b

        pool_a = [0]
        pool_b = [0]
        pool_c = [0]

        def bank_a():
            b = banks[pool_a[0] % 2]
            pool_a[0] += 1
            return b

        def bank_b():
            b = banks[2 + pool_b[0] % 3]
            pool_b[0] += 1
            return b

        def bank_c():
            b = banks[5 + pool_c[0] % 2]
            pool_c[0] += 1
            return b

        C = consts.t
        ident32 = C[:, C_ID:C_ID + 128]
        Umat = C[:, C_U:C_U + 128]
        Lsmat = C[:, C_LS:C_LS + 128]
        mask01 = C[:, C_MASK:C_MASK + 128]
        alphaI = C[:, C_AI:C_AI + 128]
        ones = C[:, C_ONES:C_ONES + 128]
        iota_p = C[:, C_MISC:C_MISC + 1]
        c2ph = C[:, C_MISC + 1:C_MISC + 3]
        sconst = C[:, C_S:C_S + NTILE]
        cconst = C[:, C_S:C_S + NCH]
        Ustrict = C[:, C_US:C_US + 128]
        rev_e = C[:, C_MISC + 35:C_MISC + 51]

        def band(kind, g):
            o = C_BAND + (kind * 4 + g) * 128
            return C[:, o:o + 128]

        P = pp.t
        RW = rowp.t

        tr.dma("sp", consts.t[:], consts_d[:, :], pwrites=[consts])
        tr.dma("sp", pp.t[:], pp_d[:, :], pwrites=[pp])
        tr.dma("sp", rowp.t[:], rowp_d.partition_broadcast(128), pwrites=[rowp])
        for i in range(2):
            tr.dma("sp", ln2b.t[:, i, :], ln2_d[i, :].partition_broadcast(128), pwrites=[ln2b])
        tr.dma("sp", wr.t[:], wr_d.rearrange("(j p) n -> p j n", p=128), pwrites=[wr])
        tr.dma("sp", identb.t[:], identb_d[:, :], pwrites=[identb])
        tr.dma("pool", wpool.t[:], w_pool_d.rearrange("g c d -> c g d"), pwrites=[wpool])
        tr.op("act", lambda e: e.activation(out=ahead.ap, in_=RW[:, RP_ALOG:RP_ALOG + 8], func=AF.Exp),
              reads=[rowp], writes=[ahead])
        tr.op("dve", lambda e: e.tensor_scalar(out=ahead.ap, in0=ahead.ap, scalar1=-1.0, scalar2=None, op0=ALU.mult),
              reads=[], writes=[ahead])
        tr.op("dve", lambda e: e.tensor_tensor(out=psb.ap, in0=P[:, PP_PB:PP_PB + 4], in1=P[:, PP_PS:PP_PS + 4], op=ALU.mult),
              reads=[pp], writes=[psb])

        tr.op("pool", lambda e: e.memset(run.t[:], 0.0), writes=[run])
        tr.op("pool", lambda e: e.memset(ones16.ap, 1.0), writes=[ones16])
        zrow_ap = io[1].t[0:1, 0:512].bitcast(BF16)
        tr.op("pool", lambda e: e.memset(io[1].t[0:1, 0:512], 0.0), writes=[io[1]])
        tr.dma("sp", x1s_d[TOK_PER_CORE:TOK_PER_CORE + 1, :], zrow_ap, reads=[io[1]], nowait=[x1s_b])
        padt_ap = io[0].t[:, 0:NSLOT // 32].bitcast(I32).rearrange("p (a f) -> p a f", f=4)
        tr.op("pool", lambda e: e.memset(padt_ap[:, :, 0:1], 1000000), pwrites=[io[0]])
        tr.op("pool", lambda e: e.memset(padt_ap[:, :, 1:2], 0), pwrites=[io[0]])
        tr.op("pool", lambda e: e.memset(padt_ap[:, :, 2:3], 1000000), pwrites=[io[0]])
        tr.op("pool", lambda e: e.memset(padt_ap[:, :, 3:4], 0), pwrites=[io[0]])
        tr.dma("sp", sinfo_d.rearrange("(p a) f -> p a f", p=128), padt_ap, reads=[io[0]], pwrites=[sinfo_pre])
        tr.op("pool", lambda e: e.memset(payall.t[:], 0), writes=[payall])

        reg_ns = nc.gpsimd.alloc_register("bc_nslot")
        nc.gpsimd.reg_mov(reg_ns, NSLOT - 1)
        reg_nx = nc.gpsimd.alloc_register("bc_nx")
        nc.gpsimd.reg_mov(reg_nx, TOK_PER_CORE)
        reg_nw = nc.gpsimd.alloc_register("bc_nw")
        nc.gpsimd.reg_mov(reg_nw, NE * 128 - 1)
        reg_ny = nc.gpsimd.alloc_register("bc_ny")
        nc.gpsimd.reg_mov(reg_ny, 2 * TOK_PER_CORE - 1)

        def rstd_from(var_ap, var_bufs, scale, eps, lnbuf, outbuf):
            tr.op("act", lambda e: e.activation(out=lnbuf.ap, in_=var_ap, func=AF.Ln, bias=eps_ap(eps), scale=scale),
                  reads=var_bufs + [epsb], writes=[lnbuf])
            tr.op("act", lambda e: e.activation(out=outbuf.ap, in_=lnbuf.ap, func=AF.Exp, scale=-0.5),
                  reads=[lnbuf], writes=[outbuf])

        epsb = small(2)
        tr.op("pool", lambda e: e.memset(epsb.ap[:, 0:1], LN_EPS), writes=[epsb])
        one_b = small(1)
        tr.op("pool", lambda e: e.memset(one_b.ap, 1.0), writes=[one_b])

        def eps_ap(eps):
            return epsb.ap[:, 0:1]

        def load_mixer_weights():
            w3 = R0.t[:, 0:8 * 2056].rearrange("p (j n) -> p j n", j=8)
            src = w_in_d.rearrange("(j p) n -> p j n", p=128)
            tr.dma("pool", w3[:, :, 0:1024], src[:, :, 0:1024], pwrites=[R0])
            tr.dma("pool", w3[:, :, 1024:2056], src[:, :, 1024:2056], pwrites=[R0])
            wo3 = R1.t[:, 0:8192].rearrange("p (j n) -> p j n", j=8)
            tr.dma("pool", wo3, w_out_d.rearrange("(j p) n -> p j n", p=128), pwrites=[R1])

        win = R0.t[:, 0:8 * 2056].rearrange("p (j n) -> p j n", j=8)
        wout = R1.t[:, 0:8192].rearrange("p (j n) -> p j n", j=8)

        def chunk_A(u, c):
            gtok = u * UNIT + c * 128
            gc = u * 8 + c
            cs = (gtok % 2048) // 128
            first = cs == 0
            xT32_ = xT32s[gc % 3]
            xt = io[c % 2]
            ubc, ubp = ub[c % 2], ub[(c + 1) % 2]
            utc = utok3[gc % 3]
            sl = gc % 2
            xsT32_, BT32_, BTb_, CTb_, szc_, dtv_, av_ = xsT32s[sl], BT32s[sl], BTbs[sl], CTbs[sl], szcs[sl], dtvs[sl], avs[sl]
            pS = banks[7]
            tr.dma("sp", xt.t[:], xc[gtok:gtok + 128, :], pwrites=[xt])
            for hlf in range(2):
                tr.op("dve", lambda e, hlf=hlf: e.bn_stats(out=stats.ap[:, hlf * 6:hlf * 6 + 6], in_=xt.t[:, hlf * 512:(hlf + 1) * 512]),
                      reads=[xt], pwrites=[stats])
            tr.op("dve", lambda e: e.bn_aggr(out=mv.ap, in_=stats.ap), reads=[stats], writes=[mv])
            rstd_from(mv.ap[:, 1:2], [mv], 1.0, LN_EPS, lnv, rs)
            tr.op("dve", lambda e: e.tensor_scalar(out=xh32.t[:], in0=xt.t[:], scalar1=mv.ap[:, 0:1], scalar2=rs.ap,
                                                   op0=ALU.subtract, op1=ALU.mult),
                  reads=[xt, mv, rs], writes=[xh32])
            pA, pB = bank_a(), bank_a()
            for hb, pb in enumerate((pA, pB)):
                tr.mm([lambda e, j=j, pb=pb: e.transpose(out=pb.t[:, (j % 4) * 128:(j % 4 + 1) * 128],
                                                         in_=xh32.t[:, j * 128:(j + 1) * 128], identity=ident32)
                       for j in range(hb * 4, hb * 4 + 4)], reads=[xh32, consts], pwrites=[pb])
            for j in range(8):
                pb = pA if j < 4 else pB
                tr.op("act", lambda e, j=j, pb=pb: e.activation(out=xT32_.t[:, j, :], in_=pb.t[:, (j % 4) * 128:(j % 4 + 1) * 128],
                                                                func=AF.Identity, scale=P[:, PP_G0 + j:PP_G0 + j + 1],
                                                                bias=P[:, PP_B0 + j:PP_B0 + j + 1]),
                      reads=[pb, pp], pwrites=[xT32_])
            tr.op("dve", lambda e: e.tensor_copy(out=xnT.t[:], in_=xT32_.t[:]), reads=[xT32_], writes=[xnT])
            pX = [bank_a(), bank_a()]
            for hb in range(2):
                fns = []
                for m in range(hb * 4, hb * 4 + 4):
                    for j in range(8):
                        fns.append(lambda e, m=m, j=j, hb=hb: e.matmul(
                            out=pX[hb].t[:, (m % 4) * 128:(m % 4 + 1) * 128],
                            lhsT=win[:, j, 512 + m * 128:512 + (m + 1) * 128], rhs=xnT.t[:, j, :],
                            start=(j == 0), stop=(j == 7)))
                tr.mm(fns, reads=[xnT, R0], pwrites=[pX[hb]])
            for hb in range(2):
                tr.op("act", lambda e, hb=hb: e.activation(out=ubc.t[:, hb * 4:hb * 4 + 4, 3:131],
                                                           in_=pX[hb].t[:].rearrange("p (m t) -> p m t", m=4), func=AF.Identity),
                      reads=[pX[hb]], pwrites=[ubc])
            pZ = bank_a()
            tr.mm([lambda e, j=j: e.matmul(out=pZ.t[:], lhsT=xnT.t[:, j, :], rhs=win[:, j, 0:512], start=(j == 0), stop=(j == 7))
                   for j in range(8)], reads=[xnT, R0], pwrites=[pZ])
            pU = bank_a()
            tr.mm([lambda e, j=j: e.matmul(out=pU.t[:], lhsT=xnT.t[:, j, :], rhs=win[:, j, 1544:2056], start=(j == 0), stop=(j == 7))
                   for j in range(8)], reads=[xnT, R0], pwrites=[pU])
            tr.mm([lambda e, j=j: e.matmul(out=pS.t[:, 0:8], lhsT=xnT.t[:, j, :], rhs=win[:, j, 1536:1544], start=(j == 0), stop=(j == 7))
                   for j in range(8)], reads=[xnT, R0], pwrites=[pS])
            if first:
                tr.op("pool", lambda e: e.memset(ubc.t[:, :, 0:3], 0.0), pwrites=[ubc])
            else:
                tr.op("pool", lambda e: e.tensor_copy(out=ubc.t[:, :, 0:3], in_=ubp.t[:, :, 128:131]), reads=[ubp], pwrites=[ubc])
            cav = X1.t[:].rearrange("p (m t) -> p m t", m=8)
            for m in range(8):
                tr.op("pool", lambda e, m=m: e.tensor_scalar(out=cav[:, m, :], in0=ubc.t[:, m, 0:128],
                                                             scalar1=P[:, PP_CW + m * 4:PP_CW + m * 4 + 1],
                                                             scalar2=P[:, PP_CB + m:PP_CB + m + 1], op0=ALU.mult, op1=ALU.add),
                      reads=[ubc, pp], writes=[cacc[m]], pwrites=[X1])
            for k in range(1, 4):
                for m in range(8):
                    tr.op("dve", lambda e, m=m, k=k: e.scalar_tensor_tensor(
                        out=cav[:, m, :], in0=ubc.t[:, m, k:k + 128], scalar=P[:, PP_CW + m * 4 + k:PP_CW + m * 4 + k + 1],
                        in1=cav[:, m, :], op0=ALU.mult, op1=ALU.add), reads=[ubc, pp], writes=[cacc[m]])
            tr.op("act", lambda e: e.activation(out=xsT32_.t[:], in_=cav[:, 0:4, :], func=AF.Silu), reads=cacc[0:4], writes=[xsT32_])
            tr.op("act", lambda e: e.activation(out=BT32_.t[:], in_=cav[:, 4:6, :], func=AF.Silu), reads=cacc[4:6], writes=[BT32_])
            tr.op("act", lambda e: e.activation(out=CTb_.t[:], in_=cav[:, 6:8, :], func=AF.Silu), reads=cacc[6:8], writes=[CTb_])
            tr.op("act", lambda e: e.activation(out=szc_.t[:], in_=pZ.t[:], func=AF.Silu), reads=[pZ], writes=[szc_])
            tr.op("pool", lambda e: e.tensor_copy(out=BTb_.t[:], in_=BT32_.t[:]), reads=[BT32_], writes=[BTb_])
            tr.op("act", lambda e: e.activation(out=utc.t[:], in_=pU.t[:], func=AF.Identity), reads=[pU], writes=[utc])
            tr.op("dve", lambda e: e.tensor_tensor(out=xdtb.ap, in0=pS.t[:, 0:8], in1=RW[:, RP_DTB:RP_DTB + 8], op=ALU.add),
                  reads=[pS, rowp], writes=[xdtb])
            tr.op("act", lambda e: e.activation(out=axb.ap, in_=xdtb.ap, func=AF.Abs), reads=[xdtb], writes=[axb])
            tr.op("act", lambda e: e.activation(out=e1.ap, in_=axb.ap, func=AF.Exp, scale=-1.0), reads=[axb], writes=[e1])
            tr.op("act", lambda e: e.activation(out=l1.ap, in_=e1.ap, func=AF.Ln, bias=one_b.ap, scale=1.0), reads=[e1, one_b], writes=[l1])
            tr.op("dve", lambda e: e.scalar_tensor_tensor(out=dtv_.ap, in0=xdtb.ap, scalar=0.0, in1=l1.ap, op0=ALU.max, op1=ALU.add),
                  reads=[xdtb, l1], writes=[dtv_])
            tr.op("dve", lambda e: e.tensor_tensor(out=av_.ap, in0=dtv_.ap, in1=ahead.ap, op=ALU.mult), reads=[dtv_, ahead], writes=[av_])
        def chunk_B(u, c):
            gtok = u * UNIT + c * 128
            gc = u * 8 + c
            cs = (gtok % 2048) // 128
            first = cs == 0
            ycatT_ = ycatTs[gc % 2]
            utc, utp = utok3[gc % 3], utok3[(gc + 2) % 3]
            sl = gc % 2
            xsT32_, BT32_, BTb_, CTb_, szc_, dtv_, av_ = xsT32s[sl], BT32s[sl], BTbs[sl], CTbs[sl], szcs[sl], dtvs[sl], avs[sl]
            pS = banks[7]
            if first:
                tr.op("pool", lambda e: e.memset(Sb.t[:], 0.0), writes=[Sb])
            tr.mm([lambda e: e.matmul(out=pS.t[:, 8:16], lhsT=Umat, rhs=av_.ap, start=True, stop=True),
                   lambda e: e.matmul(out=pS.t[:, 16:24], lhsT=ones, rhs=av_.ap, start=True, stop=True)],
                  reads=[av_, consts], pwrites=[pS])
            Rt = X2.t[:].rearrange("p (h l) -> p h l", h=8)
            for h in range(8):
                tr.op("dve", lambda e, h=h: e.tensor_scalar(out=Rt[:, h, :], in0=Umat, scalar1=av_.ap[:, h:h + 1], scalar2=None, op0=ALU.mult),
                      reads=[av_, consts], pwrites=[X2])
            pD = [bank_b(), bank_b()]
            for hb in range(2):
                tr.mm([lambda e, hb=hb: e.matmul(out=pD[hb].t[:], lhsT=Lsmat, rhs=X2.t[:, hb * 512:(hb + 1) * 512], start=True, stop=True)],
                      reads=[X2, consts], pwrites=[pD[hb]])
            for hb in range(2):
                tr.op("act", lambda e, hb=hb: e.activation(out=X2.t[:, hb * 512:(hb + 1) * 512], in_=pD[hb].t[:], func=AF.Exp),
                      reads=[pD[hb]], pwrites=[X2])
            scv = pS.t[:, 256:512].rearrange("p (g l) -> p g l", g=2)
            tr.mm([lambda e, g=g: e.matmul(out=scv[:, g, :], lhsT=BTb_.t[:, g, :], rhs=CTb_.t[:, g, :], start=True, stop=True)
                   for g in range(2)], reads=[BTb_, CTb_], pwrites=[pS])
            tr.op("dve", lambda e: e.tensor_tensor(out=scm.t[:], in0=scv, in1=mask01.unsqueeze(1).to_broadcast([128, 2, 128]), op=ALU.mult),
                  reads=[consts, pS], writes=[scm])
            for g in range(2):
                tr.op("dve", lambda e, g=g: e.tensor_tensor(out=MT.t[:, g * 4:(g + 1) * 4, :], in0=Rt[:, g * 4:(g + 1) * 4, :],
                                                            in1=scm.t[:, g, :].unsqueeze(1).to_broadcast([128, 4, 128]), op=ALU.mult),
                      reads=[X2, scm], pwrites=[MT])
            pXs = bank_b()
            tr.mm([lambda e, m=m: e.transpose(out=pXs.t[:, m * 128:(m + 1) * 128], in_=xsT32_.t[:, m, :], identity=ident32)
                   for m in range(4)], reads=[xsT32_, consts], pwrites=[pXs])
            pBt = bank_b()
            tr.mm([lambda e, g=g: e.transpose(out=pBt.t[:, g * 128:(g + 1) * 128], in_=BT32_.t[:, g, :], identity=ident32)
                   for g in range(2)], reads=[BT32_, consts], pwrites=[pBt])
            tr.op("act", lambda e: e.activation(out=xs_tok.t[:], in_=pXs.t[:], func=AF.Identity), reads=[pXs], writes=[xs_tok])
            tr.op("act", lambda e: e.activation(out=Btok.t[:], in_=pBt.t[:, 0:256], func=AF.Identity), reads=[pBt], writes=[Btok])
            tr.op("act", lambda e: e.activation(out=eac.ap, in_=pS.t[:, 8:16], func=AF.Exp), reads=[pS], writes=[eac])
            tr.op("act", lambda e: e.activation(out=acs.ap, in_=pS.t[:, 8:16], func=AF.Identity), reads=[pS], writes=[acs])
            tr.op("act", lambda e: e.activation(out=cdv.ap, in_=pS.t[:, 16:24], func=AF.Exp), reads=[pS], writes=[cdv])
            tr.op("dve", lambda e: e.tensor_tensor(out=dd.ap, in0=pS.t[:, 16:24], in1=acs.ap, op=ALU.subtract), reads=[pS, acs], writes=[dd])
            tr.op("act", lambda e: e.activation(out=dsv.ap, in_=dd.ap, func=AF.Exp), reads=[dd], writes=[dsv])
            tr.op("dve", lambda e: e.tensor_tensor(out=dtds.ap, in0=dtv_.ap, in1=dsv.ap, op=ALU.mult), reads=[dtv_, dsv], writes=[dtds])
            xs3 = pXs.t[:].rearrange("p (h q) -> p h q", h=8)
            tr.op("dve", lambda e: e.tensor_tensor(out=xdt.t[:].rearrange("p (h q) -> p h q", h=8), in0=xs3,
                                                   in1=dtv_.ap.unsqueeze(2).to_broadcast([128, 8, 64]), op=ALU.mult),
                  reads=[pXs, dtv_], writes=[xdt])
            tr.op("dve", lambda e: e.tensor_tensor(out=xdec.t[:].rearrange("p (h q) -> p h q", h=8), in0=xs3,
                                                   in1=dtds.ap.unsqueeze(2).to_broadcast([128, 8, 64]), op=ALU.mult),
                  reads=[pXs, dtds], writes=[xdec])
            pYd = bank_b()
            tr.mm([lambda e, h=h: e.matmul(out=pYd.t[:, h * 64:(h + 1) * 64], lhsT=MT.t[:, h, :], rhs=xdt.t[:, h * 64:(h + 1) * 64],
                                           start=True, stop=True) for h in range(8)], reads=[MT, xdt], pwrites=[pYd])
            pYo = bank_b()
            tr.mm([lambda e, g=g: e.matmul(out=pYo.t[:, g * 256:(g + 1) * 256], lhsT=CTb_.t[:, g, :], rhs=Sb.t[:, g * 256:(g + 1) * 256],
                                           start=True, stop=True) for g in range(2)], reads=[CTb_, Sb], pwrites=[pYo])
            pSt = bank_b()
            tr.mm([lambda e, g=g: e.matmul(out=pSt.t[:, g * 256:(g + 1) * 256], lhsT=Btok.t[:, g * 128:(g + 1) * 128],
                                           rhs=xdec.t[:, g * 256:(g + 1) * 256], start=True, stop=True) for g in range(2)],
                  reads=[Btok, xdec], pwrites=[pSt])
            y3 = ybuf.t[:].rearrange("p (h q) -> p h q", h=8)
            tr.op("dve", lambda e: e.tensor_tensor(out=y3, in0=pYo.t[:].rearrange("p (h q) -> p h q", h=8),
                                                   in1=eac.ap.unsqueeze(2).to_broadcast([128, 8, 64]), op=ALU.mult),
                  reads=[pYo, eac], writes=[ybuf])
            tr.op("dve", lambda e: e.tensor_tensor(out=ybuf.t[:], in0=pYd.t[:], in1=ybuf.t[:], op=ALU.add), reads=[pYd], writes=[ybuf])
            tr.op("pool", lambda e: e.tensor_tensor(out=xs_tok.t[:].rearrange("p (h q) -> p h q", h=8),
                                                    in0=xs_tok.t[:].rearrange("p (h q) -> p h q", h=8),
                                                    in1=RW[:, RP_DSKIP:RP_DSKIP + 8].unsqueeze(2).to_broadcast([128, 8, 64]), op=ALU.mult),
                  reads=[rowp], writes=[xs_tok])
            tr.op("pool", lambda e: e.tensor_tensor(out=ybuf.t[:], in0=ybuf.t[:], in1=xs_tok.t[:], op=ALU.add), reads=[xs_tok], writes=[ybuf])
            tr.op("pool", lambda e: e.tensor_tensor(out=ybuf.t[:], in0=ybuf.t[:], in1=szc_.t[:], op=ALU.mult), reads=[szc_], writes=[ybuf])
            for g in range(2):
                tr.op("act", lambda e, g=g: e.activation(out=ynb.t[:, g * 256:(g + 1) * 256], in_=ybuf.t[:, g * 256:(g + 1) * 256],
                                                         func=AF.Square, accum_out=ssq.ap[:, g:g + 1]),
                      reads=[ybuf], pwrites=[ynb, ssq])
            tr.op("act", lambda e: e.activation(out=lns.ap, in_=ssq.ap, func=AF.Ln, bias=epsb.ap[:, 0:1], scale=1.0 / 256.0),
                  reads=[ssq, epsb], writes=[lns])
            tr.op("act", lambda e: e.activation(out=rs2.ap, in_=lns.ap, func=AF.Exp, scale=-0.5), reads=[lns], writes=[rs2])
            for g in range(2):
                tr.op("dve", lambda e, g=g: e.scalar_tensor_tensor(out=ynb.t[:, g * 256:(g + 1) * 256], in0=ybuf.t[:, g * 256:(g + 1) * 256],
                                                                   scalar=rs2.ap[:, g:g + 1],
                                                                   in1=RW[:, RP_NG + g * 256:RP_NG + (g + 1) * 256],
                                                                   op0=ALU.mult, op1=ALU.mult),
                      reads=[ybuf, rs2, rowp], pwrites=[ynb])
            pYt = bank_b()
            pYt_b = pYt.t[:].bitcast(BF16)
            tr.mm([lambda e, m=m: e.transpose(out=pYt_b[:, m * 128:(m + 1) * 128], in_=ynb.t[:, m * 128:(m + 1) * 128], identity=identb.t[:])
                   for m in range(4)], reads=[ynb, identb], pwrites=[pYt])
            tr.op("act", lambda e: e.activation(out=ycatT_.t[:, 0:4, :], in_=pYt_b[:, 0:512].rearrange("p (m t) -> p m t", m=4), func=AF.Identity),
                  reads=[pYt], pwrites=[ycatT_])
            if first:
                tr.op("pool", lambda e: e.memset(S.t[:], 0.0), writes=[S])
            else:
                tr.op("pool", lambda e: e.tensor_tensor(out=S.t[:].rearrange("p (h q) -> p h q", h=8),
                                                        in0=S.t[:].rearrange("p (h q) -> p h q", h=8),
                                                        in1=cdv.ap.unsqueeze(2).to_broadcast([128, 8, 64]), op=ALU.mult),
                      reads=[cdv], writes=[S])
            tr.op("dve", lambda e: e.tensor_tensor(out=S.t[:], in0=pSt.t[:], in1=S.t[:], op=ALU.add), reads=[pSt], writes=[S])
            tr.op("act", lambda e: e.activation(out=Sb.t[:], in_=S.t[:], func=AF.Identity), reads=[S], writes=[Sb])
            pP = bank_b()
            fns = []
            for g in range(4):
                fns.append(lambda e, g=g: e.matmul(out=pP.t[:, g * 128:(g + 1) * 128], lhsT=utc.t[:, g * 128:(g + 1) * 128],
                                                   rhs=band(2 if first else 0, g), start=True, stop=first))
                if not first:
                    fns.append(lambda e, g=g: e.matmul(out=pP.t[:, g * 128:(g + 1) * 128], lhsT=utp.t[:, g * 128:(g + 1) * 128],
                                                       rhs=band(1, g), start=False, stop=True))
            tr.mm(fns, reads=[utc, consts] + ([] if first else [utp]), pwrites=[pP])
            tr.op("act", lambda e: e.activation(out=pT.t[:], in_=pP.t[:].rearrange("p (g t) -> p g t", g=4), func=AF.Identity),
                  reads=[pP], writes=[pT])
            pM = bank_b()
            tr.mm([lambda e, g=g: e.matmul(out=pM.t[:, g * 128:(g + 1) * 128], lhsT=wpool.t[:, g, :], rhs=pT.t[:, g, :], start=True, stop=True)
                   for g in range(4)], reads=[wpool, pT], pwrites=[pM])
            for g in range(4):
                tr.op("act", lambda e, g=g: e.activation(out=ycatT_.t[:, 4 + g, :], in_=pM.t[:, g * 128:(g + 1) * 128], func=AF.Identity,
                                                         scale=P[:, PP_PS + g:PP_PS + g + 1], bias=psb.ap[:, g:g + 1]),
                      reads=[pM, pp, psb], pwrites=[ycatT_])
        def chunk_C(u, c):
            gtok = u * UNIT + c * 128
            gc = u * 8 + c
            xT32_ = xT32s[gc % 3]
            ycatT_ = ycatTs[gc % 2]
            pS = banks[7]
            pO = [bank_c(), bank_c()]
            for hb in range(2):
                fns = [lambda e, j=j, hb=hb: e.matmul(out=pO[hb].t[:], lhsT=ycatT_.t[:, j, :], rhs=wout[:, j, hb * 512:(hb + 1) * 512],
                                                      start=(j == 0), stop=(j == 7)) for j in range(8)]
                for jj in range(4):
                    fns.append(lambda e, jj=jj, hb=hb: e.matmul(out=pO[hb].t[:, jj * 128:(jj + 1) * 128], lhsT=xT32_.t[:, hb * 4 + jj, :],
                                                                rhs=alphaI, start=False, stop=True, skip_group_check=True))
                tr.mm(fns, reads=[ycatT_, R1, xT32_, consts], pwrites=[pO[hb]])
            for hb in range(2):
                tr.op("dve", lambda e, hb=hb: e.bn_stats(out=statsC.ap[:, hb * 6:hb * 6 + 6], in_=pO[hb].t[:]), reads=[pO[hb]], pwrites=[statsC])
            tr.op("dve", lambda e: e.bn_aggr(out=mvC.ap, in_=statsC.ap), reads=[statsC], writes=[mvC])
            rstd_from(mvC.ap[:, 1:2], [mvC], 1.0, LN_EPS, lnvC, rsC)
            for hb in range(2):
                tr.op("dve", lambda e, hb=hb: e.tensor_scalar(out=xh32c.t[:, hb * 512:(hb + 1) * 512], in0=pO[hb].t[:], scalar1=mvC.ap[:, 0:1],
                                                              scalar2=rsC.ap, op0=ALU.subtract, op1=ALU.mult),
                      reads=[pO[hb], mvC, rsC], pwrites=[xh32c])
            pA, pB = bank_c(), bank_c()
            for hb, pb in enumerate((pA, pB)):
                tr.mm([lambda e, j=j, pb=pb: e.transpose(out=pb.t[:, (j % 4) * 128:(j % 4 + 1) * 128],
                                                         in_=xh32c.t[:, j * 128:(j + 1) * 128], identity=ident32)
                       for j in range(hb * 4, hb * 4 + 4)], reads=[xh32c, consts], pwrites=[pb])
            for j in range(8):
                pb = pA if j < 4 else pB
                tr.op("act", lambda e, j=j, pb=pb: e.activation(out=x1T32.t[:, j, :], in_=pb.t[:, (j % 4) * 128:(j % 4 + 1) * 128],
                                                                func=AF.Identity, scale=P[:, PP_G1 + j:PP_G1 + j + 1],
                                                                bias=P[:, PP_B1 + j:PP_B1 + j + 1]),
                      reads=[pb, pp], pwrites=[x1T32])
            pC = [bank_c(), bank_c()]
            rs_, xs_ = rstage[c % 2], x1stage[c % 2]
            for hb in range(2):
                tr.mm([lambda e, jj=jj, hb=hb: e.matmul(out=pC[hb].t[:, jj * 128:(jj + 1) * 128], lhsT=x1T32.t[:, hb * 4 + jj, :], rhs=alphaI,
                                                        start=True, stop=True) for jj in range(4)], reads=[x1T32, consts], pwrites=[pC[hb]])
                tr.op("act", lambda e, hb=hb: e.activation(out=rs_.t[:, hb * 512:(hb + 1) * 512], in_=pC[hb].t[:], func=AF.Identity),
                      reads=[pC[hb]], pwrites=[rs_])
                tr.op("act", lambda e, hb=hb: e.activation(out=xs_.t[:, hb * 512:(hb + 1) * 512], in_=pC[hb].t[:], func=AF.Identity,
                                                           scale=1.0 / ALPHA),
                      reads=[pC[hb]], pwrites=[xs_])
            tr.dma("sp", res_d[gtok:gtok + 128, :], rs_.t[:], reads=[rs_], nowait=[res_b])
            tr.dma("sp", x1s_d[gtok:gtok + 128, :], xs_.t[:], reads=[xs_], nowait=[x1s_b])
            tr.mm([lambda e, j=j: e.matmul(out=pS.t[:, 32:52], lhsT=x1T32.t[:, j, :], rhs=wr.t[:, j, :], start=(j == 0), stop=(j == 7))
                   for j in range(8)], reads=[x1T32, wr], pwrites=[pS])
            V = lambda en, fn, r, w: tr.op(en, fn, reads=r, writes=w)
            V("dve", lambda e: e.tensor_tensor(out=lg.ap, in0=pS.t[:, 32:52], in1=RW[:, RP_BR:RP_BR + 20], op=ALU.add), [pS, rowp], [lg])
            V("dve", lambda e: e.tensor_reduce(out=gmax.ap, in_=lg.ap[:, 0:4], axis=AX.X, op=ALU.max), [lg], [gmax])
            V("dve", lambda e: e.tensor_scalar(out=ngmax.ap, in0=gmax.ap, scalar1=-1.0, scalar2=None, op0=ALU.mult), [gmax], [ngmax])
            V("act", lambda e: e.activation(out=gexp.ap, in_=lg.ap[:, 0:4], func=AF.Exp, bias=ngmax.ap, scale=1.0, accum_out=gsum.ap),
              [lg, ngmax], [gexp, gsum])
            V("dve", lambda e: e.reciprocal(out=gw.ap, in_=gsum.ap), [gsum], [gw])
            V("dve", lambda e: e.tensor_scalar(out=gmask.ap, in0=lg.ap[:, 0:4], scalar1=gmax.ap, scalar2=None, op0=ALU.is_equal), [lg, gmax], [gmask])
            el = lg.ap[:, 4:20].rearrange("p (g i) -> p g i", g=4)
            t44v = t44.ap.rearrange("p (g i) -> p g i", g=4)
            V("dve", lambda e: e.tensor_tensor(out=t44v, in0=el, in1=gmask.ap.unsqueeze(2).to_broadcast([128, 4, 4]), op=ALU.mult),
              [lg, gmask], [t44])
            V("dve", lambda e: e.tensor_reduce(out=esel.ap, in_=t44.ap.rearrange("p (g i) -> p i g", g=4), axis=AX.X, op=ALU.add), [t44], [esel])
            V("dve", lambda e: e.tensor_reduce(out=m1.ap, in_=esel.ap, axis=AX.X, op=ALU.max), [esel], [m1])
            V("dve", lambda e: e.tensor_scalar(out=mask1.ap, in0=esel.ap, scalar1=m1.ap, scalar2=None, op0=ALU.is_equal), [esel, m1], [mask1])
            V("dve", lambda e: e.scalar_tensor_tensor(out=esel2.ap, in0=mask1.ap, scalar=-1e30, in1=esel.ap, op0=ALU.mult, op1=ALU.add),
              [mask1, esel], [esel2])
            V("dve", lambda e: e.tensor_reduce(out=m2.ap, in_=esel2.ap, axis=AX.X, op=ALU.max), [esel2], [m2])
            V("dve", lambda e: e.tensor_scalar(out=mask2.ap, in0=esel2.ap, scalar1=m2.ap, scalar2=None, op0=ALU.is_equal), [esel2, m2], [mask2])
            V("dve", lambda e: e.tensor_tensor(out=d21.ap, in0=m2.ap, in1=m1.ap, op=ALU.subtract), [m1, m2], [d21])
            V("act", lambda e: e.activation(out=e21.ap, in_=d21.ap, func=AF.Exp), [d21], [e21])
            V("dve", lambda e: e.tensor_scalar(out=den.ap, in0=e21.ap, scalar1=1.0, scalar2=None, op0=ALU.add), [e21], [den])
            V("dve", lambda e: e.reciprocal(out=p1.ap, in_=den.ap), [den], [p1])
            V("dve", lambda e: e.tensor_tensor(out=c1.ap, in0=p1.ap, in1=gw.ap, op=ALU.mult), [p1, gw], [c1])
            V("dve", lambda e: e.tensor_tensor(out=c2.ap, in0=c1.ap, in1=e21.ap, op=ALU.mult), [c1, e21], [c2])
            V("dve", lambda e: e.tensor_scalar(out=wsel.ap, in0=mask1.ap, scalar1=c1.ap, scalar2=None, op0=ALU.mult), [mask1, c1], [wsel])
            V("dve", lambda e: e.scalar_tensor_tensor(out=wsel.ap, in0=mask2.ap, scalar=c2.ap, in1=wsel.ap, op0=ALU.mult, op1=ALU.add),
              [mask2, c2], [wsel])
            V("dve", lambda e: e.tensor_tensor(out=dwall_t.t[:, gc, :].rearrange("p (g i) -> p g i", g=4),
                                               in0=gmask.ap.unsqueeze(2).to_broadcast([128, 4, 4]),
                                               in1=wsel.ap.unsqueeze(1).to_broadcast([128, 4, 4]), op=ALU.mult), [gmask, wsel], [dwall[gc]])
            V("dve", lambda e: e.tensor_scalar(out=ind.ap, in0=dwall_t.t[:, gc, :], scalar1=0.0, scalar2=None, op0=ALU.is_gt), [dwall[gc]], [ind])
            tr.mm([lambda e: e.matmul(out=pS.t[:, 64:80], lhsT=Ustrict, rhs=ind.ap, start=True, stop=True),
                   lambda e: e.matmul(out=pS.t[:, 80:96], lhsT=ones, rhs=ind.ap, start=True, stop=True)],
                  reads=[ind, consts], pwrites=[pS])
            V("dve", lambda e: e.tensor_tensor(out=posall_t.t[:, gc, :], in0=pS.t[:, 64:80], in1=run.t[:], op=ALU.add), [pS, run], [posall[gc]])
            V("dve", lambda e: e.tensor_tensor(out=run.t[:], in0=pS.t[:, 80:96], in1=run.t[:], op=ALU.add), [pS], [run])

        w_src = (w_gate_d, w_up_d, w_down_d)
        pend_store = []

        def conv_store():
            while pend_store:
                i = pend_store.pop(0)
                mi, ex = i % 3, i // 3
                stg = xgT[i % 2]
                sv = stg.t[:].rearrange("p j n -> p (j n)").rearrange("p (h n) -> p h n", h=2)
                tr.dma("sp", wsc_d[ex * 128:(ex + 1) * 128, mi * 4096:(mi + 1) * 4096], stg.t[:].rearrange("p j n -> p (j n)"),
                       reads=[stg], nowait=[wsc_b], semb=cvs[i % 2])

        def conv_load(i):
            mi, ex = i % 3, i // 3
            stg = xgT[i % 2]
            sv = stg.t[:].rearrange("p j n -> p (j n)").rearrange("p (h n) -> p h n", h=2)
            tr.dma("pool", sv, w_src[mi][ex * 256:(ex + 1) * 256, :].rearrange("(p h) n -> p h n", h=2), pwrites=[stg])
            pend_store.append(i)

        def routing_tables():
            V = lambda en, fn, r, w: tr.op(en, fn, reads=r, writes=w)
            V("dve", lambda e: e.tensor_scalar(out=nt.ap, in0=run.t[:], scalar1=0.0, scalar2=None, op0=ALU.is_gt), [run], [nt])
            for k in range(1, (2 * TOK_PER_CORE // 2) // TS):
                V("dve", lambda e, k=k: e.scalar_tensor_tensor(out=nt.ap, in0=run.t[:], scalar=float(TS * k), in1=nt.ap, op0=ALU.is_gt, op1=ALU.add),
                  [run], [nt])
            V("dve", lambda e: e.tensor_tensor_scan(out=incl.ap, data0=ones16.ap, data1=nt.ap, initial=0.0, op0=ALU.mult, op1=ALU.add),
              [ones16, nt], [incl])
            V("dve", lambda e: e.tensor_tensor(out=base.ap, in0=incl.ap, in1=nt.ap, op=ALU.subtract), [incl, nt], [base])
            V("dve", lambda e: e.tensor_scalar(out=base.ap, in0=base.ap, scalar1=float(TS), scalar2=None, op0=ALU.mult), [], [base])
            cmp_ap = X2.t[:, 0:NTILE * 16].rearrange("p (s e) -> p s e", s=NTILE)
            V("dve", lambda e: e.tensor_tensor(out=cmp_ap, in0=incl.ap.unsqueeze(1).to_broadcast([128, NTILE, 16]),
                                               in1=sconst.unsqueeze(2).to_broadcast([128, NTILE, 16]), op=ALU.is_le), [incl, consts], [X2])
            V("dve", lambda e: e.tensor_reduce(out=teb.t[:], in_=cmp_ap, axis=AX.X, op=ALU.add), [X2], [teb])
            V("dve", lambda e: e.tensor_scalar(out=teb.t[:], in0=teb.t[:], scalar1=15.0, scalar2=None, op0=ALU.min), [], [teb])
            wf = X2.t[:, NTILE * 16:NTILE * 17]
            V("dve", lambda e: e.tensor_scalar(out=wf, in0=teb.t[:], scalar1=128.0, scalar2=iota_p, op0=ALU.mult, op1=ALU.add),
              [teb, consts], [X2])
            V("dve", lambda e: e.tensor_copy(out=widx.t[:], in_=wf), [X2], [widx])
            A3 = lambda ap, off: ap[:, off:off + NCH * 16].rearrange("p (c e) -> p c e", c=NCH)
            indA = A3(xh32.t[:], 0)
            tA = A3(xh32.t[:], 512)
            slotA = A3(X1.t[:], 0)
            selA = [A3(X1.t[:], 512), A3(xT32s[0].t[:].rearrange("p j t -> p (j t)"), 0)]
            V("dve", lambda e: e.tensor_scalar(out=indA, in0=dwall_t.t[:], scalar1=0.0, scalar2=None, op0=ALU.is_gt), dwall, [xh32])
            V("dve", lambda e: e.tensor_tensor(out=slotA, in0=posall_t.t[:], in1=base.ap.unsqueeze(1).to_broadcast([128, NCH, 16]), op=ALU.add),
              posall + [base], [X1])
            V("dve", lambda e: e.tensor_tensor(out=tA, in0=indA, in1=rev_e.unsqueeze(1).to_broadcast([128, NCH, 16]), op=ALU.mult), [consts], [xh32])
            mxA = smallsB.t[:, 0:NCH]
            V("dve", lambda e: e.tensor_reduce(out=mxA, in_=tA, axis=AX.X, op=ALU.max), [xh32], [smallsB])
            tr.op("dve", lambda e: e.tensor_tensor(out=selA[0], in0=tA, in1=mxA.unsqueeze(2).to_broadcast([128, NCH, 16]), op=ALU.is_equal),
                  reads=[xh32, smallsB], pwrites=[X1])
            tr.op("dve", lambda e: e.tensor_tensor(out=selA[1], in0=indA, in1=selA[0], op=ALU.subtract), reads=[xh32, X1], writes=[xT32s[0]])
            tokA = smallsB.t[:, 32:64]
            tr.op("dve", lambda e: e.scalar_tensor_tensor(out=tokA, in0=cconst, scalar=128.0, in1=iota_p.to_broadcast([128, NCH]), op0=ALU.mult, op1=ALU.add),
                  reads=[consts], pwrites=[smallsB])
            payA = payall.t[:]
            payAf = payA.bitcast(F32)
            for k in range(2):
                skA = smallsB.t[:, 64 + k * 32:96 + k * 32]
                tr.op("dve", lambda e, k=k: e.tensor_tensor(out=tA, in0=selA[k], in1=slotA, op=ALU.mult), reads=[X1, xT32s[0]], writes=[xh32])
                tr.op("dve", lambda e, k=k, skA=skA: e.tensor_reduce(out=skA, in_=tA, axis=AX.X, op=ALU.add), reads=[xh32], pwrites=[smallsB])
                tr.op("dve", lambda e, k=k, skA=skA: e.tensor_copy(out=slotiA.t[:, :, k], in_=skA), reads=[smallsB], pwrites=[slotiA])
                tr.op("dve", lambda e, k=k: e.tensor_tensor(out=tA, in0=selA[k], in1=dwall_t.t[:], op=ALU.mult), reads=[X1, xT32s[0]] + dwall, writes=[xh32])
                tr.op("dve", lambda e, k=k: e.tensor_reduce(out=payAf[:, :, k, 1], in_=tA, axis=AX.X, op=ALU.add), reads=[xh32], pwrites=[payall])
                tr.op("dve", lambda e, k=k: e.tensor_copy(out=payA[:, :, k, 0], in_=tokA), reads=[smallsB], pwrites=[payall])
                dkA = smallsB.t[:, 128 + k * 32:160 + k * 32]
                tr.op("dve", lambda e, k=k, dkA=dkA: e.tensor_scalar(out=dkA, in0=tokA, scalar1=float(k * TOK_PER_CORE), scalar2=None, op0=ALU.add),
                      reads=[], pwrites=[smallsB])
                tr.op("dve", lambda e, k=k, dkA=dkA: e.tensor_copy(out=payA[:, :, k, 2], in_=dkA), reads=[smallsB], pwrites=[payall])
            for gc in range(NCH):
                for k in range(2):
                    tr.dma("pool", None, None, reads=[payall, slotiA, sinfo_pre], nowait=[sinfo_b],
                           fn=lambda e, k=k, gc=gc: e.indirect_dma_start(out=sinfo_d[:, :],
                                                                         out_offset=bass.IndirectOffsetOnAxis(ap=slotiA.t[:, gc, k:k + 1], axis=0),
                                                                         in_=payall.t[:, gc, k, :], in_offset=None, bounds_check=reg_ns, oob_is_err=False))

        slots = [R0, R1]
        hTs = [X1, X2]
        sgs = [szc, xs_tok]

        def tile_fetch(s_):
            slot = slots[s_ % 2]
            si_, xg_ = sit[s_ % 2], xg[s_ % 2]
            tr.dma("sp", si_.t[:], sinfo_d[s_ * TS:(s_ + 1) * TS, :].rearrange("(q p) f -> p q f", p=128), reads=[sinfo_b], pwrites=[si_])
            for q in range(TQ):
                tr.dma("pool", None, None, reads=[si_, x1s_b], pwrites=[xg_] + XG_ALIAS[s_ % 2],
                       fn=lambda e, q=q: e.indirect_dma_start(out=xg_.t[:, q, :], out_offset=None, in_=x1s_d[:, :],
                                                              in_offset=bass.IndirectOffsetOnAxis(ap=si_.t[:, q, 0:1], axis=0),
                                                              bounds_check=reg_nx, oob_is_err=False))
            tr.dma("pool", None, None, reads=[widx, wsc_b], pwrites=[slot],
                   fn=lambda e: e.indirect_dma_start(out=slot.t[:, 0:12288], out_offset=None, in_=wsc_d[:, :],
                                                     in_offset=bass.IndirectOffsetOnAxis(ap=widx.t[:, s_:s_ + 1], axis=0),
                                                     bounds_check=reg_nw, oob_is_err=False))

        def tile_compute(s_):
            slot = slots[s_ % 2]
            si_, xg_, xgT_ = sit[s_ % 2], xg[s_ % 2], xgT[s_ % 2]
            sif = si_.t[:].bitcast(F32)
            wg = slot.t[:, 0:4096].rearrange("p (j n) -> p j n", j=8)
            wu = slot.t[:, 4096:8192].rearrange("p (j n) -> p j n", j=8)
            wd = slot.t[:, 8192:12288].rearrange("p (f n) -> p f n", f=4)
            for q in range(TQ):
                pT_ = bank()
                pTb = pT_.t[:].bitcast(BF16)
                tr.mm([lambda e, j=j, q=q: e.transpose(out=pTb[:, j * 128:(j + 1) * 128], in_=xg_.t[:, q, j * 128:(j + 1) * 128], identity=identb.t[:])
                       for j in range(8)], reads=[xg_, identb], pwrites=[pT_])
                tr.op("act", lambda e, q=q: e.activation(out=xgT_.t[:, :, q * 128:(q + 1) * 128], in_=pTb.rearrange("p (j t) -> p j t", j=8),
                                                         func=AF.Identity), reads=[pT_], pwrites=[xgT_])
            hT = hTs[s_ % 2]
            hv = hT.t[:].bitcast(BF16).rearrange("p (f n) -> p f n", f=4)
            for f in range(4):
                sg = sgs[f % 2]
                if TS <= 256:
                    pG = bank()
                    gv, uv = pG.t[:, 0:TS], pG.t[:, TS:2 * TS]
                    pU2 = pG
                else:
                    pG, pU2 = bank(), bank()
                    gv, uv = pG.t[:], pU2.t[:]
                tr.mm([lambda e, j=j, f=f: e.matmul(out=gv, lhsT=wg[:, j, f * 128:(f + 1) * 128], rhs=xgT_.t[:, j, 0:TS],
                                                    start=(j == 0), stop=(j == 7)) for j in range(8)], reads=[slot, xgT_], pwrites=[pG])
                tr.mm([lambda e, j=j, f=f: e.matmul(out=uv, lhsT=wu[:, j, f * 128:(f + 1) * 128], rhs=xgT_.t[:, j, 0:TS],
                                                    start=(j == 0), stop=(j == 7)) for j in range(8)], reads=[slot, xgT_], pwrites=[pU2])
                tr.op("act", lambda e: e.activation(out=sg.t[:, 0:TS], in_=gv, func=AF.Silu), reads=[pG], writes=[sg])
                tr.op("dve", lambda e, f=f: e.tensor_tensor(out=hv[:, f, 0:TS], in0=uv, in1=sg.t[:, 0:TS], op=ALU.mult),
                      reads=[pU2, sg], pwrites=[hT] + (cacc if hT is X1 else []))
            for q in range(TQ):
                yo = io[q % 2]
                for hb in range(2):
                    pO = bank()
                    tr.mm([lambda e, f=f, q=q, hb=hb: e.matmul(out=pO.t[:], lhsT=hv[:, f, q * 128:(q + 1) * 128], rhs=wd[:, f, hb * 512:(hb + 1) * 512],
                                                               start=(f == 0), stop=(f == 3)) for f in range(4)], reads=[hT, slot], pwrites=[pO])
                    tr.op("dve", lambda e, q=q, hb=hb: e.tensor_scalar(out=yo.t[:, hb * 512:(hb + 1) * 512], in0=pO.t[:], scalar1=sif[:, q, 1:2],
                                                                       scalar2=None, op0=ALU.mult), reads=[pO, si_], pwrites=[yo])
                tr.dma("pool", None, None, reads=[yo, si_], nowait=[ybuf_b], semb=ysem[q % 2],
                       fn=lambda e, q=q: e.indirect_dma_start(out=ybuf_d[:, :], out_offset=bass.IndirectOffsetOnAxis(ap=si_.t[:, q, 2:3], axis=0),
                                                              in_=yo.t[:], in_offset=None, bounds_check=reg_ny, oob_is_err=False))

        io4 = [io[0], io[1], rstage[0], rstage[1]]
        yb4 = [xg[0], xg[1], xgT[0], xgT[1]]

        def yview(b):
            ap = b.t[:]
            ap = ap.rearrange("p q d -> p (q d)")
            return ap.bitcast(F32).rearrange("p (k d) -> p k d", k=2)

        def ln2_chunk(gc):
            gtok = gc * 128
            ot, yb = io4[gc % 4], yb4[gc % 4]
            yv = yview(yb)
            st_, mv_, lnv_, rs_, nmr_ = stats4[gc % 4], mv4[gc % 4], lnv4[gc % 4], rs4[gc % 4], nmr4[gc % 4]
            tr.dma("sp", ot.t[:], res_d[gtok:gtok + 128, :], reads=[res_b], pwrites=[ot])
            tr.dma("act", yv, ybuf_d.rearrange("(k t) d -> t k d", k=2)[gtok:gtok + 128, :, :], reads=[ybuf_b], pwrites=[yb],
                   semb=l2s[gc % 4])
            tr.op("pool", lambda e: e.tensor_tensor(out=ot.t[:], in0=ot.t[:], in1=yv[:, 0, :], op=ALU.add), reads=[yb], writes=[ot])
            tr.op("dve", lambda e: e.tensor_tensor(out=ot.t[:], in0=ot.t[:], in1=yv[:, 1, :], op=ALU.add), reads=[yb], writes=[ot])
            for hb in range(2):
                tr.op("dve", lambda e, hb=hb: e.bn_stats(out=st_.ap[:, hb * 6:hb * 6 + 6], in_=ot.t[:, hb * 512:(hb + 1) * 512]),
                      reads=[ot], pwrites=[st_])
            tr.op("dve", lambda e: e.bn_aggr(out=mv_.ap, in_=st_.ap), reads=[st_], writes=[mv_])
            rstd_from(mv_.ap[:, 1:2], [mv_], 1.0, LN_EPS, lnv_, rs_)
            tr.op("dve", lambda e: e.scalar_tensor_tensor(out=nmr_.ap, in0=mv_.ap[:, 0:1], scalar=-1.0, in1=rs_.ap, op0=ALU.mult, op1=ALU.mult),
                  reads=[mv_, rs_], writes=[nmr_])
            tr.op("act", lambda e: e.activation(out=ot.t[:], in_=ot.t[:], func=AF.Identity, scale=rs_.ap, bias=nmr_.ap),
                  reads=[rs_, nmr_], writes=[ot])
            tr.op("dve", lambda e: e.tensor_tensor(out=ot.t[:], in0=ot.t[:], in1=ln2b.t[:, 0, :], op=ALU.mult), reads=[ln2b], writes=[ot])
            tr.op("pool", lambda e: e.tensor_tensor(out=ot.t[:], in0=ot.t[:], in1=ln2b.t[:, 1, :], op=ALU.add), reads=[ln2b], writes=[ot])
            tr.dma("act", yc[gtok:gtok + 128, :], ot.t[:], reads=[ot], is_out=True)

        load_mixer_weights()

        def interleave(*lists):
            lists = [l for l in lists if l]
            idx = [0] * len(lists)
            while True:
                best, bt = None, None
                for k, l in enumerate(lists):
                    if idx[k] < len(l):
                        t = tr.est_start(l[idx[k]]) if SCHED else idx[k] / len(l)
                        if bt is None or t < bt - 1e-9:
                            best, bt = k, t
                if best is None:
                    break
                tr.emit(lists[best][idx[best]])
                idx[best] += 1

        def rec(fn, i):
            if i >= len(chunks):
                return []
            tr.record()
            fn(*chunks[i])
            return tr.stop()

        chunks = [(u, c) for u in range(4) for c in range(8)]
        NCK = len(chunks)
        cstate = {"ci": 0}

        def conv_step(i):
            conv_store()
            if cstate["ci"] < 3 * NE:
                conv_load(cstate["ci"])
                cstate["ci"] += 1
            if i % 2 == 1 and cstate["ci"] < 3 * NE:
                conv_store()
                conv_load(cstate["ci"])
                cstate["ci"] += 1

        chunk_A(*chunks[0])
        done = {"A": 0, "B": -1, "C": -1}
        cur = {"A": None, "B": None, "C": None}
        pos = {"A": 0, "B": 0, "C": 0}
        fns = {"A": chunk_A, "B": chunk_B, "C": chunk_C}

        def eligible(stg):
            k = done[stg] + 1
            if k >= NCK:
                return False
            if stg == "A":
                return done["C"] >= k - 3 and done["B"] >= k - 2
            if stg == "B":
                return done["A"] >= k and done["C"] >= k - 2
            return done["B"] >= k

        while True:
            for stg in ("C", "B", "A"):
                if cur[stg] is None and eligible(stg):
                    k = done[stg] + 1
                    if stg == "A":
                        conv_step(k - 2 if k >= 2 else 0)
                    tr.record()
                    fns[stg](*chunks[k])
                    cur[stg] = tr.stop()
                    pos[stg] = 0
            best, bt = None, None
            for stg in ("C", "B", "A"):
                if cur[stg] is not None:
                    t = tr.est_start(cur[stg][pos[stg]])
                    if bt is None or t < bt - 1e-9:
                        best, bt = stg, t
            if best is None:
                break
            tr.emit(cur[best][pos[best]])
            pos[best] += 1
            if pos[best] >= len(cur[best]):
                done[best] += 1
                cur[best] = None
        while cstate["ci"] < 3 * NE:
            conv_store()
            conv_load(cstate["ci"])
            cstate["ci"] += 1
        ci = cstate["ci"]
        conv_store()
        assert ci == 3 * NE
        if stop_after >= 2:
            routing_tables()
        if stop_after >= 3:
            for i_ in range(2):
                tr.op("dve", lambda e, i_=i_: e.memset(xg[i_].t[:], 0.0), writes=[xg[i_]], pwrites=XG_ALIAS[i_])
            tile_fetch(0)
            for s_ in range(NTILE):
                if s_ + 1 < NTILE:
                    tile_fetch(s_ + 1)
                tile_compute(s_)
        if stop_after >= 4:
            for g0 in range(0, NCH, 4):
                ls = []
                for gc in range(g0, g0 + 4):
                    tr.record()
                    ln2_chunk(gc)
                    ls.append(tr.stop())
                interleave(*ls)
        if debug:
            dbg_d = dt_("dbg", [128, 512], F32, "ExternalOutput")
            dbgs = sb([128, 512], F32, dma=True)
            tr.op("dve", lambda e: e.memset(dbgs.t[:], 0.0), writes=[dbgs])
            tr.op("dve", lambda e: e.tensor_copy(out=dbgs.t[:, 0:16], in_=run.t[:]), reads=[run], pwrites=[dbgs])
            if stop_after >= 2:
                tr.op("dve", lambda e: e.tensor_copy(out=dbgs.t[:, 16:32], in_=nt.ap), reads=[nt], pwrites=[dbgs])
                tr.op("dve", lambda e: e.tensor_copy(out=dbgs.t[:, 32:48], in_=base.ap), reads=[base], pwrites=[dbgs])
                tr.op("dve", lambda e: e.tensor_copy(out=dbgs.t[:, 48:48 + NTILE], in_=teb.t[:]), reads=[teb], pwrites=[dbgs])
                tr.op("dve", lambda e: e.tensor_copy(out=dbgs.t[:, 128:128 + NTILE], in_=widx.t[:]), reads=[widx], pwrites=[dbgs])
            tr.dma("sp", dbg_d[:, :], dbgs.t[:], reads=[dbgs], is_out=True)
            e_ = tr.E["sp"]
            for b_ in (x1s_b, res_b, sinfo_b, ybuf_b):
                tr._wait(e_, b_.w)

        e = tr.E["sp"]
        tr._wait(e, tr.out_toks)
    return nc


def _host_consts():
    c = np.zeros((128, NCONST), np.float32)
    idx = np.arange(128)
    c[:, C_ID:C_ID + 128] = np.eye(128)
    c[:, C_U:C_U + 128] = (idx[:, None] <= idx[None, :])
    c[:, C_LS:C_LS + 128] = (idx[:, None] > idx[None, :])
    c[:, C_MASK:C_MASK + 128] = (idx[None, :] >= idx[:, None])
    c[:, C_AI:C_AI + 128] = np.eye(128) * np.float32(ALPHA)
    c[:, C_ONES:C_ONES + 128] = 1.0
    for g, w in enumerate(WINDOWS):
        tp = idx[:, None]
        t = idx[None, :]
        cur = ((tp <= t) & (tp > t - w)).astype(np.float64) / w - np.eye(128)
        prev = ((tp - 128) > (t - w)).astype(np.float64) / w
        cntf = np.minimum(t + 1, w).astype(np.float64)
        first = ((tp <= t) & (tp > t - w)).astype(np.float64) / cntf - np.eye(128)
        for kind, m in enumerate((cur, prev, first)):
            o = C_BAND + (kind * 4 + g) * 128
            c[:, o:o + 128] = m.astype(np.float32)
    c[:, C_MISC] = idx
    c[:, C_MISC + 1] = 2 * idx
    c[:, C_MISC + 2] = 2 * idx + 1
    c[:, C_MISC + 3:C_MISC + 35] = np.arange(32)[None, :]
    c[:, C_MISC + 35:C_MISC + 51] = (16 - np.arange(16))[None, :]
    c[:, C_US:C_US + 128] = (idx[:, None] < idx[None, :])
    c[:, C_S:C_S + 64] = np.arange(64)[None, :]
    return c


_NC_CACHE = {}


def _prep_inputs(inp):
    f = lambda a: np.ascontiguousarray(np.asarray(a, dtype=np.float32))
    pp = np.zeros((128, NPP), np.float32)
    pp[:, PP_G0:PP_G0 + 8] = f(inp["ln0_g"]).reshape(8, 128).T
    pp[:, PP_B0:PP_B0 + 8] = f(inp["ln0_b"]).reshape(8, 128).T
    pp[:, PP_G1:PP_G1 + 8] = f(inp["ln1_g"])[0].reshape(8, 128).T
    pp[:, PP_B1:PP_B1 + 8] = f(inp["ln1_b"])[0].reshape(8, 128).T
    cw = f(inp["conv_w"])[0]
    pp[:, PP_CW:PP_CW + 32] = cw.reshape(4, 8, 128).transpose(2, 1, 0).reshape(128, 32)
    pp[:, PP_CB:PP_CB + 8] = f(inp["conv_b"])[0].reshape(8, 128).T
    pp[:, PP_PS:PP_PS + 4] = f(inp["pool_scale"])[0].reshape(4, 128).T
    pp[:, PP_PB:PP_PB + 4] = f(inp["b_pool"])[0].reshape(4, 128).T
    rowp = np.concatenate([f(inp["dt_bias"])[0], f(inp["a_log"])[0], f(inp["d_skip"])[0],
                           f(inp["b_router_group"])[0], f(inp["b_router_expert"])[0], f(inp["ssm_norm_g"])[0]]).astype(np.float32)
    assert rowp.shape[0] == NRP
    ln2rows = np.stack([f(inp["ln2_g"])[0], f(inp["ln2_b"])[0]])
    wr = np.ascontiguousarray(np.concatenate([f(inp["w_router_group"])[0], f(inp["w_router_expert"])[0]], axis=1))
    shared = {
        "consts": _host_consts(), "pp": pp, "rowp": rowp, "ln2rows": ln2rows, "wr": wr,
        "identb": np.eye(128, dtype=np.float32).astype(ml_dtypes.bfloat16),
        "w_in": f(inp["w_in"])[0], "w_out": f(inp["w_out"])[0], "w_pool": f(inp["w_pool"])[0],
        "w_gate": np.ascontiguousarray(f(inp["w_gate"])[0].reshape(NE, 8, 128, 512).transpose(0, 2, 1, 3)).reshape(NE * 256, 2048),
        "w_up": np.ascontiguousarray(f(inp["w_up"])[0].reshape(NE, 8, 128, 512).transpose(0, 2, 1, 3)).reshape(NE * 256, 2048),
        "w_down": np.ascontiguousarray(f(inp["w_down"])[0].reshape(NE, 4, 128, 1024).transpose(0, 2, 1, 3)).reshape(NE * 256, 2048),
    }
    return shared


def kernel(**inputs):
    x = np.asarray(inputs["x"], dtype=np.float32)
    shared = _prep_inputs(inputs)
    if "nc" not in _NC_CACHE:
        _NC_CACHE["nc"] = build(4)
    nc = _NC_CACHE["nc"]
    in_maps = []
    for i in range(NCORES):
        m = dict(shared)
        m["xc"] = np.ascontiguousarray(x[2 * i:2 * i + 2].reshape(TOK_PER_CORE, D))
        in_maps.append(m)
    res = run_bass_kernel_spmd(nc, in_maps, core_ids=list(range(NCORES)))
    out = np.empty((16, 2048, D), np.float32)
    for i in range(NCORES):
        out[2 * i:2 * i + 2] = np.asarray(res.results[i]["yc"]).reshape(2, 2048, D)
    return out
```

```python
import contextlib
import math

import numpy as np
import ml_dtypes

import concourse.bass as bass
import concourse.mybir as mybir
from concourse.bass_utils import run_bass_kernel_spmd

F32 = mybir.dt.float32
BF16 = mybir.dt.bfloat16
I32 = mybir.dt.int32
AF = mybir.ActivationFunctionType
ALU = mybir.AluOpType
AX = mybir.AxisListType

D = 1024
NCORES = 8
TOK_PER_CORE = 4096
UNIT = 1024
LN_EPS = 1e-5
RMS_EPS = 1e-5
ALPHA = 2.0 ** 0.25
NE = 16
WINDOWS = (2, 4, 8, 16)

C_ID, C_U, C_LS, C_MASK, C_AI, C_ONES, C_BAND = 0, 128, 256, 384, 512, 640, 768
C_MISC = 768 + 12 * 128
C_US = C_MISC + 64
C_S = C_US + 128
NCONST = C_S + 64
NCH = 32
TQ = 4
TS = 128 * TQ
NTILE = NE + (2 * TOK_PER_CORE) // TS
NSLOT = NTILE * TS
PP_G0, PP_B0, PP_G1, PP_B1, PP_CW, PP_CB, PP_PS, PP_PB = 0, 8, 16, 24, 32, 64, 72, 76
NPP = 80
RP_DTB, RP_ALOG, RP_DSKIP, RP_BR, RP_NG = 0, 8, 16, 24, 44
NRP = 556


class Buf:
    def __init__(self, t, dsem=None):
        self.t = t
        self.w = {}
        self.r = {}
        self.dsem = dsem
        self.dcount = 0


class Eng:
    def __init__(self, name, eng, sem):
        self.name, self.eng, self.sem = name, eng, sem
        self.count = 0
        self.waited = {}
        self.free = 0.0


class _FakeEng:
    def __getattr__(self, name):
        def f(*a, **k):
            k["_op"] = name
            k["_args"] = a
            return k
        return f


def _free_size(ap):
    try:
        n = 1
        for d in ap.shape[1:]:
            n *= int(d)
        return n
    except Exception:
        return 256


def _est_cost(kind, en, fn):
    fe = _FakeEng()
    try:
        if kind == "mm":
            tot = 0.0
            for f in fn:
                k = f(fe)
                n = _free_size(k.get("out"))
                src = k.get("lhsT", k.get("in_"))
                f32 = getattr(src, "dtype", None) == F32
                if k["_op"] == "transpose":
                    tot += 0.45 if f32 else 0.11
                elif f32:
                    tot += max(0.2, n / 600.0)
                else:
                    tot += max(0.06, n / 2400.0 + 0.01)
            return tot
        if kind == "dma":
            return 0.15 if en in ("sp", "act") else 1.2
        k = fn(fe)
        out = k.get("out", k.get("ap", k["_args"][0] if k["_args"] else None))
        n = _free_size(out)
        if en == "act":
            return 0.27 + n * 0.00065
        if en == "dve":
            return 0.1 + n / 960.0
        if en == "pool":
            return 0.3 + n * 0.002
    except Exception:
        pass
    return 0.5


SCHED = True
HOP_PE = 12.0
HOP = 3.0
DMA_LAT = 3.0


class _Rec:
    __slots__ = ("kind", "en", "fn", "reads", "writes", "pwrites", "nowait", "extra", "cost")


class Tracker:
    def __init__(self, nc, st):
        self.nc = nc
        self.st = st
        self.E = {}
        for name, eng in (("pe", nc.tensor), ("act", nc.scalar), ("dve", nc.vector),
                          ("pool", nc.gpsimd), ("sp", nc.sync)):
            self.E[name] = Eng(name, eng, st.enter_context(nc.semaphore("sem_" + name)))
        self.out_toks = {}
        self.rec = None
        self.fin = {}

    def record(self):
        self.rec = []

    def stop(self):
        r, self.rec = self.rec, None
        return r

    def _wait(self, e, toks, skip=None):
        for k, (sem, val) in toks.items():
            if k == skip:
                continue
            if e.waited.get(k, 0) >= val:
                continue
            e.eng.wait_ge(sem, val)
            e.waited[k] = val

    def _pre(self, e, reads, writes, pwrites, skip_self=False):
        for b in reads:
            self._wait(e, b.w)
        for b in writes:
            self._wait(e, b.w)
            self._wait(e, b.r)
        for b in pwrites:
            self._wait(e, b.w, skip=id(e.sem))
            self._wait(e, b.r)

    def _post(self, tok, reads, writes, pwrites):
        k = id(tok[0])
        for b in reads:
            b.r[k] = tok
        for b in writes:
            b.w = {k: tok}
            b.r = {}
        for b in pwrites:
            b.w[k] = tok

    def op(self, en, fn, reads=(), writes=(), pwrites=()):
        if self.rec is not None:
            r = _Rec(); r.kind, r.en, r.fn, r.reads, r.writes, r.pwrites, r.nowait, r.extra = "op", en, fn, reads, writes, pwrites, (), None
            r.cost = _est_cost("op", en, fn)
            self.rec.append(r)
            return None
        return self._op(en, fn, reads, writes, pwrites)

    def _op(self, en, fn, reads=(), writes=(), pwrites=()):
        e = self.E[en]
        self._pre(e, reads, writes, pwrites)
        inst = fn(e.eng)
        e.count += 1
        inst.then_inc(e.sem, 1)
        tok = (e.sem, e.count)
        self._post(tok, reads, writes, pwrites)
        return tok

    def mm(self, fns, reads=(), pwrites=()):
        if self.rec is not None:
            r = _Rec(); r.kind, r.en, r.fn, r.reads, r.writes, r.pwrites, r.nowait, r.extra = "mm", "pe", fns, reads, (), pwrites, (), None
            r.cost = _est_cost("mm", "pe", fns)
            self.rec.append(r)
            return None
        return self._mm(fns, reads, pwrites)

    def _mm(self, fns, reads=(), pwrites=()):
        e = self.E["pe"]
        self._pre(e, reads, (), pwrites)
        inst = None
        for fn in fns:
            inst = fn(e.eng)
        e.count += 1
        inst.then_inc(e.sem, 1)
        tok = (e.sem, e.count)
        self._post(tok, reads, (), pwrites)
        return tok

    def dma(self, qn, out, in_, reads=(), pwrites=(), nowait=(), semb=None, is_out=False, fn=None):
        if self.rec is not None:
            r = _Rec(); r.kind, r.en, r.fn, r.reads, r.writes, r.pwrites, r.nowait = "dma", qn, fn, reads, (), pwrites, nowait
            r.extra = (out, in_, semb, is_out)
            r.cost = _est_cost("dma", qn, fn)
            self.rec.append(r)
            return None
        return self._dma(qn, out, in_, reads, pwrites, nowait, semb, is_out, fn)

    def _ready(self, en, reads, writes, pwrites):
        t = 0.0
        own = id(self.E[en].sem)
        hop = HOP_PE if en == "pe" else HOP
        for b in reads:
            for k, (sem, val) in b.w.items():
                t = max(t, self.fin.get((k, val), 0.0) + (0.0 if k == own else hop))
        for b in list(writes) + list(pwrites):
            for d in (b.w, b.r):
                for k, (sem, val) in d.items():
                    t = max(t, self.fin.get((k, val), 0.0) + (0.0 if k == own else hop))
        return t

    def est_start(self, r):
        return max(self.E[r.en].free, self._ready(r.en, r.reads, r.writes, r.pwrites))

    def emit(self, r):
        e = self.E[r.en]
        start = self.est_start(r)
        if r.kind == "op":
            tok = self._op(r.en, r.fn, r.reads, r.writes, r.pwrites)
        elif r.kind == "mm":
            tok = self._mm(r.fn, r.reads, r.pwrites)
        else:
            out, in_, semb, is_out = r.extra
            tok = self._dma(r.en, out, in_, r.reads, r.pwrites, r.nowait, semb, is_out, r.fn)
        e.free = start + r.cost
        fin = e.free + (DMA_LAT if r.kind == "dma" else 0.0)
        self.fin[(id(tok[0]), tok[1])] = fin

    def _dma(self, qn, out, in_, reads=(), pwrites=(), nowait=(), semb=None, is_out=False, fn=None):
        e = self.E[qn]
        if semb is not None:
            sb = semb
        elif nowait:
            sb = next((b for b in reads if b.dsem is not None), nowait[0])
        elif pwrites:
            sb = pwrites[0]
        else:
            sb = reads[0]
        for b in reads:
            self._wait(e, b.w)
        for b in pwrites:
            self._wait(e, b.w, skip=id(sb.dsem))
            self._wait(e, b.r)
        inst = fn(e.eng) if fn is not None else e.eng.dma_start(out=out, in_=in_)
        sb.dcount += 16
        inst.then_inc(sb.dsem, 16)
        tok = (sb.dsem, sb.dcount)
        self._post(tok, reads, (), list(pwrites) + list(nowait))
        if is_out:
            self.out_toks[id(sb.dsem)] = tok
        return tok


def build(n_units=4, stop_after=4, debug=False):
    nc = bass.Bass("TRN2", target_bir_lowering=False)
    ntok = n_units * UNIT
    dt_ = lambda name, shape, dt=F32, kind="ExternalInput": nc.dram_tensor(name, shape, dt, kind=kind).ap()
    xc = dt_("xc", [TOK_PER_CORE, D])
    consts_d = dt_("consts", [128, NCONST])
    pp_d = dt_("pp", [128, NPP])
    rowp_d = dt_("rowp", [NRP])
    ln2_d = dt_("ln2rows", [2, D])
    wr_d = dt_("wr", [D, 20])
    identb_d = dt_("identb", [128, 128], BF16)
    w_in_d = dt_("w_in", [D, 2056])
    w_out_d = dt_("w_out", [D, D])
    w_pool_d = dt_("w_pool", [4, 128, 128])
    w_gate_d = dt_("w_gate", [NE * 256, 2048])
    w_up_d = dt_("w_up", [NE * 256, 2048])
    w_down_d = dt_("w_down", [NE * 256, 2048])
    yc = dt_("yc", [TOK_PER_CORE, D], F32, "ExternalOutput")
    SK = "ExternalOutput" if debug else "Internal"
    x1s_d = dt_("x1s", [TOK_PER_CORE + 1, D], BF16, SK)
    res_d = dt_("res", [TOK_PER_CORE, D], F32, SK)
    sinfo_d = dt_("sinfo", [NSLOT, 4], I32, SK)
    ybuf_d = dt_("ybuf", [2 * TOK_PER_CORE, D], F32, SK)
    wsc_d = dt_("wsc", [NE * 128, 12288], BF16, "Internal")

    with contextlib.ExitStack() as st:
        tr = Tracker(nc, st)
        cnt = [0]

        def sb(shape, dt=F32, dma=False):
            cnt[0] += 1
            t = st.enter_context(nc.sbuf_tensor("sb%d" % cnt[0], shape, dt))
            ds = st.enter_context(nc.semaphore("ds%d" % cnt[0])) if dma else None
            return Buf(t, ds)

        def view(b, ap):
            return ap

        consts = sb([128, NCONST], F32, dma=True)
        pp = sb([128, NPP], F32, dma=True)
        rowp = sb([128, NRP], F32, dma=True)
        ln2b = sb([128, 2, D], F32, dma=True)
        wr = sb([128, 8, 20], F32, dma=True)
        wpool = sb([128, 4, 128], BF16, dma=True)
        identb = sb([128, 128], BF16, dma=True)
        R0 = sb([128, 8 * 2056], BF16, dma=True)
        R1 = sb([128, 12288], BF16, dma=True)
        dwall_t = sb([128, NCH, 16], F32)
        dwall = [Buf(dwall_t.t) for _ in range(NCH)]
        posall_t = sb([128, NCH, 16], F32)
        posall = [Buf(posall_t.t) for _ in range(NCH)]
        run = sb([128, 16], F32)
        xg = [sb([128, 4, D], BF16, dma=True) for _ in range(2)]
        xgT = [sb([128, 8, 512], BF16, dma=True) for _ in range(2)]
        sit = [sb([128, TQ, 4], I32, dma=True) for _ in range(2)]
        rstage = [sb([128, D], F32, dma=True) for _ in range(2)]
        x1stage = [sb([128, D], BF16, dma=True) for _ in range(2)]
        payall = sb([128, NCH, 2, 4], I32, dma=True)
        slotiA = sb([128, NCH, 2], I32)
        smallsB = sb([128, 192], F32)
        widx = sb([128, NTILE], I32)
        teb = sb([128, NTILE], F32)
        x1s_b = Buf(None, st.enter_context(nc.semaphore("dsx1s")))
        res_b = Buf(None, st.enter_context(nc.semaphore("dsres")))
        sinfo_b = Buf(None, st.enter_context(nc.semaphore("dssinfo")))
        sinfo_pre = Buf(None, st.enter_context(nc.semaphore("dssinfopre")))
        ybuf_b = Buf(None, st.enter_context(nc.semaphore("dsybuf")))
        wsc_b = Buf(None, st.enter_context(nc.semaphore("dswsc")))
        cvs = [Buf(None, st.enter_context(nc.semaphore("dscv%d" % i))) for i in range(2)]
        ysem = [Buf(None, st.enter_context(nc.semaphore("dsys%d" % i))) for i in range(2)]
        l2s = [Buf(None, st.enter_context(nc.semaphore("dsl2%d" % i))) for i in range(4)]
        S = sb([128, 512], F32)
        Sb = sb([128, 512], BF16)
        utok = [sb([128, 512], F32) for _ in range(2)]
        ub = [sb([128, 8, 131], F32) for _ in range(2)]
        io = [sb([128, D], F32, dma=True) for _ in range(2)]
        xh32 = sb([128, D], F32)
        xh32c = sb([128, D], F32)
        xT32s = [sb([128, 8, 128], F32) for _ in range(2)]
        x1T32 = sb([128, 8, 128], F32)
        xnT = sb([128, 8, 128], BF16)
        X1 = sb([128, 1024], F32)
        cacc = [Buf(X1.t) for _ in range(8)]
        X2 = sb([128, 1024], F32)
        xsT32 = sb([128, 4, 128], F32)
        BT32 = sb([128, 2, 128], F32)
        BTb = sb([128, 2, 128], BF16)
        CTb = sb([128, 2, 128], BF16)
        szc = sb([128, 512], F32)
        MT = sb([128, 8, 128], BF16)
        scm = sb([128, 2, 128], F32)
        xs_tok = sb([128, 512], F32)
        xdt = sb([128, 512], BF16)
        xdec = sb([128, 512], BF16)
        Btok = sb([128, 256], BF16)
        ybuf = sb([128, 512], F32)
        ynb = sb([128, 512], BF16)
        ycatTs = [sb([128, 8, 128], BF16) for _ in range(2)]
        pT = sb([128, 4, 128], BF16)
        smalls_t = sb([128, 512], F32)
        sm_off = [0]

        def small(n):
            o = sm_off[0]
            sm_off[0] += n
            assert sm_off[0] <= 512
            b = Buf(smalls_t.t)
            b.ap = smalls_t.t[:, o:o + n]
            return b

        stats = small(12); mv = small(2); lnv = small(1); rs = small(1)
        statsC = small(12); mvC = small(2); lnvC = small(1); rsC = small(1)
        stats4 = [small(12) for _ in range(4)]; mv4 = [small(2) for _ in range(4)]; lnv4 = [small(1) for _ in range(4)]
        rs4 = [small(1) for _ in range(4)]; nmr4 = [small(1) for _ in range(4)]
        ahead = small(8); psb = small(4)
        xdtb = small(8); axb = small(8); e1 = small(8); l1 = small(8); dtv = small(8); av = small(8)
        acs = small(8); eac = small(8); dd = small(8); dsv = small(8); dtds = small(8); cdv = small(8)
        ssq = small(2); lns = small(2); rs2 = small(2)
        lg = small(20); gmax = small(1); ngmax = small(1); gexp = small(4); gsum = small(1); gw = small(1)
        gmask = small(4); t44 = small(16); esel = small(4); m1 = small(1); mask1 = small(4); esel2 = small(4)
        m2 = small(1); mask2 = small(4); d21 = small(1); e21 = small(1); den = small(1); p1 = small(1)
        c1 = small(1); c2 = small(1); wsel = small(4)
        dtvs = [dtv, small(8)]; avs = [av, small(8)]
        ind = small(16); nt = small(16); incl = small(16); base = small(16); ones16 = small(16)
        slotf = small(16); cin = small(16); cex = small(16); sel = [small(16), small(16)]; tmp16 = [small(16), small(16)]
        slk = small(2); wk = small(2); tokf = small(1); dstf = small(2)

        class _V:
            pass

        def alias(ap):
            b = Buf(None)
            v = _V()
            v.ap = ap
            b.t = _T(ap)
            return b

        class _T:
            def __init__(self, ap):
                self._ap = ap

            def __getitem__(self, key):
                return self._ap[key] if key != slice(None) else self._ap

        xg0f = xg[0].t[:].rearrange("p q d -> p (q d)").bitcast(F32)
        xg1f = xg[1].t[:].rearrange("p q d -> p (q d)").bitcast(F32)
        xsT32s = [xsT32, alias(xg0f[:, 0:512].rearrange("p (m t) -> p m t", m=4))]
        BT32s = [BT32, alias(xg0f[:, 512:768].rearrange("p (g t) -> p g t", g=2))]
        szcs = [szc, alias(xg0f[:, 768:1280])]
        utok3 = [utok[0], utok[1], alias(xg0f[:, 1280:1792])]
        BTbs = [BTb, alias(xg0f[:, 1792:1920].bitcast(BF16).rearrange("p (g t) -> p g t", g=2))]
        CTbs = [CTb, alias(xg0f[:, 1920:2048].bitcast(BF16).rearrange("p (g t) -> p g t", g=2))]
        xT32s.append(alias(xg1f[:, 0:1024].rearrange("p (j t) -> p j t", j=8)))
        XG_ALIAS = [[xsT32s[1], BT32s[1], szcs[1], utok3[2], BTbs[1], CTbs[1]], [xT32s[2]]]
        banks = []
        for i in range(8):
            t = st.enter_context(nc.psum_tensor("ps%d" % i, [128, 512], F32))
            banks.append(Buf(t))
        bank_i = [0]

        def bank():
            b = banks[bank_i[0] % 7]
            bank_i[0] += 1
            return b

        pool_a = [0]
        pool_b = [0]
        pool_c = [0]

        def bank_a():
            b = banks[pool_a[0] % 2]
            pool_a[0] += 1
            return b

        def bank_b():
            b = banks[2 + pool_b[0] % 3]
            pool_b[0] += 1
            return b

        def bank_c():
            b = banks[5 + pool_c[0] % 2]
            pool_c[0] += 1
            return b

        C = consts.t
        ident32 = C[:, C_ID:C_ID + 128]
        Umat = C[:, C_U:C_U + 128]
        Lsmat = C[:, C_LS:C_LS + 128]
        mask01 = C[:, C_MASK:C_MASK + 128]
        alphaI = C[:, C_AI:C_AI + 128]
        ones = C[:, C_ONES:C_ONES + 128]
        iota_p = C[:, C_MISC:C_MISC + 1]
        c2ph = C[:, C_MISC + 1:C_MISC + 3]
        sconst = C[:, C_S:C_S + NTILE]
        cconst = C[:, C_S:C_S + NCH]
        Ustrict = C[:, C_US:C_US + 128]
        rev_e = C[:, C_MISC + 35:C_MISC + 51]

        def band(kind, g):
            o = C_BAND + (kind * 4 + g) * 128
            return C[:, o:o + 128]

        P = pp.t
        RW = rowp.t

        tr.dma("sp", consts.t[:], consts_d[:, :], pwrites=[consts])
        tr.dma("sp", pp.t[:], pp_d[:, :], pwrites=[pp])
        tr.dma("sp", rowp.t[:], rowp_d.partition_broadcast(128), pwrites=[rowp])
        for i in range(2):
            tr.dma("sp", ln2b.t[:, i, :], ln2_d[i, :].partition_broadcast(128), pwrites=[ln2b])
        tr.dma("sp", wr.t[:], wr_d.rearrange("(j p) n -> p j n", p=128), pwrites=[wr])
        tr.dma("sp", identb.t[:], identb_d[:, :], pwrites=[identb])
        tr.dma("pool", wpool.t[:], w_pool_d.rearrange("g c d -> c g d"), pwrites=[wpool])
        tr.op("act", lambda e: e.activation(out=ahead.ap, in_=RW[:, RP_ALOG:RP_ALOG + 8], func=AF.Exp),
              reads=[rowp], writes=[ahead])
        tr.op("dve", lambda e: e.tensor_scalar(out=ahead.ap, in0=ahead.ap, scalar1=-1.0, scalar2=None, op0=ALU.mult),
              reads=[], writes=[ahead])
        tr.op("dve", lambda e: e.tensor_tensor(out=psb.ap, in0=P[:, PP_PB:PP_PB + 4], in1=P[:, PP_PS:PP_PS + 4], op=ALU.mult),
              reads=[pp], writes=[psb])

        tr.op("pool", lambda e: e.memset(run.t[:], 0.0), writes=[run])
        tr.op("pool", lambda e: e.memset(ones16.ap, 1.0), writes=[ones16])
        zrow_ap = io[1].t[0:1, 0:512].bitcast(BF16)
        tr.op("pool", lambda e: e.memset(io[1].t[0:1, 0:512], 0.0), writes=[io[1]])
        tr.dma("sp", x1s_d[TOK_PER_CORE:TOK_PER_CORE + 1, :], zrow_ap, reads=[io[1]], nowait=[x1s_b])
        padt_ap = io[0].t[:, 0:NSLOT // 32].bitcast(I32).rearrange("p (a f) -> p a f", f=4)
        tr.op("pool", lambda e: e.memset(padt_ap[:, :, 0:1], 1000000), pwrites=[io[0]])
        tr.op("pool", lambda e: e.memset(padt_ap[:, :, 1:2], 0), pwrites=[io[0]])
        tr.op("pool", lambda e: e.memset(padt_ap[:, :, 2:3], 1000000), pwrites=[io[0]])
        tr.op("pool", lambda e: e.memset(padt_ap[:, :, 3:4], 0), pwrites=[io[0]])
        tr.dma("sp", sinfo_d.rearrange("(p a) f -> p a f", p=128), padt_ap, reads=[io[0]], pwrites=[sinfo_pre])
        tr.op("pool", lambda e: e.memset(payall.t[:], 0), writes=[payall])

        reg_ns = nc.gpsimd.alloc_register("bc_nslot")
        nc.gpsimd.reg_mov(reg_ns, NSLOT - 1)
        reg_nx = nc.gpsimd.alloc_register("bc_nx")
        nc.gpsimd.reg_mov(reg_nx, TOK_PER_CORE)
        reg_nw = nc.gpsimd.alloc_register("bc_nw")
        nc.gpsimd.reg_mov(reg_nw, NE * 128 - 1)
        reg_ny = nc.gpsimd.alloc_register("bc_ny")
        nc.gpsimd.reg_mov(reg_ny, 2 * TOK_PER_CORE - 1)

        def rstd_from(var_ap, var_bufs, scale, eps, lnbuf, outbuf):
            tr.op("act", lambda e: e.activation(out=lnbuf.ap, in_=var_ap, func=AF.Ln, bias=eps_ap(eps), scale=scale),
                  reads=var_bufs + [epsb], writes=[lnbuf])
            tr.op("act", lambda e: e.activation(out=outbuf.ap, in_=lnbuf.ap, func=AF.Exp, scale=-0.5),
                  reads=[lnbuf], writes=[outbuf])

        epsb = small(2)
        tr.op("pool", lambda e: e.memset(epsb.ap[:, 0:1], LN_EPS), writes=[epsb])
        one_b = small(1)
        tr.op("pool", lambda e: e.memset(one_b.ap, 1.0), writes=[one_b])

        def eps_ap(eps):
            return epsb.ap[:, 0:1]

        def load_mixer_weights():
            w3 = R0.t[:, 0:8 * 2056].rearrange("p (j n) -> p j n", j=8)
            src = w_in_d.rearrange("(j p) n -> p j n", p=128)
            tr.dma("pool", w3[:, :, 0:1024], src[:, :, 0:1024], pwrites=[R0])
            tr.dma("pool", w3[:, :, 1024:2056], src[:, :, 1024:2056], pwrites=[R0])
            wo3 = R1.t[:, 0:8192].rearrange("p (j n) -> p j n", j=8)
            tr.dma("pool", wo3, w_out_d.rearrange("(j p) n -> p j n", p=128), pwrites=[R1])

        win = R0.t[:, 0:8 * 2056].rearrange("p (j n) -> p j n", j=8)
        wout = R1.t[:, 0:8192].rearrange("p (j n) -> p j n", j=8)

        def chunk_A(u, c):
            gtok = u * UNIT + c * 128
            gc = u * 8 + c
            cs = (gtok % 2048) // 128
            first = cs == 0
            xT32_ = xT32s[gc % 3]
            xt = io[c % 2]
            ubc, ubp = ub[c % 2], ub[(c + 1) % 2]
            utc = utok3[gc % 3]
            sl = gc % 2
            xsT32_, BT32_, BTb_, CTb_, szc_, dtv_, av_ = xsT32s[sl], BT32s[sl], BTbs[sl], CTbs[sl], szcs[sl], dtvs[sl], avs[sl]
            pS = banks[7]
            if gc == 0:
                tr.dma("sp", xt.t[:], xc[gtok:gtok + 128, :], pwrites=[xt])
            if gc + 1 < NCH:
                xtn = io[(c + 1) % 2]
                tr.dma("sp", xtn.t[:], xc[gtok + 128:gtok + 256, :], pwrites=[xtn])
            for hlf in range(2):
                tr.op("dve", lambda e, hlf=hlf: e.bn_stats(out=stats.ap[:, hlf * 6:hlf * 6 + 6], in_=xt.t[:, hlf * 512:(hlf + 1) * 512]),
                      reads=[xt], pwrites=[stats])
            tr.op("dve", lambda e: e.bn_aggr(out=mv.ap, in_=stats.ap), reads=[stats], writes=[mv])
            rstd_from(mv.ap[:, 1:2], [mv], 1.0, LN_EPS, lnv, rs)
            tr.op("dve", lambda e: e.tensor_scalar(out=xh32.t[:], in0=xt.t[:], scalar1=mv.ap[:, 0:1], scalar2=rs.ap,
                                                   op0=ALU.subtract, op1=ALU.mult),
                  reads=[xt, mv, rs], writes=[xh32])
            pA, pB = bank_a(), bank_a()
            for hb, pb in enumerate((pA, pB)):
                tr.mm([lambda e, j=j, pb=pb: e.transpose(out=pb.t[:, (j % 4) * 128:(j % 4 + 1) * 128],
                                                         in_=xh32.t[:, j * 128:(j + 1) * 128], identity=ident32)
                       for j in range(hb * 4, hb * 4 + 4)], reads=[xh32, consts], pwrites=[pb])
            for j in range(8):
                pb = pA if j < 4 else pB
                tr.op("act", lambda e, j=j, pb=pb: e.activation(out=xT32_.t[:, j, :], in_=pb.t[:, (j % 4) * 128:(j % 4 + 1) * 128],
                                                                func=AF.Identity, scale=P[:, PP_G0 + j:PP_G0 + j + 1],
                                                                bias=P[:, PP_B0 + j:PP_B0 + j + 1]),
                      reads=[pb, pp], pwrites=[xT32_])
            tr.op("dve", lambda e: e.tensor_copy(out=xnT.t[:], in_=xT32_.t[:]), reads=[xT32_], writes=[xnT])
            pX = [bank_a(), bank_a()]
            for hb in range(2):
                fns = []
                for m in range(hb * 4, hb * 4 + 4):
                    for j in range(8):
                        fns.append(lambda e, m=m, j=j, hb=hb: e.matmul(
                            out=pX[hb].t[:, (m % 4) * 128:(m % 4 + 1) * 128],
                            lhsT=win[:, j, 512 + m * 128:512 + (m + 1) * 128], rhs=xnT.t[:, j, :],
                            start=(j == 0), stop=(j == 7)))
                tr.mm(fns, reads=[xnT, R0], pwrites=[pX[hb]])
            for hb in range(2):
                tr.op("act", lambda e, hb=hb: e.activation(out=ubc.t[:, hb * 4:hb * 4 + 4, 3:131],
                                                           in_=pX[hb].t[:].rearrange("p (m t) -> p m t", m=4), func=AF.Identity),
                      reads=[pX[hb]], pwrites=[ubc])
            pZ = bank_a()
            tr.mm([lambda e, j=j: e.matmul(out=pZ.t[:], lhsT=xnT.t[:, j, :], rhs=win[:, j, 0:512], start=(j == 0), stop=(j == 7))
                   for j in range(8)], reads=[xnT, R0], pwrites=[pZ])
            pU = bank_a()
            tr.mm([lambda e, j=j: e.matmul(out=pU.t[:], lhsT=xnT.t[:, j, :], rhs=win[:, j, 1544:2056], start=(j == 0), stop=(j == 7))
                   for j in range(8)], reads=[xnT, R0], pwrites=[pU])
            tr.mm([lambda e, j=j: e.matmul(out=pS.t[:, 0:8], lhsT=xnT.t[:, j, :], rhs=win[:, j, 1536:1544], start=(j == 0), stop=(j == 7))
                   for j in range(8)], reads=[xnT, R0], pwrites=[pS])
            if first:
                tr.op("pool", lambda e: e.memset(ubc.t[:, :, 0:3], 0.0), pwrites=[ubc])
            else:
                tr.op("pool", lambda e: e.tensor_copy(out=ubc.t[:, :, 0:3], in_=ubp.t[:, :, 128:131]), reads=[ubp], pwrites=[ubc])
            cav = X1.t[:].rearrange("p (m t) -> p m t", m=8)
            for m in range(8):
                tr.op("pool", lambda e, m=m: e.tensor_scalar(out=cav[:, m, :], in0=ubc.t[:, m, 0:128],
                                                             scalar1=P[:, PP_CW + m * 4:PP_CW + m * 4 + 1],
                                                             scalar2=P[:, PP_CB + m:PP_CB + m + 1], op0=ALU.mult, op1=ALU.add),
                      reads=[ubc, pp], writes=[cacc[m]], pwrites=[X1])
            for k in range(1, 4):
                for m in range(8):
                    tr.op("dve", lambda e, m=m, k=k: e.scalar_tensor_tensor(
                        out=cav[:, m, :], in0=ubc.t[:, m, k:k + 128], scalar=P[:, PP_CW + m * 4 + k:PP_CW + m * 4 + k + 1],
                        in1=cav[:, m, :], op0=ALU.mult, op1=ALU.add), reads=[ubc, pp], writes=[cacc[m]])
            tr.op("act", lambda e: e.activation(out=xsT32_.t[:], in_=cav[:, 0:4, :], func=AF.Silu), reads=cacc[0:4], writes=[xsT32_])
            tr.op("act", lambda e: e.activation(out=BT32_.t[:], in_=cav[:, 4:6, :], func=AF.Silu), reads=cacc[4:6], writes=[BT32_])
            tr.op("act", lambda e: e.activation(out=CTb_.t[:], in_=cav[:, 6:8, :], func=AF.Silu), reads=cacc[6:8], writes=[CTb_])
            tr.op("act", lambda e: e.activation(out=szc_.t[:], in_=pZ.t[:], func=AF.Silu), reads=[pZ], writes=[szc_])
            tr.op("pool", lambda e: e.tensor_copy(out=BTb_.t[:], in_=BT32_.t[:]), reads=[BT32_], writes=[BTb_])
            tr.op("act", lambda e: e.activation(out=utc.t[:], in_=pU.t[:], func=AF.Identity), reads=[pU], writes=[utc])
            tr.op("dve", lambda e: e.tensor_tensor(out=xdtb.ap, in0=pS.t[:, 0:8], in1=RW[:, RP_DTB:RP_DTB + 8], op=ALU.add),
                  reads=[pS, rowp], writes=[xdtb])
            tr.op("act", lambda e: e.activation(out=axb.ap, in_=xdtb.ap, func=AF.Abs), reads=[xdtb], writes=[axb])
            tr.op("act", lambda e: e.activation(out=e1.ap, in_=axb.ap, func=AF.Exp, scale=-1.0), reads=[axb], writes=[e1])
            tr.op("act", lambda e: e.activation(out=l1.ap, in_=e1.ap, func=AF.Ln, bias=one_b.ap, scale=1.0), reads=[e1, one_b], writes=[l1])
            tr.op("dve", lambda e: e.scalar_tensor_tensor(out=dtv_.ap, in0=xdtb.ap, scalar=0.0, in1=l1.ap, op0=ALU.max, op1=ALU.add),
                  reads=[xdtb, l1], writes=[dtv_])
            tr.op("dve", lambda e: e.tensor_tensor(out=av_.ap, in0=dtv_.ap, in1=ahead.ap, op=ALU.mult), reads=[dtv_, ahead], writes=[av_])
        def chunk_B(u, c):
            gtok = u * UNIT + c * 128
            gc = u * 8 + c
            cs = (gtok % 2048) // 128
            first = cs == 0
            ycatT_ = ycatTs[gc % 2]
            utc, utp = utok3[gc % 3], utok3[(gc + 2) % 3]
            sl = gc % 2
            xsT32_, BT32_, BTb_, CTb_, szc_, dtv_, av_ = xsT32s[sl], BT32s[sl], BTbs[sl], CTbs[sl], szcs[sl], dtvs[sl], avs[sl]
            pS = banks[7]
            if first:
                tr.op("pool", lambda e: e.memset(Sb.t[:], 0.0), writes=[Sb])
            tr.mm([lambda e: e.matmul(out=pS.t[:, 8:16], lhsT=Umat, rhs=av_.ap, start=True, stop=True),
                   lambda e: e.matmul(out=pS.t[:, 16:24], lhsT=ones, rhs=av_.ap, start=True, stop=True)],
                  reads=[av_, consts], pwrites=[pS])
            Rt = X2.t[:].rearrange("p (h l) -> p h l", h=8)
            for h in range(8):
                tr.op("dve", lambda e, h=h: e.tensor_scalar(out=Rt[:, h, :], in0=Umat, scalar1=av_.ap[:, h:h + 1], scalar2=None, op0=ALU.mult),
                      reads=[av_, consts], pwrites=[X2])
            pD = [bank_b(), bank_b()]
            for hb in range(2):
                tr.mm([lambda e, hb=hb: e.matmul(out=pD[hb].t[:], lhsT=Lsmat, rhs=X2.t[:, hb * 512:(hb + 1) * 512], start=True, stop=True)],
                      reads=[X2, consts], pwrites=[pD[hb]])
            for hb in range(2):
                tr.op("act", lambda e, hb=hb: e.activation(out=X2.t[:, hb * 512:(hb + 1) * 512], in_=pD[hb].t[:], func=AF.Exp),
                      reads=[pD[hb]], pwrites=[X2])
            scv = pS.t[:, 256:512].rearrange("p (g l) -> p g l", g=2)
            tr.mm([lambda e, g=g: e.matmul(out=scv[:, g, :], lhsT=BTb_.t[:, g, :], rhs=CTb_.t[:, g, :], start=True, stop=True)
                   for g in range(2)], reads=[BTb_, CTb_], pwrites=[pS])
            tr.op("dve", lambda e: e.tensor_tensor(out=scm.t[:], in0=scv, in1=mask01.unsqueeze(1).to_broadcast([128, 2, 128]), op=ALU.mult),
                  reads=[consts, pS], writes=[scm])
            for g in range(2):
                tr.op("dve", lambda e, g=g: e.tensor_tensor(out=MT.t[:, g * 4:(g + 1) * 4, :], in0=Rt[:, g * 4:(g + 1) * 4, :],
                                                            in1=scm.t[:, g, :].unsqueeze(1).to_broadcast([128, 4, 128]), op=ALU.mult),
                      reads=[X2, scm], pwrites=[MT])
            pXs = bank_b()
            tr.mm([lambda e, m=m: e.transpose(out=pXs.t[:, m * 128:(m + 1) * 128], in_=xsT32_.t[:, m, :], identity=ident32)
                   for m in range(4)], reads=[xsT32_, consts], pwrites=[pXs])
            pBt = bank_b()
            tr.mm([lambda e, g=g: e.transpose(out=pBt.t[:, g * 128:(g + 1) * 128], in_=BT32_.t[:, g, :], identity=ident32)
                   for g in range(2)], reads=[BT32_, consts], pwrites=[pBt])
            tr.op("act", lambda e: e.activation(out=xs_tok.t[:], in_=pXs.t[:], func=AF.Identity), reads=[pXs], writes=[xs_tok])
            tr.op("act", lambda e: e.activation(out=Btok.t[:], in_=pBt.t[:, 0:256], func=AF.Identity), reads=[pBt], writes=[Btok])
            tr.op("act", lambda e: e.activation(out=eac.ap, in_=pS.t[:, 8:16], func=AF.Exp), reads=[pS], writes=[eac])
            tr.op("act", lambda e: e.activation(out=acs.ap, in_=pS.t[:, 8:16], func=AF.Identity), reads=[pS], writes=[acs])
            tr.op("act", lambda e: e.activation(out=cdv.ap, in_=pS.t[:, 16:24], func=AF.Exp), reads=[pS], writes=[cdv])
            tr.op("dve", lambda e: e.tensor_tensor(out=dd.ap, in0=pS.t[:, 16:24], in1=acs.ap, op=ALU.subtract), reads=[pS, acs], writes=[dd])
            tr.op("act", lambda e: e.activation(out=dsv.ap, in_=dd.ap, func=AF.Exp), reads=[dd], writes=[dsv])
            tr.op("dve", lambda e: e.tensor_tensor(out=dtds.ap, in0=dtv_.ap, in1=dsv.ap, op=ALU.mult), reads=[dtv_, dsv], writes=[dtds])
            xs3 = pXs.t[:].rearrange("p (h q) -> p h q", h=8)
            tr.op("dve", lambda e: e.tensor_tensor(out=xdt.t[:].rearrange("p (h q) -> p h q", h=8), in0=xs3,
                                                   in1=dtv_.ap.unsqueeze(2).to_broadcast([128, 8, 64]), op=ALU.mult),
                  reads=[pXs, dtv_], writes=[xdt])
            tr.op("dve", lambda e: e.tensor_tensor(out=xdec.t[:].rearrange("p (h q) -> p h q", h=8), in0=xs3,
                                                   in1=dtds.ap.unsqueeze(2).to_broadcast([128, 8, 64]), op=ALU.mult),
                  reads=[pXs, dtds], writes=[xdec])
            pYd = bank_b()
            tr.mm([lambda e, h=h: e.matmul(out=pYd.t[:, h * 64:(h + 1) * 64], lhsT=MT.t[:, h, :], rhs=xdt.t[:, h * 64:(h + 1) * 64],
                                           start=True, stop=True) for h in range(8)], reads=[MT, xdt], pwrites=[pYd])
            pYo = bank_b()
            tr.mm([lambda e, g=g: e.matmul(out=pYo.t[:, g * 256:(g + 1) * 256], lhsT=CTb_.t[:, g, :], rhs=Sb.t[:, g * 256:(g + 1) * 256],
                                           start=True, stop=True) for g in range(2)], reads=[CTb_, Sb], pwrites=[pYo])
            pSt = bank_b()
            tr.mm([lambda e, g=g: e.matmul(out=pSt.t[:, g * 256:(g + 1) * 256], lhsT=Btok.t[:, g * 128:(g + 1) * 128],
                                           rhs=xdec.t[:, g * 256:(g + 1) * 256], start=True, stop=True) for g in range(2)],
                  reads=[Btok, xdec], pwrites=[pSt])
            y3 = ybuf.t[:].rearrange("p (h q) -> p h q", h=8)
            tr.op("dve", lambda e: e.tensor_tensor(out=y3, in0=pYo.t[:].rearrange("p (h q) -> p h q", h=8),
                                                   in1=eac.ap.unsqueeze(2).to_broadcast([128, 8, 64]), op=ALU.mult),
                  reads=[pYo, eac], writes=[ybuf])
            tr.op("dve", lambda e: e.tensor_tensor(out=ybuf.t[:], in0=pYd.t[:], in1=ybuf.t[:], op=ALU.add), reads=[pYd], writes=[ybuf])
            tr.op("pool", lambda e: e.tensor_tensor(out=xs_tok.t[:].rearrange("p (h q) -> p h q", h=8),
                                                    in0=xs_tok.t[:].rearrange("p (h q) -> p h q", h=8),
                                                    in1=RW[:, RP_DSKIP:RP_DSKIP + 8].unsqueeze(2).to_broadcast([128, 8, 64]), op=ALU.mult),
                  reads=[rowp], writes=[xs_tok])
            tr.op("pool", lambda e: e.tensor_tensor(out=ybuf.t[:], in0=ybuf.t[:], in1=xs_tok.t[:], op=ALU.add), reads=[xs_tok], writes=[ybuf])
            tr.op("pool", lambda e: e.tensor_tensor(out=ybuf.t[:], in0=ybuf.t[:], in1=szc_.t[:], op=ALU.mult), reads=[szc_], writes=[ybuf])
            for g in range(2):
                tr.op("act", lambda e, g=g: e.activation(out=ynb.t[:, g * 256:(g + 1) * 256], in_=ybuf.t[:, g * 256:(g + 1) * 256],
                                                         func=AF.Square, accum_out=ssq.ap[:, g:g + 1]),
                      reads=[ybuf], pwrites=[ynb, ssq])
            tr.op("act", lambda e: e.activation(out=lns.ap, in_=ssq.ap, func=AF.Ln, bias=epsb.ap[:, 0:1], scale=1.0 / 256.0),
                  reads=[ssq, epsb], writes=[lns])
            tr.op("act", lambda e: e.activation(out=rs2.ap, in_=lns.ap, func=AF.Exp, scale=-0.5), reads=[lns], writes=[rs2])
            for g in range(2):
                tr.op("dve", lambda e, g=g: e.scalar_tensor_tensor(out=ynb.t[:, g * 256:(g + 1) * 256], in0=ybuf.t[:, g * 256:(g + 1) * 256],
                                                                   scalar=rs2.ap[:, g:g + 1],
                                                                   in1=RW[:, RP_NG + g * 256:RP_NG + (g + 1) * 256],
                                                                   op0=ALU.mult, op1=ALU.mult),
                      reads=[ybuf, rs2, rowp], pwrites=[ynb])
            pYt = bank_b()
            pYt_b = pYt.t[:].bitcast(BF16)
            tr.mm([lambda e, m=m: e.transpose(out=pYt_b[:, m * 128:(m + 1) * 128], in_=ynb.t[:, m * 128:(m + 1) * 128], identity=identb.t[:])
                   for m in range(4)], reads=[ynb, identb], pwrites=[pYt])
            tr.op("act", lambda e: e.activation(out=ycatT_.t[:, 0:4, :], in_=pYt_b[:, 0:512].rearrange("p (m t) -> p m t", m=4), func=AF.Identity),
                  reads=[pYt], pwrites=[ycatT_])
            if first:
                tr.op("pool", lambda e: e.memset(S.t[:], 0.0), writes=[S])
            else:
                tr.op("pool", lambda e: e.tensor_tensor(out=S.t[:].rearrange("p (h q) -> p h q", h=8),
                                                        in0=S.t[:].rearrange("p (h q) -> p h q", h=8),
                                                        in1=cdv.ap.unsqueeze(2).to_broadcast([128, 8, 64]), op=ALU.mult),
                      reads=[cdv], writes=[S])
            tr.op("dve", lambda e: e.tensor_tensor(out=S.t[:], in0=pSt.t[:], in1=S.t[:], op=ALU.add), reads=[pSt], writes=[S])
            tr.op("act", lambda e: e.activation(out=Sb.t[:], in_=S.t[:], func=AF.Identity), reads=[S], writes=[Sb])
            pP = bank_b()
            fns = []
            for g in range(4):
                fns.append(lambda e, g=g: e.matmul(out=pP.t[:, g * 128:(g + 1) * 128], lhsT=utc.t[:, g * 128:(g + 1) * 128],
                                                   rhs=band(2 if first else 0, g), start=True, stop=first))
                if not first:
                    fns.append(lambda e, g=g: e.matmul(out=pP.t[:, g * 128:(g + 1) * 128], lhsT=utp.t[:, g * 128:(g + 1) * 128],
                                                       rhs=band(1, g), start=False, stop=True))
            tr.mm(fns, reads=[utc, consts] + ([] if first else [utp]), pwrites=[pP])
            tr.op("act", lambda e: e.activation(out=pT.t[:], in_=pP.t[:].rearrange("p (g t) -> p g t", g=4), func=AF.Identity),
                  reads=[pP], writes=[pT])
            pM = bank_b()
            tr.mm([lambda e, g=g: e.matmul(out=pM.t[:, g * 128:(g + 1) * 128], lhsT=wpool.t[:, g, :], rhs=pT.t[:, g, :], start=True, stop=True)
                   for g in range(4)], reads=[wpool, pT], pwrites=[pM])
            for g in range(4):
                tr.op("act", lambda e, g=g: e.activation(out=ycatT_.t[:, 4 + g, :], in_=pM.t[:, g * 128:(g + 1) * 128], func=AF.Identity,
                                                         scale=P[:, PP_PS + g:PP_PS + g + 1], bias=psb.ap[:, g:g + 1]),
                      reads=[pM, pp, psb], pwrites=[ycatT_])
        def chunk_C(u, c):
            gtok = u * UNIT + c * 128
            gc = u * 8 + c
            xT32_ = xT32s[gc % 3]
            ycatT_ = ycatTs[gc % 2]
            pS = banks[7]
            pO = [bank_c(), bank_c()]
            for hb in range(2):
                fns = [lambda e, j=j, hb=hb: e.matmul(out=pO[hb].t[:], lhsT=ycatT_.t[:, j, :], rhs=wout[:, j, hb * 512:(hb + 1) * 512],
                                                      start=(j == 0), stop=(j == 7)) for j in range(8)]
                for jj in range(4):
                    fns.append(lambda e, jj=jj, hb=hb: e.matmul(out=pO[hb].t[:, jj * 128:(jj + 1) * 128], lhsT=xT32_.t[:, hb * 4 + jj, :],
                                                                rhs=alphaI, start=False, stop=True, skip_group_check=True))
                tr.mm(fns, reads=[ycatT_, R1, xT32_, consts], pwrites=[pO[hb]])
            for hb in range(2):
                tr.op("dve", lambda e, hb=hb: e.bn_stats(out=statsC.ap[:, hb * 6:hb * 6 + 6], in_=pO[hb].t[:]), reads=[pO[hb]], pwrites=[statsC])
            tr.op("dve", lambda e: e.bn_aggr(out=mvC.ap, in_=statsC.ap), reads=[statsC], writes=[mvC])
            rstd_from(mvC.ap[:, 1:2], [mvC], 1.0, LN_EPS, lnvC, rsC)
            for hb in range(2):
                tr.op("dve", lambda e, hb=hb: e.tensor_scalar(out=xh32c.t[:, hb * 512:(hb + 1) * 512], in0=pO[hb].t[:], scalar1=mvC.ap[:, 0:1],
                                                              scalar2=rsC.ap, op0=ALU.subtract, op1=ALU.mult),
                      reads=[pO[hb], mvC, rsC], pwrites=[xh32c])
            pA, pB = bank_c(), bank_c()
            for hb, pb in enumerate((pA, pB)):
                tr.mm([lambda e, j=j, pb=pb: e.transpose(out=pb.t[:, (j % 4) * 128:(j % 4 + 1) * 128],
                                                         in_=xh32c.t[:, j * 128:(j + 1) * 128], identity=ident32)
                       for j in range(hb * 4, hb * 4 + 4)], reads=[xh32c, consts], pwrites=[pb])
            for j in range(8):
                pb = pA if j < 4 else pB
                tr.op("act", lambda e, j=j, pb=pb: e.activation(out=x1T32.t[:, j, :], in_=pb.t[:, (j % 4) * 128:(j % 4 + 1) * 128],
                                                                func=AF.Identity, scale=P[:, PP_G1 + j:PP_G1 + j + 1],
                                                                bias=P[:, PP_B1 + j:PP_B1 + j + 1]),
                      reads=[pb, pp], pwrites=[x1T32])
            pC = [bank_c(), bank_c()]
            rs_, xs_ = rstage[c % 2], x1stage[c % 2]
            for hb in range(2):
                tr.mm([lambda e, jj=jj, hb=hb: e.matmul(out=pC[hb].t[:, jj * 128:(jj + 1) * 128], lhsT=x1T32.t[:, hb * 4 + jj, :], rhs=alphaI,
                                                        start=True, stop=True) for jj in range(4)], reads=[x1T32, consts], pwrites=[pC[hb]])
                tr.op("act", lambda e, hb=hb: e.activation(out=rs_.t[:, hb * 512:(hb + 1) * 512], in_=pC[hb].t[:], func=AF.Identity),
                      reads=[pC[hb]], pwrites=[rs_])
                tr.op("act", lambda e, hb=hb: e.activation(out=xs_.t[:, hb * 512:(hb + 1) * 512], in_=pC[hb].t[:], func=AF.Identity,
                                                           scale=1.0 / ALPHA),
                      reads=[pC[hb]], pwrites=[xs_])
            tr.dma("sp", res_d[gtok:gtok + 128, :], rs_.t[:], reads=[rs_], nowait=[res_b])
            tr.dma("sp", x1s_d[gtok:gtok + 128, :], xs_.t[:], reads=[xs_], nowait=[x1s_b])
            tr.mm([lambda e, j=j: e.matmul(out=pS.t[:, 32:52], lhsT=x1T32.t[:, j, :], rhs=wr.t[:, j, :], start=(j == 0), stop=(j == 7))
                   for j in range(8)], reads=[x1T32, wr], pwrites=[pS])
            V = lambda en, fn, r, w: tr.op(en, fn, reads=r, writes=w)
            V("dve", lambda e: e.tensor_tensor(out=lg.ap, in0=pS.t[:, 32:52], in1=RW[:, RP_BR:RP_BR + 20], op=ALU.add), [pS, rowp], [lg])
            V("dve", lambda e: e.tensor_reduce(out=gmax.ap, in_=lg.ap[:, 0:4], axis=AX.X, op=ALU.max), [lg], [gmax])
            V("dve", lambda e: e.tensor_scalar(out=ngmax.ap, in0=gmax.ap, scalar1=-1.0, scalar2=None, op0=ALU.mult), [gmax], [ngmax])
            V("act", lambda e: e.activation(out=gexp.ap, in_=lg.ap[:, 0:4], func=AF.Exp, bias=ngmax.ap, scale=1.0, accum_out=gsum.ap),
              [lg, ngmax], [gexp, gsum])
            V("dve", lambda e: e.reciprocal(out=gw.ap, in_=gsum.ap), [gsum], [gw])
            V("dve", lambda e: e.tensor_scalar(out=gmask.ap, in0=lg.ap[:, 0:4], scalar1=gmax.ap, scalar2=None, op0=ALU.is_equal), [lg, gmax], [gmask])
            el = lg.ap[:, 4:20].rearrange("p (g i) -> p g i", g=4)
            t44v = t44.ap.rearrange("p (g i) -> p g i", g=4)
            V("dve", lambda e: e.tensor_tensor(out=t44v, in0=el, in1=gmask.ap.unsqueeze(2).to_broadcast([128, 4, 4]), op=ALU.mult),
              [lg, gmask], [t44])
            V("dve", lambda e: e.tensor_reduce(out=esel.ap, in_=t44.ap.rearrange("p (g i) -> p i g", g=4), axis=AX.X, op=ALU.add), [t44], [esel])
            V("dve", lambda e: e.tensor_reduce(out=m1.ap, in_=esel.ap, axis=AX.X, op=ALU.max), [esel], [m1])
            V("dve", lambda e: e.tensor_scalar(out=mask1.ap, in0=esel.ap, scalar1=m1.ap, scalar2=None, op0=ALU.is_equal), [esel, m1], [mask1])
            V("dve", lambda e: e.scalar_tensor_tensor(out=esel2.ap, in0=mask1.ap, scalar=-1e30, in1=esel.ap, op0=ALU.mult, op1=ALU.add),
              [mask1, esel], [esel2])
            V("dve", lambda e: e.tensor_reduce(out=m2.ap, in_=esel2.ap, axis=AX.X, op=ALU.max), [esel2], [m2])
            V("dve", lambda e: e.tensor_scalar(out=mask2.ap, in0=esel2.ap, scalar1=m2.ap, scalar2=None, op0=ALU.is_equal), [esel2, m2], [mask2])
            V("dve", lambda e: e.tensor_tensor(out=d21.ap, in0=m2.ap, in1=m1.ap, op=ALU.subtract), [m1, m2], [d21])
            V("act", lambda e: e.activation(out=e21.ap, in_=d21.ap, func=AF.Exp), [d21], [e21])
            V("dve", lambda e: e.tensor_scalar(out=den.ap, in0=e21.ap, scalar1=1.0, scalar2=None, op0=ALU.add), [e21], [den])
            V("dve", lambda e: e.reciprocal(out=p1.ap, in_=den.ap), [den], [p1])
            V("dve", lambda e: e.tensor_tensor(out=c1.ap, in0=p1.ap, in1=gw.ap, op=ALU.mult), [p1, gw], [c1])
            V("dve", lambda e: e.tensor_tensor(out=c2.ap, in0=c1.ap, in1=e21.ap, op=ALU.mult), [c1, e21], [c2])
            V("dve", lambda e: e.tensor_scalar(out=wsel.ap, in0=mask1.ap, scalar1=c1.ap, scalar2=None, op0=ALU.mult), [mask1, c1], [wsel])
            V("dve", lambda e: e.scalar_tensor_tensor(out=wsel.ap, in0=mask2.ap, scalar=c2.ap, in1=wsel.ap, op0=ALU.mult, op1=ALU.add),
              [mask2, c2], [wsel])
            V("dve", lambda e: e.tensor_tensor(out=dwall_t.t[:, gc, :].rearrange("p (g i) -> p g i", g=4),
                                               in0=gmask.ap.unsqueeze(2).to_broadcast([128, 4, 4]),
                                               in1=wsel.ap.unsqueeze(1).to_broadcast([128, 4, 4]), op=ALU.mult), [gmask, wsel], [dwall[gc]])
            V("dve", lambda e: e.tensor_scalar(out=ind.ap, in0=dwall_t.t[:, gc, :], scalar1=0.0, scalar2=None, op0=ALU.is_gt), [dwall[gc]], [ind])
            tr.mm([lambda e: e.matmul(out=pS.t[:, 64:80], lhsT=Ustrict, rhs=ind.ap, start=True, stop=True),
                   lambda e: e.matmul(out=pS.t[:, 80:96], lhsT=ones, rhs=ind.ap, start=True, stop=True)],
                  reads=[ind, consts], pwrites=[pS])
            V("dve", lambda e: e.tensor_tensor(out=posall_t.t[:, gc, :], in0=pS.t[:, 64:80], in1=run.t[:], op=ALU.add), [pS, run], [posall[gc]])
            V("dve", lambda e: e.tensor_tensor(out=run.t[:], in0=pS.t[:, 80:96], in1=run.t[:], op=ALU.add), [pS], [run])

        w_src = (w_gate_d, w_up_d, w_down_d)
        pend_store = []

        def conv_store():
            while pend_store:
                i = pend_store.pop(0)
                mi, ex = i % 3, i // 3
                stg = xgT[i % 2]
                sv = stg.t[:].rearrange("p j n -> p (j n)").rearrange("p (h n) -> p h n", h=2)
                tr.dma("sp", wsc_d[ex * 128:(ex + 1) * 128, mi * 4096:(mi + 1) * 4096], stg.t[:].rearrange("p j n -> p (j n)"),
                       reads=[stg], nowait=[wsc_b], semb=cvs[i % 2])

        def conv_load(i):
            mi, ex = i % 3, i // 3
            stg = xgT[i % 2]
            sv = stg.t[:].rearrange("p j n -> p (j n)").rearrange("p (h n) -> p h n", h=2)
            tr.dma("pool", sv, w_src[mi][ex * 256:(ex + 1) * 256, :].rearrange("(p h) n -> p h n", h=2), pwrites=[stg])
            pend_store.append(i)

        def routing_tables():
            V = lambda en, fn, r, w: tr.op(en, fn, reads=r, writes=w)
            V("dve", lambda e: e.tensor_scalar(out=nt.ap, in0=run.t[:], scalar1=0.0, scalar2=None, op0=ALU.is_gt), [run], [nt])
            for k in range(1, (2 * TOK_PER_CORE // 2) // TS):
                V("dve", lambda e, k=k: e.scalar_tensor_tensor(out=nt.ap, in0=run.t[:], scalar=float(TS * k), in1=nt.ap, op0=ALU.is_gt, op1=ALU.add),
                  [run], [nt])
            V("dve", lambda e: e.tensor_tensor_scan(out=incl.ap, data0=ones16.ap, data1=nt.ap, initial=0.0, op0=ALU.mult, op1=ALU.add),
              [ones16, nt], [incl])
            V("dve", lambda e: e.tensor_tensor(out=base.ap, in0=incl.ap, in1=nt.ap, op=ALU.subtract), [incl, nt], [base])
            V("dve", lambda e: e.tensor_scalar(out=base.ap, in0=base.ap, scalar1=float(TS), scalar2=None, op0=ALU.mult), [], [base])
            cmp_ap = X2.t[:, 0:NTILE * 16].rearrange("p (s e) -> p s e", s=NTILE)
            V("dve", lambda e: e.tensor_tensor(out=cmp_ap, in0=incl.ap.unsqueeze(1).to_broadcast([128, NTILE, 16]),
                                               in1=sconst.unsqueeze(2).to_broadcast([128, NTILE, 16]), op=ALU.is_le), [incl, consts], [X2])
            V("dve", lambda e: e.tensor_reduce(out=teb.t[:], in_=cmp_ap, axis=AX.X, op=ALU.add), [X2], [teb])
            V("dve", lambda e: e.tensor_scalar(out=teb.t[:], in0=teb.t[:], scalar1=15.0, scalar2=None, op0=ALU.min), [], [teb])
            wf = X2.t[:, NTILE * 16:NTILE * 17]
            V("dve", lambda e: e.tensor_scalar(out=wf, in0=teb.t[:], scalar1=128.0, scalar2=iota_p, op0=ALU.mult, op1=ALU.add),
              [teb, consts], [X2])
            V("dve", lambda e: e.tensor_copy(out=widx.t[:], in_=wf), [X2], [widx])
            A3 = lambda ap, off: ap[:, off:off + NCH * 16].rearrange("p (c e) -> p c e", c=NCH)
            indA = A3(xh32.t[:], 0)
            tA = A3(xh32.t[:], 512)
            slotA = A3(X1.t[:], 0)
            selA = [A3(X1.t[:], 512), A3(xT32s[0].t[:].rearrange("p j t -> p (j t)"), 0)]
            V("dve", lambda e: e.tensor_scalar(out=indA, in0=dwall_t.t[:], scalar1=0.0, scalar2=None, op0=ALU.is_gt), dwall, [xh32])
            V("dve", lambda e: e.tensor_tensor(out=slotA, in0=posall_t.t[:], in1=base.ap.unsqueeze(1).to_broadcast([128, NCH, 16]), op=ALU.add),
              posall + [base], [X1])
            V("dve", lambda e: e.tensor_tensor(out=tA, in0=indA, in1=rev_e.unsqueeze(1).to_broadcast([128, NCH, 16]), op=ALU.mult), [consts], [xh32])
            mxA = smallsB.t[:, 0:NCH]
            V("dve", lambda e: e.tensor_reduce(out=mxA, in_=tA, axis=AX.X, op=ALU.max), [xh32], [smallsB])
            tr.op("dve", lambda e: e.tensor_tensor(out=selA[0], in0=tA, in1=mxA.unsqueeze(2).to_broadcast([128, NCH, 16]), op=ALU.is_equal),
                  reads=[xh32, smallsB], pwrites=[X1])
            tr.op("dve", lambda e: e.tensor_tensor(out=selA[1], in0=indA, in1=selA[0], op=ALU.subtract), reads=[xh32, X1], writes=[xT32s[0]])
            tokA = smallsB.t[:, 32:64]
            tr.op("dve", lambda e: e.scalar_tensor_tensor(out=tokA, in0=cconst, scalar=128.0, in1=iota_p.to_broadcast([128, NCH]), op0=ALU.mult, op1=ALU.add),
                  reads=[consts], pwrites=[smallsB])
            payA = payall.t[:]
            payAf = payA.bitcast(F32)
            for k in range(2):
                skA = smallsB.t[:, 64 + k * 32:96 + k * 32]
                tr.op("dve", lambda e, k=k: e.tensor_tensor(out=tA, in0=selA[k], in1=slotA, op=ALU.mult), reads=[X1, xT32s[0]], writes=[xh32])
                tr.op("dve", lambda e, k=k, skA=skA: e.tensor_reduce(out=skA, in_=tA, axis=AX.X, op=ALU.add), reads=[xh32], pwrites=[smallsB])
                tr.op("dve", lambda e, k=k, skA=skA: e.tensor_copy(out=slotiA.t[:, :, k], in_=skA), reads=[smallsB], pwrites=[slotiA])
                tr.op("dve", lambda e, k=k: e.tensor_tensor(out=tA, in0=selA[k], in1=dwall_t.t[:], op=ALU.mult), reads=[X1, xT32s[0]] + dwall, writes=[xh32])
                tr.op("dve", lambda e, k=k: e.tensor_reduce(out=payAf[:, :, k, 1], in_=tA, axis=AX.X, op=ALU.add), reads=[xh32], pwrites=[payall])
                tr.op("dve", lambda e, k=k: e.tensor_copy(out=payA[:, :, k, 0], in_=tokA), reads=[smallsB], pwrites=[payall])
                dkA = smallsB.t[:, 128 + k * 32:160 + k * 32]
                tr.op("dve", lambda e, k=k, dkA=dkA: e.tensor_scalar(out=dkA, in0=tokA, scalar1=float(k * TOK_PER_CORE), scalar2=None, op0=ALU.add),
                      reads=[], pwrites=[smallsB])
                tr.op("dve", lambda e, k=k, dkA=dkA: e.tensor_copy(out=payA[:, :, k, 2], in_=dkA), reads=[smallsB], pwrites=[payall])
            for gc in range(NCH):
                for k in range(2):
                    tr.dma("pool", None, None, reads=[payall, slotiA, sinfo_pre], nowait=[sinfo_b],
                           fn=lambda e, k=k, gc=gc: e.indirect_dma_start(out=sinfo_d[:, :],
                                                                         out_offset=bass.IndirectOffsetOnAxis(ap=slotiA.t[:, gc, k:k + 1], axis=0),
                                                                         in_=payall.t[:, gc, k, :], in_offset=None, bounds_check=reg_ns, oob_is_err=False))

        slots = [R0, R1]
        hTs = [X1, X2]
        sgs = [szc, xs_tok]

        def tile_fetch(s_):
            slot = slots[s_ % 2]
            si_, xg_ = sit[s_ % 2], xg[s_ % 2]
            tr.dma("sp", si_.t[:], sinfo_d[s_ * TS:(s_ + 1) * TS, :].rearrange("(q p) f -> p q f", p=128), reads=[sinfo_b], pwrites=[si_])
            for q in range(TQ):
                tr.dma("pool", None, None, reads=[si_, x1s_b], pwrites=[xg_] + XG_ALIAS[s_ % 2],
                       fn=lambda e, q=q: e.indirect_dma_start(out=xg_.t[:, q, :], out_offset=None, in_=x1s_d[:, :],
                                                              in_offset=bass.IndirectOffsetOnAxis(ap=si_.t[:, q, 0:1], axis=0),
                                                              bounds_check=reg_nx, oob_is_err=False))
            tr.dma("pool", None, None, reads=[widx, wsc_b], pwrites=[slot],
                   fn=lambda e: e.indirect_dma_start(out=slot.t[:, 0:12288], out_offset=None, in_=wsc_d[:, :],
                                                     in_offset=bass.IndirectOffsetOnAxis(ap=widx.t[:, s_:s_ + 1], axis=0),
                                                     bounds_check=reg_nw, oob_is_err=False))

        def tile_compute(s_):
            slot = slots[s_ % 2]
            si_, xg_, xgT_ = sit[s_ % 2], xg[s_ % 2], xgT[s_ % 2]
            sif = si_.t[:].bitcast(F32)
            wg = slot.t[:, 0:4096].rearrange("p (j n) -> p j n", j=8)
            wu = slot.t[:, 4096:8192].rearrange("p (j n) -> p j n", j=8)
            wd = slot.t[:, 8192:12288].rearrange("p (f n) -> p f n", f=4)
            for q in range(TQ):
                pT_ = bank()
                pTb = pT_.t[:].bitcast(BF16)
                tr.mm([lambda e, j=j, q=q: e.transpose(out=pTb[:, j * 128:(j + 1) * 128], in_=xg_.t[:, q, j * 128:(j + 1) * 128], identity=identb.t[:])
                       for j in range(8)], reads=[xg_, identb], pwrites=[pT_])
                tr.op("act", lambda e, q=q: e.activation(out=xgT_.t[:, :, q * 128:(q + 1) * 128], in_=pTb.rearrange("p (j t) -> p j t", j=8),
                                                         func=AF.Identity), reads=[pT_], pwrites=[xgT_])
            hT = hTs[s_ % 2]
            hv = hT.t[:].bitcast(BF16).rearrange("p (f n) -> p f n", f=4)
            for f in range(4):
                sg = sgs[f % 2]
                if TS <= 256:
                    pG = bank()
                    gv, uv = pG.t[:, 0:TS], pG.t[:, TS:2 * TS]
                    pU2 = pG
                else:
                    pG, pU2 = bank(), bank()
                    gv, uv = pG.t[:], pU2.t[:]
                tr.mm([lambda e, j=j, f=f: e.matmul(out=gv, lhsT=wg[:, j, f * 128:(f + 1) * 128], rhs=xgT_.t[:, j, 0:TS],
                                                    start=(j == 0), stop=(j == 7)) for j in range(8)], reads=[slot, xgT_], pwrites=[pG])
                tr.mm([lambda e, j=j, f=f: e.matmul(out=uv, lhsT=wu[:, j, f * 128:(f + 1) * 128], rhs=xgT_.t[:, j, 0:TS],
                                                    start=(j == 0), stop=(j == 7)) for j in range(8)], reads=[slot, xgT_], pwrites=[pU2])
                tr.op("act", lambda e: e.activation(out=sg.t[:, 0:TS], in_=gv, func=AF.Silu), reads=[pG], writes=[sg])
                tr.op("dve", lambda e, f=f: e.tensor_tensor(out=hv[:, f, 0:TS], in0=uv, in1=sg.t[:, 0:TS], op=ALU.mult),
                      reads=[pU2, sg], pwrites=[hT] + (cacc if hT is X1 else []))
            for q in range(TQ):
                yo = io[q % 2]
                for hb in range(2):
                    pO = bank()
                    tr.mm([lambda e, f=f, q=q, hb=hb: e.matmul(out=pO.t[:], lhsT=hv[:, f, q * 128:(q + 1) * 128], rhs=wd[:, f, hb * 512:(hb + 1) * 512],
                                                               start=(f == 0), stop=(f == 3)) for f in range(4)], reads=[hT, slot], pwrites=[pO])
                    tr.op("dve", lambda e, q=q, hb=hb: e.tensor_scalar(out=yo.t[:, hb * 512:(hb + 1) * 512], in0=pO.t[:], scalar1=sif[:, q, 1:2],
                                                                       scalar2=None, op0=ALU.mult), reads=[pO, si_], pwrites=[yo])
                tr.dma("pool", None, None, reads=[yo, si_], nowait=[ybuf_b], semb=ysem[q % 2],
                       fn=lambda e, q=q: e.indirect_dma_start(out=ybuf_d[:, :], out_offset=bass.IndirectOffsetOnAxis(ap=si_.t[:, q, 2:3], axis=0),
                                                              in_=yo.t[:], in_offset=None, bounds_check=reg_ny, oob_is_err=False))

        io4 = [io[0], io[1], rstage[0], rstage[1]]
        yb4 = [xg[0], xg[1], xgT[0], xgT[1]]

        def yview(b):
            ap = b.t[:]
            ap = ap.rearrange("p q d -> p (q d)")
            return ap.bitcast(F32).rearrange("p (k d) -> p k d", k=2)

        def ln2_chunk(gc):
            gtok = gc * 128
            ot, yb = io4[gc % 4], yb4[gc % 4]
            yv = yview(yb)
            st_, mv_, lnv_, rs_, nmr_ = stats4[gc % 4], mv4[gc % 4], lnv4[gc % 4], rs4[gc % 4], nmr4[gc % 4]
            tr.dma("sp", ot.t[:], res_d[gtok:gtok + 128, :], reads=[res_b], pwrites=[ot])
            tr.dma("sp", yv, ybuf_d.rearrange("(k t) d -> t k d", k=2)[gtok:gtok + 128, :, :], reads=[ybuf_b], pwrites=[yb],
                   semb=l2s[gc % 4])
            tr.op("pool", lambda e: e.tensor_tensor(out=ot.t[:], in0=ot.t[:], in1=yv[:, 0, :], op=ALU.add), reads=[yb], writes=[ot])
            tr.op("dve", lambda e: e.tensor_tensor(out=ot.t[:], in0=ot.t[:], in1=yv[:, 1, :], op=ALU.add), reads=[yb], writes=[ot])
            for hb in range(2):
                tr.op("dve", lambda e, hb=hb: e.bn_stats(out=st_.ap[:, hb * 6:hb * 6 + 6], in_=ot.t[:, hb * 512:(hb + 1) * 512]),
                      reads=[ot], pwrites=[st_])
            tr.op("dve", lambda e: e.bn_aggr(out=mv_.ap, in_=st_.ap), reads=[st_], writes=[mv_])
            rstd_from(mv_.ap[:, 1:2], [mv_], 1.0, LN_EPS, lnv_, rs_)
            tr.op("dve", lambda e: e.scalar_tensor_tensor(out=nmr_.ap, in0=mv_.ap[:, 0:1], scalar=-1.0, in1=rs_.ap, op0=ALU.mult, op1=ALU.mult),
                  reads=[mv_, rs_], writes=[nmr_])
            tr.op("act", lambda e: e.activation(out=ot.t[:], in_=ot.t[:], func=AF.Identity, scale=rs_.ap, bias=nmr_.ap),
                  reads=[rs_, nmr_], writes=[ot])
            tr.op("dve", lambda e: e.tensor_tensor(out=ot.t[:], in0=ot.t[:], in1=ln2b.t[:, 0, :], op=ALU.mult), reads=[ln2b], writes=[ot])
            tr.op("pool", lambda e: e.tensor_tensor(out=ot.t[:], in0=ot.t[:], in1=ln2b.t[:, 1, :], op=ALU.add), reads=[ln2b], writes=[ot])
            tr.dma("act", yc[gtok:gtok + 128, :], ot.t[:], reads=[ot], is_out=True)

        load_mixer_weights()

        def interleave(*lists):
            lists = [l for l in lists if l]
            idx = [0] * len(lists)
            while True:
                best, bt = None, None
                for k, l in enumerate(lists):
                    if idx[k] < len(l):
                        t = tr.est_start(l[idx[k]]) if SCHED else idx[k] / len(l)
                        if bt is None or t < bt - 1e-9:
                            best, bt = k, t
                if best is None:
                    break
                tr.emit(lists[best][idx[best]])
                idx[best] += 1

        def rec(fn, i):
            if i >= len(chunks):
                return []
            tr.record()
            fn(*chunks[i])
            return tr.stop()

        chunks = [(u, c) for u in range(4) for c in range(8)]
        NCK = len(chunks)
        cstate = {"ci": 0}

        def conv_step(i):
            conv_store()
            if cstate["ci"] < 3 * NE:
                conv_load(cstate["ci"])
                cstate["ci"] += 1
            if i % 2 == 1 and cstate["ci"] < 3 * NE:
                conv_store()
                conv_load(cstate["ci"])
                cstate["ci"] += 1

        chunk_A(*chunks[0])
        done = {"A": 0, "B": -1, "C": -1}
        cur = {"A": None, "B": None, "C": None}
        pos = {"A": 0, "B": 0, "C": 0}
        fns = {"A": chunk_A, "B": chunk_B, "C": chunk_C}

        def eligible(stg):
            k = done[stg] + 1
            if k >= NCK:
                return False
            if stg == "A":
                return done["C"] >= k - 3 and done["B"] >= k - 2
            if stg == "B":
                return done["A"] >= k and done["C"] >= k - 2
            return done["B"] >= k

        while True:
            for stg in ("C", "B", "A"):
                if cur[stg] is None and eligible(stg):
                    k = done[stg] + 1
                    if stg == "A":
                        conv_step(k - 2 if k >= 2 else 0)
                    tr.record()
                    fns[stg](*chunks[k])
                    cur[stg] = tr.stop()
                    pos[stg] = 0
            best, bt = None, None
            for stg in ("C", "B", "A"):
                if cur[stg] is not None:
                    t = tr.est_start(cur[stg][pos[stg]])
                    if bt is None or t < bt - 1e-9:
                        best, bt = stg, t
            if best is None:
                break
            tr.emit(cur[best][pos[best]])
            pos[best] += 1
            if pos[best] >= len(cur[best]):
                done[best] += 1
                cur[best] = None
        while cstate["ci"] < 3 * NE:
            conv_store()
            conv_load(cstate["ci"])
            cstate["ci"] += 1
        ci = cstate["ci"]
        conv_store()
        assert ci == 3 * NE
        if stop_after >= 2:
            routing_tables()
        if stop_after >= 3:
            for i_ in range(2):
                tr.op("dve", lambda e, i_=i_: e.memset(xg[i_].t[:], 0.0), writes=[xg[i_]], pwrites=XG_ALIAS[i_])
            tile_fetch(0)
            for s_ in range(NTILE):
                if s_ + 1 < NTILE:
                    tile_fetch(s_ + 1)
                tile_compute(s_)
        if stop_after >= 4:
            for g0 in range(0, NCH, 4):
                ls = []
                for gc in range(g0, g0 + 4):
                    tr.record()
                    ln2_chunk(gc)
                    ls.append(tr.stop())
                interleave(*ls)
        if debug:
            dbg_d = dt_("dbg", [128, 512], F32, "ExternalOutput")
            dbgs = sb([128, 512], F32, dma=True)
            tr.op("dve", lambda e: e.memset(dbgs.t[:], 0.0), writes=[dbgs])
            tr.op("dve", lambda e: e.tensor_copy(out=dbgs.t[:, 0:16], in_=run.t[:]), reads=[run], pwrites=[dbgs])
            if stop_after >= 2:
                tr.op("dve", lambda e: e.tensor_copy(out=dbgs.t[:, 16:32], in_=nt.ap), reads=[nt], pwrites=[dbgs])
                tr.op("dve", lambda e: e.tensor_copy(out=dbgs.t[:, 32:48], in_=base.ap), reads=[base], pwrites=[dbgs])
                tr.op("dve", lambda e: e.tensor_copy(out=dbgs.t[:, 48:48 + NTILE], in_=teb.t[:]), reads=[teb], pwrites=[dbgs])
                tr.op("dve", lambda e: e.tensor_copy(out=dbgs.t[:, 128:128 + NTILE], in_=widx.t[:]), reads=[widx], pwrites=[dbgs])
            tr.dma("sp", dbg_d[:, :], dbgs.t[:], reads=[dbgs], is_out=True)
            e_ = tr.E["sp"]
            for b_ in (x1s_b, res_b, sinfo_b, ybuf_b):
                tr._wait(e_, b_.w)

        e = tr.E["sp"]
        tr._wait(e, tr.out_toks)
    return nc


def _host_consts():
    c = np.zeros((128, NCONST), np.float32)
    idx = np.arange(128)
    c[:, C_ID:C_ID + 128] = np.eye(128)
    c[:, C_U:C_U + 128] = (idx[:, None] <= idx[None, :])
    c[:, C_LS:C_LS + 128] = (idx[:, None] > idx[None, :])
    c[:, C_MASK:C_MASK + 128] = (idx[None, :] >= idx[:, None])
    c[:, C_AI:C_AI + 128] = np.eye(128) * np.float32(ALPHA)
    c[:, C_ONES:C_ONES + 128] = 1.0
    for g, w in enumerate(WINDOWS):
        tp = idx[:, None]
        t = idx[None, :]
        cur = ((tp <= t) & (tp > t - w)).astype(np.float64) / w - np.eye(128)
        prev = ((tp - 128) > (t - w)).astype(np.float64) / w
        cntf = np.minimum(t + 1, w).astype(np.float64)
        first = ((tp <= t) & (tp > t - w)).astype(np.float64) / cntf - np.eye(128)
        for kind, m in enumerate((cur, prev, first)):
            o = C_BAND + (kind * 4 + g) * 128
            c[:, o:o + 128] = m.astype(np.float32)
    c[:, C_MISC] = idx
    c[:, C_MISC + 1] = 2 * idx
    c[:, C_MISC + 2] = 2 * idx + 1
    c[:, C_MISC + 3:C_MISC + 35] = np.arange(32)[None, :]
    c[:, C_MISC + 35:C_MISC + 51] = (16 - np.arange(16))[None, :]
    c[:, C_US:C_US + 128] = (idx[:, None] < idx[None, :])
    c[:, C_S:C_S + 64] = np.arange(64)[None, :]
    return c


_NC_CACHE = {}


def _prep_inputs(inp):
    f = lambda a: np.ascontiguousarray(np.asarray(a, dtype=np.float32))
    pp = np.zeros((128, NPP), np.float32)
    pp[:, PP_G0:PP_G0 + 8] = f(inp["ln0_g"]).reshape(8, 128).T
    pp[:, PP_B0:PP_B0 + 8] = f(inp["ln0_b"]).reshape(8, 128).T
    pp[:, PP_G1:PP_G1 + 8] = f(inp["ln1_g"])[0].reshape(8, 128).T
    pp[:, PP_B1:PP_B1 + 8] = f(inp["ln1_b"])[0].reshape(8, 128).T
    cw = f(inp["conv_w"])[0]
    pp[:, PP_CW:PP_CW + 32] = cw.reshape(4, 8, 128).transpose(2, 1, 0).reshape(128, 32)
    pp[:, PP_CB:PP_CB + 8] = f(inp["conv_b"])[0].reshape(8, 128).T
    pp[:, PP_PS:PP_PS + 4] = f(inp["pool_scale"])[0].reshape(4, 128).T
    pp[:, PP_PB:PP_PB + 4] = f(inp["b_pool"])[0].reshape(4, 128).T
    rowp = np.concatenate([f(inp["dt_bias"])[0], f(inp["a_log"])[0], f(inp["d_skip"])[0],
                           f(inp["b_router_group"])[0], f(inp["b_router_expert"])[0], f(inp["ssm_norm_g"])[0]]).astype(np.float32)
    assert rowp.shape[0] == NRP
    ln2rows = np.stack([f(inp["ln2_g"])[0], f(inp["ln2_b"])[0]])
    wr = np.ascontiguousarray(np.concatenate([f(inp["w_router_group"])[0], f(inp["w_router_expert"])[0]], axis=1))
    shared = {
        "consts": _host_consts(), "pp": pp, "rowp": rowp, "ln2rows": ln2rows, "wr": wr,
        "identb": np.eye(128, dtype=np.float32).astype(ml_dtypes.bfloat16),
        "w_in": f(inp["w_in"])[0], "w_out": f(inp["w_out"])[0], "w_pool": f(inp["w_pool"])[0],
        "w_gate": np.ascontiguousarray(f(inp["w_gate"])[0].reshape(NE, 8, 128, 512).transpose(0, 2, 1, 3)).reshape(NE * 256, 2048),
        "w_up": np.ascontiguousarray(f(inp["w_up"])[0].reshape(NE, 8, 128, 512).transpose(0, 2, 1, 3)).reshape(NE * 256, 2048),
        "w_down": np.ascontiguousarray(f(inp["w_down"])[0].reshape(NE, 4, 128, 1024).transpose(0, 2, 1, 3)).reshape(NE * 256, 2048),
    }
    return shared


def kernel(**inputs):
    x = np.asarray(inputs["x"], dtype=np.float32)
    shared = _prep_inputs(inputs)
    if "nc" not in _NC_CACHE:
        _NC_CACHE["nc"] = build(4)
    nc = _NC_CACHE["nc"]
    in_maps = []
    for i in range(NCORES):
        m = dict(shared)
        m["xc"] = np.ascontiguousarray(x[2 * i:2 * i + 2].reshape(TOK_PER_CORE, D))
        in_maps.append(m)
    res = run_bass_kernel_spmd(nc, in_maps, core_ids=list(range(NCORES)))
    out = np.empty((16, 2048, D), np.float32)
    for i in range(NCORES):
        out[2 * i:2 * i + 2] = np.asarray(res.results[i]["yc"]).reshape(2, 2048, D)
    return out
```
